# Optimizing a Trainium2 kernel written in Bass

```python
import math
import jax, jax.numpy as jnp
from jax import lax
import numpy as np

D_MODEL = 1024
BATCH = 8
SEQ = 4096
DEPTH = 2

MEM_LEN = 256
HEAD_DIM = 64
ROPE_THETA = 10000.0
BLOCK_Q = 128
EPS = 1e-5
A_HEADS = 4
A_QK = A_HEADS * 2 * HEAD_DIM
A_V = A_HEADS * 2 * HEAD_DIM
A_OUT = A_V
B_HEADS = 8
B_W = B_HEADS * HEAD_DIM
B_OUT = B_W
FORGET_BIAS_LO = 1.0
FORGET_BIAS_HI = 5.0
C_PATTERNS = ((128, 1), (512, 4), (2048, 16))
C_GROUPS = len(C_PATTERNS)
C_SLOTS = 4
C_W = C_GROUPS * C_SLOTS * HEAD_DIM
C_OUT = C_SLOTS * HEAD_DIM
N_BRANCH = 3
IN_SIZES = (A_QK, A_QK, A_V, B_W, B_W, B_W, B_HEADS, C_W, C_W, C_W)
IN_COLS = sum(IN_SIZES)
IN_SPLITS = np.cumsum(IN_SIZES)[:-1].tolist()
X_HEADS = 4
X_HEAD_DIM = D_MODEL // X_HEADS
N_GROUPS = 4
EXPERTS_PER_GROUP = 4
EXPERT_TOPK = 2
D_EXPERT = D_MODEL // 4
DN_ALPHA = (2 * DEPTH) ** 0.25
DN_BETA = (8 * DEPTH) ** -0.25
N_SUBLAYERS = 3

kernel_name = "hybrid_diff_fox_dilated_hmoe_deepnorm"


def layer_norm(x, g, b):
    xf = x.astype(jnp.float32)
    mu = jnp.mean(xf, axis=-1, keepdims=True)
    var = jnp.mean(jnp.square(xf - mu), axis=-1, keepdims=True)
    return ((xf - mu) * lax.rsqrt(var + EPS) * g + b).astype(x.dtype)


def rms_norm(x, g):
    xf = x.astype(jnp.float32)
    return (xf * lax.rsqrt(jnp.mean(jnp.square(xf), axis=-1, keepdims=True) + EPS) * g).astype(x.dtype)


def rope(x, positions):
    half = x.shape[-1] // 2
    inv = ROPE_THETA ** (-jnp.arange(half, dtype=jnp.float32) / half)
    ang = positions.astype(jnp.float32)[:, :, None, None] * inv
    cos, sin = jnp.cos(ang), jnp.sin(ang)
    xf = x.astype(jnp.float32)
    x1, x2 = xf[..., :half], xf[..., half:]
    return jnp.concatenate([x1 * cos - x2 * sin, x2 * cos + x1 * sin], axis=-1).astype(x.dtype)


def _to_blocks(t):
    b, s = t.shape[:2]
    return jnp.moveaxis(t.reshape((b, s // BLOCK_Q, BLOCK_Q) + t.shape[2:]), 1, 0)


def _from_blocks(t):
    n, b, q = t.shape[:3]
    return jnp.moveaxis(t, 0, 1).reshape((b, n * q) + t.shape[3:])


def diff_attention(q1, q2, k1, k2, v, lam):
    seq = q1.shape[1]
    scale = HEAD_DIM ** -0.5
    kpos = jnp.arange(seq)

    def block(args):
        i, qb1, qb2 = args
        qpos = i * BLOCK_Q + jnp.arange(BLOCK_Q)
        causal = kpos[None, :] <= qpos[:, None]

        def probs(qb, k):
            sc = jnp.einsum('bqhd,bkhd->bhqk', qb, k).astype(jnp.float32) * scale
            return jax.nn.softmax(jnp.where(causal, sc, -jnp.inf), axis=-1)

        p = probs(qb1, k1) - lam * probs(qb2, k2)
        return jnp.einsum('bhqk,bkhe->bqhe', p.astype(v.dtype), v)

    out = lax.map(block, (jnp.arange(seq // BLOCK_Q), _to_blocks(q1), _to_blocks(q2)))
    return _from_blocks(out)


def forgetting_attention(q, k, v, logf):
    seq = q.shape[1]
    scale = HEAD_DIM ** -0.5
    c = jnp.cumsum(logf, axis=1)
    c_keys = jnp.transpose(c, (0, 2, 1))
    kpos = jnp.arange(seq)

    def block(args):
        i, qb, cb = args
        qpos = i * BLOCK_Q + jnp.arange(BLOCK_Q)
        causal = kpos[None, :] <= qpos[:, None]
        decay = jnp.transpose(cb, (0, 2, 1))[..., :, None] - c_keys[:, :, None, :]
        sc = jnp.einsum('bqhd,bkhd->bhqk', qb, k).astype(jnp.float32) * scale + decay
        p = jax.nn.softmax(jnp.where(causal, sc, -jnp.inf), axis=-1)
        return jnp.einsum('bhqk,bkhe->bqhe', p.astype(v.dtype), v)

    out = lax.map(block, (jnp.arange(seq // BLOCK_Q), _to_blocks(q), _to_blocks(c)))
    return _from_blocks(out)


def dilated_window_attention(q, k, v, window, dilation):
    b, seq, h, dh = q.shape
    n = window // dilation
    span = n * dilation
    padded = -(-seq // span) * span
    nb = padded // span
    m_len = padded // dilation
    scale = HEAD_DIM ** -0.5

    def strided(t):
        t = jnp.pad(t, ((0, 0), (0, padded - seq), (0, 0), (0, 0)))
        t = jnp.transpose(t.reshape(b, m_len, dilation, h, dh), (0, 2, 1, 3, 4))
        return t.reshape(b, dilation, nb, n, h, dh)

    def band(t):
        prev = jnp.pad(t, ((0, 0), (0, 0), (1, 0), (0, 0), (0, 0), (0, 0)))[:, :, :-1]
        return jnp.concatenate([prev, t], axis=3)

    qs, kb, vb = strided(q), band(strided(k)), band(strided(v))
    sc = jnp.einsum('bcnqhe,bcnkhe->bcnhqk', qs, kb).astype(jnp.float32) * scale
    qi = jnp.arange(n)[:, None]
    kj = jnp.arange(2 * n)[None, :]
    dist = n + qi - kj
    in_window = (dist >= 0) & (dist <= n)
    has_prev = (jnp.arange(nb)[:, None, None] > 0) | (kj[None] >= n)
    mask = (in_window[None] & has_prev)[:, None]
    sc = jnp.where(mask, sc, -jnp.inf)
    lse = jax.nn.logsumexp(sc, axis=-1)
    p = jnp.exp(sc - lse[..., None])
    o = jnp.einsum('bcnhqk,bcnkhe->bcnqhe', p.astype(v.dtype), vb)
    o = jnp.transpose(o.reshape(b, dilation, m_len, h, dh), (0, 2, 1, 3, 4)).reshape(b, padded, h, dh)[:, :seq]
    lse = jnp.transpose(lse, (0, 1, 2, 4, 3)).reshape(b, dilation, m_len, h)
    lse = jnp.transpose(lse, (0, 2, 1, 3)).reshape(b, padded, h)[:, :seq]
    return o, lse


def dilated_mixture(q, k, v):
    outs, lses = [], []
    for g, (window, dilation) in enumerate(C_PATTERNS):
        o, l = dilated_window_attention(q[:, :, g], k[:, :, g], v[:, :, g], window, dilation)
        outs.append(o)
        lses.append(l)
    wts = jax.nn.softmax(jnp.stack(lses, axis=0), axis=0)
    return jnp.einsum('gbsh,gbshe->bshe', wts.astype(q.dtype), jnp.stack(outs, axis=0))


def hybrid_mixer(x, positions, w_in, b_forget, diff_lambda, diff_subln, lam_init,
                 w_branch_a, w_branch_b, w_branch_c, w_gate, b_gate, w_out):
    b, s, _ = x.shape
    proj = x @ w_in
    qa, ka, va, qb, kb, vb, fb, qc, kc, vc = jnp.split(proj, IN_SPLITS, axis=-1)
    qa = rope(qa.reshape(b, s, 2 * A_HEADS, HEAD_DIM), positions).reshape(b, s, A_HEADS, 2, HEAD_DIM)
    ka = rope(ka.reshape(b, s, 2 * A_HEADS, HEAD_DIM), positions).reshape(b, s, A_HEADS, 2, HEAD_DIM)
    va = va.reshape(b, s, A_HEADS, 2 * HEAD_DIM)
    lf = diff_lambda.astype(jnp.float32)
    lam = jnp.exp(jnp.dot(lf[0], lf[1])) - jnp.exp(jnp.dot(lf[2], lf[3])) + lam_init
    oa = diff_attention(qa[:, :, :, 0], qa[:, :, :, 1], ka[:, :, :, 0], ka[:, :, :, 1], va, lam)
    oa = (rms_norm(oa, diff_subln) * (1.0 - lam_init)).reshape(b, s, A_OUT)
    logf = jax.nn.log_sigmoid(fb.astype(jnp.float32) + b_forget.astype(jnp.float32))
    ob = forgetting_attention(qb.reshape(b, s, B_HEADS, HEAD_DIM), kb.reshape(b, s, B_HEADS, HEAD_DIM),
                              vb.reshape(b, s, B_HEADS, HEAD_DIM), logf).reshape(b, s, B_OUT)
    ch = C_GROUPS * C_SLOTS
    qc = rope(qc.reshape(b, s, ch, HEAD_DIM), positions).reshape(b, s, C_GROUPS, C_SLOTS, HEAD_DIM)
    kc = rope(kc.reshape(b, s, ch, HEAD_DIM), positions).reshape(b, s, C_GROUPS, C_SLOTS, HEAD_DIM)
    vc = vc.reshape(b, s, C_GROUPS, C_SLOTS, HEAD_DIM)
    oc = dilated_mixture(qc, kc, vc).reshape(b, s, C_OUT)
    ga, gb, gc = jnp.split(jax.nn.sigmoid(x @ w_gate + b_gate), N_BRANCH, axis=-1)
    merged = ga * (oa @ w_branch_a) + gb * (ob @ w_branch_b) + gc * (oc @ w_branch_c)
    return merged @ w_out


def memory_cross_attention(x, mem, w_q, w_k, w_v, w_o):
    b, s, _ = x.shape
    m = mem.shape[1]
    q = (x @ w_q).reshape(b, s, X_HEADS, X_HEAD_DIM)
    k = (mem @ w_k).reshape(b, m, X_HEADS, X_HEAD_DIM)
    v = (mem @ w_v).reshape(b, m, X_HEADS, X_HEAD_DIM)
    sc = jnp.einsum('bqhd,bkhd->bhqk', q, k).astype(jnp.float32) * (X_HEAD_DIM ** -0.5)
    p = jax.nn.softmax(sc, axis=-1)
    o = jnp.einsum('bhqk,bkhd->bqhd', p.astype(v.dtype), v).reshape(b, s, D_MODEL)
    return o @ w_o


def hierarchical_moe(x, w_rg, b_rg, w_re, b_re, w_gate_up, w_up, w_down):
    b, s, d = x.shape
    t = x.reshape(b * s, d)
    g_logits = (t @ w_rg).astype(jnp.float32) + b_rg
    g_probs = jax.nn.softmax(g_logits, axis=-1)
    g_top, g_idx = lax.top_k(g_logits, 1)
    g_idx = g_idx[:, 0]
    g_gate = jnp.take_along_axis(g_probs, g_idx[:, None], axis=-1)[:, 0]
    e_logits = jnp.einsum('td,gde->tge', t, w_re).astype(jnp.float32) + b_re
    e_sel = jnp.take_along_axis(e_logits, g_idx[:, None, None], axis=1)[:, 0]
    top_v, top_i = lax.top_k(e_sel, EXPERT_TOPK)
    top_w = jax.nn.softmax(top_v, axis=-1)
    e_w = jnp.sum(jax.nn.one_hot(top_i, EXPERTS_PER_GROUP, dtype=jnp.float32) * top_w[..., None], axis=1)
    gate = jax.nn.one_hot(g_idx, N_GROUPS, dtype=jnp.float32)[:, :, None] * (g_gate[:, None] * e_w)[:, None, :]
    y = jnp.zeros_like(t)
    for g in range(N_GROUPS):
        h = jax.nn.silu(jnp.einsum('td,edf->tef', t, w_gate_up[g])) * jnp.einsum('td,edf->tef', t, w_up[g])
        y = y + jnp.einsum('tef,efd->td', h * gate[:, g, :, None].astype(h.dtype), w_down[g])
    return y.reshape(b, s, d)


def setup_inputs(seed: int = 0) -> dict:
    key = jax.random.key(seed)
    ks = iter(jax.random.split(key, 32))

    def nrm(shape, scale):
        return jax.random.normal(next(ks), shape, jnp.float32) * scale

    fan = D_MODEL ** -0.5
    L = DEPTH
    x = nrm((BATCH, SEQ, D_MODEL), 1.0)
    mem = nrm((BATCH, MEM_LEN, D_MODEL), 1.0)
    offsets = jax.random.randint(next(ks), (BATCH, 1), 0, 1024, dtype=jnp.int32)
    positions = offsets + jnp.arange(SEQ, dtype=jnp.int32)[None, :]
    return {
        "x": x,
        "mem": mem,
        "positions": positions,
        "w_in": nrm((L, D_MODEL, IN_COLS), fan),
        "b_forget": jax.random.uniform(next(ks), (L, B_HEADS), jnp.float32, FORGET_BIAS_LO, FORGET_BIAS_HI),
        "diff_lambda": nrm((L, 4, HEAD_DIM), 0.1),
        "diff_subln": 1.0 + nrm((L, 2 * HEAD_DIM), 0.02),
        "w_branch_a": nrm((L, A_OUT, D_MODEL), A_OUT ** -0.5 * DN_BETA),
        "w_branch_b": nrm((L, B_OUT, D_MODEL), B_OUT ** -0.5 * DN_BETA),
        "w_branch_c": nrm((L, C_OUT, D_MODEL), C_OUT ** -0.5 * DN_BETA),
        "w_gate": nrm((L, D_MODEL, N_BRANCH * D_MODEL), fan),
        "b_gate": nrm((L, N_BRANCH * D_MODEL), 0.02),
        "w_out": nrm((L, D_MODEL, D_MODEL), fan * DN_BETA),
        "w_xq": nrm((L, D_MODEL, D_MODEL), fan),
        "w_xk": nrm((L, D_MODEL, D_MODEL), fan),
        "w_xv": nrm((L, D_MODEL, D_MODEL), fan),
        "w_xo": nrm((L, D_MODEL, D_MODEL), fan * DN_BETA),
        "w_route_group": nrm((L, D_MODEL, N_GROUPS), fan),
        "b_route_group": nrm((L, N_GROUPS), 0.01),
        "w_route_expert": nrm((L, N_GROUPS, D_MODEL, EXPERTS_PER_GROUP), fan),
        "b_route_expert": nrm((L, N_GROUPS, EXPERTS_PER_GROUP), 0.01),
        "w_expert_gate": nrm((L, N_GROUPS, EXPERTS_PER_GROUP, D_MODEL, D_EXPERT), fan),
        "w_expert_up": nrm((L, N_GROUPS, EXPERTS_PER_GROUP, D_MODEL, D_EXPERT), fan),
        "w_expert_down": nrm((L, N_GROUPS, EXPERTS_PER_GROUP, D_EXPERT, D_MODEL), D_EXPERT ** -0.5 * DN_BETA),
        "ln_g": 1.0 + nrm((L, N_SUBLAYERS, D_MODEL), 0.02),
        "ln_b": nrm((L, N_SUBLAYERS, D_MODEL), 0.02),
    }


def reference(x, mem, positions, w_in, b_forget, diff_lambda, diff_subln, w_branch_a, w_branch_b,
              w_branch_c, w_gate, b_gate, w_out, w_xq, w_xk, w_xv, w_xo, w_route_group, b_route_group,
              w_route_expert, b_route_expert, w_expert_gate, w_expert_up, w_expert_down, ln_g, ln_b):
    for l in range(DEPTH):
        lam_init = 0.8 - 0.6 * math.exp(-0.3 * l)
        h = hybrid_mixer(x, positions, w_in[l], b_forget[l], diff_lambda[l], diff_subln[l], lam_init,
                         w_branch_a[l], w_branch_b[l], w_branch_c[l], w_gate[l], b_gate[l], w_out[l])
        x = layer_norm(DN_ALPHA * x + h, ln_g[l, 0], ln_b[l, 0])
        h = memory_cross_attention(x, mem, w_xq[l], w_xk[l], w_xv[l], w_xo[l])
        x = layer_norm(DN_ALPHA * x + h, ln_g[l, 1], ln_b[l, 1])
        h = hierarchical_moe(x, w_route_group[l], b_route_group[l], w_route_expert[l], b_route_expert[l],
                             w_expert_gate[l], w_expert_up[l], w_expert_down[l])
        x = layer_norm(DN_ALPHA * x + h, ln_g[l, 2], ln_b[l, 2])
    return x
```

```python
import contextlib
import math
import numpy as np
import concourse.bass as bass
import concourse.mybir as mybir
from concourse.bass_utils import run_bass_kernel_spmd

F32 = mybir.dt.float32
BF16 = mybir.dt.bfloat16
I32 = mybir.dt.int32
AF = mybir.ActivationFunctionType
ALU = mybir.AluOpType
AX = mybir.AxisListType

T = 4096
D = 1024
NT = 8
TS = 512
DEPTH = 2
NCOL = 5384
NEG = -30000.0
EPS = 1e-5
ALPHA = (2 * DEPTH) ** 0.25
KDMA = 8


class Buf:
    def __init__(self, ap, disjoint=False):
        self.ap = ap
        self.w = {}
        self.r = {}
        self.disjoint = disjoint

    def __getitem__(self, k):
        return self.ap[k]


class Sched:
    def __init__(self, nc, es):
        self.nc = nc
        self.E = {'pe': nc.tensor, 'act': nc.scalar, 'dve': nc.vector, 'pool': nc.gpsimd, 'sp': nc.sync,
                  'bg': nc.gpsimd}
        self.psem = {}
        self.pcnt = {}
        for e in ['pe', 'act', 'dve', 'pool']:
            self.psem[e] = es.enter_context(nc.semaphore('p_' + e))
            self.pcnt[e] = 0
        self.dsem = {}
        self.dcnt = {}
        self.drr = {}
        for q in ['sp', 'pool', 'bg']:
            self.dsem[q] = [es.enter_context(nc.semaphore('d_%s%d' % (q, i))) for i in range(KDMA)]
            self.dcnt[q] = [0] * KDMA
            self.drr[q] = 0
        self.seen = {}
        self.nops = 0

    def _wait(self, e, deps):
        if e == 'bg':
            e = 'pool'
        for name, (sem, val) in deps.items():
            key = (e, name)
            if self.seen.get(key, 0) >= val:
                continue
            self.E[e].wait_ge(sem, val)
            self.seen[key] = val

    def _deps(self, e, reads, writes):
        deps = {}

        def add(d):
            for name, (sem, val) in d.items():
                if val > deps.get(name, (None, 0))[1]:
                    deps[name] = (sem, val)
        for b in reads:
            add(b.w)
        for b in writes:
            add(b.r)
            if not b.disjoint:
                add(b.w)
        if e == 'pe':
            deps.pop('p_pe', None)
        return deps

    def _record(self, tok, reads, writes):
        name, sem, val = tok
        for b in reads:
            if val > b.r.get(name, (None, 0))[1]:
                b.r[name] = (sem, val)
        for b in writes:
            if b.disjoint:
                if val > b.w.get(name, (None, 0))[1]:
                    b.w[name] = (sem, val)
            else:
                b.w = {name: (sem, val)}
                b.r = {}

    def op(self, e, emit, reads=(), writes=()):
        self._wait(e, self._deps(e, reads, writes))
        inst = emit(self.E[e])
        self.pcnt[e] += 1
        inst.then_inc(self.psem[e], 1)
        self._record(('p_' + e, self.psem[e], self.pcnt[e]), reads, writes)
        self.nops += 1

    def dma(self, q, out, in_, reads=(), writes=()):
        deps = self._deps(q, reads, writes)
        i = self.drr[q]
        self.drr[q] = (i + 1) % KDMA
        sem = self.dsem[q][i]
        name = 'd_%s%d' % (q, i)
        if self.dcnt[q][i] > 0:
            deps[name] = (sem, self.dcnt[q][i])
        self._wait(q, deps)
        self.E[q].dma_start(out=out, in_=in_).then_inc(sem, 16)
        self.dcnt[q][i] += 16
        self._record((name, sem, self.dcnt[q][i]), reads, writes)
        self.nops += 1

    def barrier(self, final=False):
        allt = {}
        for e in self.psem:
            if self.pcnt[e] > 0:
                allt['p_' + e] = (self.psem[e], self.pcnt[e])
        for q in self.dsem:
            if q == 'bg' and not final:
                continue
            for i in range(KDMA):
                if self.dcnt[q][i] > 0:
                    allt['d_%s%d' % (q, i)] = (self.dsem[q][i], self.dcnt[q][i])
        for e in ['pe', 'act', 'dve', 'pool', 'sp']:
            d = dict(allt)
            if e in self.psem:
                d.pop('p_' + e, None)
            self._wait(e, d)


class Ring:
    def __init__(self, bufs):
        self.bufs = bufs
        self.i = 0

    def next(self):
        b = self.bufs[self.i]
        self.i = (self.i + 1) % len(self.bufs)
        return b


def pipeline(n, first, second, depth=1):
    for i in range(n + depth):
        if i < n:
            first(i)
        if i >= depth:
            second(i - depth)


class K:
    def __init__(self, nc, es, debug=False, layers=DEPTH, phases=None):
        self.nc = nc
        self.es = es
        self.S = Sched(nc, es)
        self.debug = debug
        self.layers = layers
        self.phases = phases
        self.dram = {}
        self.wbf = {}
        self.experts_cast = set()
        self._deferred_loads = []

    def din(self, name, shape, dt=F32):
        t = self.nc.dram_tensor(name, list(shape), dt, kind="ExternalInput").ap()
        self.dram[name] = t
        return t

    def dscr(self, name, shape, dt):
        kind = "ExternalOutput" if self.debug else "Internal"
        t = self.nc.dram_tensor(name, list(shape), dt, kind=kind).ap()
        return Buf(t, disjoint=True)

    def sb(self, st, name, shape, dt, disjoint=False):
        self.uid = getattr(self, 'uid', 0) + 1
        t = st.enter_context(self.nc.sbuf_tensor('%s_%d' % (name, self.uid), list(shape), dt))
        return Buf(t, disjoint=disjoint)

    def ps(self, st, name, shape=(128, 512), dt=F32):
        self.uid = getattr(self, 'uid', 0) + 1
        t = st.enter_context(self.nc.psum_tensor('%s_%d' % (name, self.uid), list(shape), dt))
        return Buf(t)

    def ring(self, st, name, n, shape, dt, psum=False):
        return Ring([(self.ps if psum else self.sb)(st, '%s%d' % (name, i), shape, dt) for i in range(n)])

    def mm(self, out_b, out_ap, lhsT_b, lhsT_ap, rhs_b, rhs_ap, start, stop):
        self.S.op('pe', lambda e: e.matmul(out_ap, lhsT=lhsT_ap, rhs=rhs_ap, start=start, stop=stop),
                  reads=[lhsT_b, rhs_b], writes=[out_b])

    def act(self, out_b, out_ap, in_b, in_ap, func, extra_reads=(), **kw):
        self.S.op('act', lambda e: e.activation(out=out_ap, in_=in_ap, func=func, **kw),
                  reads=[in_b] + list(extra_reads), writes=[out_b])

    def tt(self, out_b, out_ap, a_b, a_ap, b_b, b_ap, op, eng='dve'):
        self.S.op(eng, lambda e: e.tensor_tensor(out=out_ap, in0=a_ap, in1=b_ap, op=op),
                  reads=[a_b, b_b], writes=[out_b])

    def ts(self, out_b, out_ap, a_b, a_ap, s1, s2, op0, op1=None, extra_reads=(), eng='dve'):
        if op1 is None:
            f = lambda e: e.tensor_scalar(out=out_ap, in0=a_ap, scalar1=s1, scalar2=None, op0=op0)
        else:
            f = lambda e: e.tensor_scalar(out=out_ap, in0=a_ap, scalar1=s1, scalar2=s2, op0=op0, op1=op1)
        self.S.op(eng, f, reads=[a_b] + list(extra_reads), writes=[out_b])

    def stt(self, out_b, out_ap, a_b, a_ap, scalar, b_b, b_ap, op0, op1, extra_reads=()):
        self.S.op('dve', lambda e: e.scalar_tensor_tensor(out=out_ap, in0=a_ap, scalar=scalar, in1=b_ap,
                                                          op0=op0, op1=op1),
                  reads=[a_b, b_b] + list(extra_reads), writes=[out_b])

    def cp(self, out_b, out_ap, in_b, in_ap, eng='dve'):
        self.S.op(eng, lambda e: e.tensor_copy(out=out_ap, in_=in_ap), reads=[in_b], writes=[out_b])

    def memset(self, b, ap, val, eng='dve'):
        self.S.op(eng, lambda e: e.memset(ap, val), writes=[b])

    def recip(self, out_b, out_ap, in_b, in_ap):
        self.S.op('dve', lambda e: e.reciprocal(out=out_ap, in_=in_ap), reads=[in_b], writes=[out_b])

    def ld(self, dst_b, dst_ap, src, q='sp', src_b=None):
        self.S.dma(q, dst_ap, src, reads=[src_b] if src_b is not None else [], writes=[dst_b])

    def stor(self, dst_b, dst_ap, src_b, src_ap, q='sp'):
        self.S.dma(q, dst_ap, src_ap, reads=[src_b], writes=[dst_b])

    def declare(self):
        L = DEPTH
        d = self.din
        self.x = d("x", [T, D])
        self.mem = d("mem", [256, D])
        self.pos = d("pos", [1, T], I32)
        self.w_in = d("w_in", [L, D, NCOL])
        self.b_forget = d("b_forget", [L, 8, 1])
        self.diff_lambda = d("diff_lambda", [L, 1, 256])
        self.diff_subln = d("diff_subln", [L, 128, 1])
        self.w_ba = d("w_branch_a", [L, 512, D])
        self.w_bb = d("w_branch_b", [L, 512, D])
        self.w_bc = d("w_branch_c", [L, 256, D])
        self.w_gate = d("w_gate", [L, D, 3072])
        self.b_gate = d("b_gate", [L, 128, 24])
        self.w_out = d("w_out", [L, D, D])
        self.w_xq = d("w_xq", [L, D, D])
        self.w_xk = d("w_xk", [L, D, D])
        self.w_xv = d("w_xv", [L, D, D])
        self.w_xo = d("w_xo", [L, D, D])
        self.w_r = d("w_r", [L, D, 20])
        self.b_r = d("b_r", [L, 1, 20])
        self.w_eg = d("w_eg", [L, 16, D, 256])
        self.w_eu = d("w_eu", [L, 16, D, 256])
        self.w_ed = d("w_ed", [L, 16, 256, D])
        self.ln_g = d("ln_g", [L, 3, D])
        self.ln_b = d("ln_b", [L, 3, D])
        self.c_ident = d("c_ident", [128, 128])
        self.c_masks = d("c_masks", [128, 4 * 512])
        self.c_mprev = d("c_mprev", [128, 128])
        self.c_sel = d("c_sel", [16, 2048])
        self.c_invf = d("c_invf", [128, 1])
        self.out = Buf(self.nc.dram_tensor("out", [T, D], F32, kind="ExternalOutput").ap(), disjoint=True)
        s = self.dscr
        self.xT = s("s_xT", [D, T], BF16)
        self.xres = s("s_xres", [T, D], F32)
        self.qaT = s("s_qaT", [512, T], BF16)
        self.kaT = s("s_kaT", [512, T], BF16)
        self.qbT = s("s_qbT", [512, T], BF16)
        self.kbT = s("s_kbT", [512, T], BF16)
        self.qbaug = s("s_qbaug", [8, 6, T], BF16)
        self.kbaug = s("s_kbaug", [8, 6, T], BF16)
        self.qcT = s("s_qcT", [768, T], BF16)
        self.kcT = s("s_kcT", [768, T], BF16)
        self.va = s("s_va", [T, 512], BF16)
        self.vb = s("s_vb", [T, 520], BF16)
        self.vc = s("s_vc", [T, 780], BF16)
        self.oaT = s("s_oaT", [512, T], BF16)
        self.obT = s("s_obT", [512, T], BF16)
        self.ocT = s("s_ocT", [256, T], BF16)
        self.x1 = s("s_x1", [T, D], F32)
        self.x1T = s("s_x1T", [D, T], BF16)
        self.x2 = s("s_x2", [T, D], F32)
        self.x2T = s("s_x2T", [D, T], BF16)
        self.gateT = s("s_gateT", [16, T], F32)
        mk = lambda n, shp: Buf(self.nc.dram_tensor(n, shp, BF16, kind="Internal").ap(), disjoint=True)
        self.wegb = mk("s_wegb", [DEPTH, 16, D, 256])
        self.weub = mk("s_weub", [DEPTH, 16, D, 256])
        self.wedb = mk("s_wedb", [DEPTH, 16, 256, D])

    def want(self, ph):
        return self.phases is None or ph in self.phases

    def consts(self, st, need_masks=False):
        c = {}
        c['ident_f'] = self.sb(st, 'ident_f', [128, 128], F32)
        c['ident_b'] = self.sb(st, 'ident_b', [128, 128], BF16)
        self.ld(c['ident_f'], c['ident_f'][:], self.c_ident)
        self.ld(c['ident_b'], c['ident_b'][:], self.c_ident, q='pool')
        c['ones_b'] = self.sb(st, 'ones_b', [128, 128], BF16)
        self.memset(c['ones_b'], c['ones_b'][:], 1.0)
        c['ones_f'] = self.sb(st, 'ones_f', [128, 128], F32)
        self.memset(c['ones_f'], c['ones_f'][:], 1.0)
        if need_masks:
            c['masks'] = self.sb(st, 'masks', [128, 4 * 512], BF16)
            self.ld(c['masks'], c['masks'][:], self.c_masks, q='pool')
            c['mprev'] = self.sb(st, 'mprev', [128, 128], BF16)
            self.ld(c['mprev'], c['mprev'][:], self.c_mprev, q='pool')
        return c

    def transpose_to_xT(self, c, x_b, x_ap, psT, xT_b, sub, f32_b=None):
        for half in range(2):
            p = psT.next()
            for k in range(4):
                cc = half * 4 + k
                self.S.op('pe', lambda e, cc=cc, k=k, p=p: e.transpose(p[:, k * 128:(k + 1) * 128],
                                                                     x_ap[:, cc * 128:(cc + 1) * 128],
                                                                     c['ident_f'][:]),
                          reads=[x_b, c['ident_f']], writes=[p])
            self.S.op('act', lambda e, p=p, half=half: e.activation(
                out=xT_b[:, half * 4:half * 4 + 4, sub * 128:(sub + 1) * 128],
                in_=p[:].rearrange("p (k t) -> p k t", k=4), func=AF.Copy), reads=[p], writes=[xT_b])
            if f32_b is not None:
                self.S.op('act', lambda e, p=p, half=half: e.activation(
                    out=f32_b[:, half * 4:half * 4 + 4, :],
                    in_=p[:].rearrange("p (k t) -> p k t", k=4), func=AF.Copy), reads=[p], writes=[f32_b])

    def layer_norm(self, st_bufs, r_b, r_ap, g_b, b_b, out_b, out_ap, gb_eng='dve'):
        stats, mv, sc = st_bufs['stats'], st_bufs['mv'], st_bufs['sc']
        for k in range(2):
            self.S.op('dve', lambda e, k=k: e.bn_stats(out=stats[:, k * 6:(k + 1) * 6],
                                                      in_=r_ap[:, k * 512:(k + 1) * 512]),
                      reads=[r_b], writes=[stats])
        self.S.op('dve', lambda e: e.bn_aggr(out=mv[:, 0:2], in_=stats[:, 0:12]), reads=[stats], writes=[mv])
        self.ts(sc, sc[:, 0:1], mv, mv[:, 1:2], EPS, None, ALU.add)
        self.act(sc, sc[:, 1:2], sc, sc[:, 0:1], AF.Ln)
        self.act(sc, sc[:, 2:3], sc, sc[:, 1:2], AF.Exp, scale=-0.5)
        self.ts(sc, sc[:, 3:4], mv, mv[:, 0:1], sc[:, 2:3], -1.0, ALU.mult, ALU.mult, extra_reads=[sc])
        self.act(out_b, out_ap, r_b, r_ap, AF.Identity, extra_reads=[sc], scale=sc[:, 2:3], bias=sc[:, 3:4])
        self.tt(out_b, out_ap, out_b, out_ap, g_b, g_b[:], ALU.mult, eng=gb_eng)
        self.tt(out_b, out_ap, out_b, out_ap, b_b, b_b[:], ALU.add, eng=gb_eng)

    def phase0(self):
        with contextlib.ExitStack() as st:
            c = self.consts(st)
            xin = self.ring(st, 'p0x', 3, [128, D], F32)
            xTt = self.ring(st, 'p0xT', 2, [128, 8, TS], BF16)
            psT = self.ring(st, 'p0ps', 4, [128, 512], F32, psum=True)
            for t in range(NT):
                xt = xTt.next()
                for sub in range(4):
                    xb = xin.next()
                    r0 = t * TS + sub * 128
                    self.ld(xb, xb[:], self.x[r0:r0 + 128, :])
                    self.transpose_to_xT(c, xb, xb[:], psT, xt, sub)
                self.stor(self.xT, self.xT.ap[:, t * TS:(t + 1) * TS].rearrange("(c p) t -> p c t", p=128),
                          xt, xt[:])
            self.S.barrier()

    def phaseP(self, l):
        S = self.S
        st0 = contextlib.ExitStack()
        fbuf = self.sb(st0, 'fbuf', [8, T], F32, disjoint=True)
        with contextlib.ExitStack() as st:
            groups = [(0, 1536), (1536, 3080), (3080, 4616), (4616, 5384)]
            wg = []
            for gi, (a, b) in enumerate(groups):
                wb = self.sb(st, 'win%d' % gi, [128, 8, b - a], BF16, disjoint=True)
                wg.append(wb)
            order = [0, 2, 1, 3]
            for gi in order:
                a, b = groups[gi]
                if ('w_in', l) in self.wbf:
                    wbb_ = self.wbf[('w_in', l)]
                    for c0 in range(0, 8, 2):
                        self.ld(wg[gi], wg[gi][:, c0:c0 + 2, :],
                                wbb_.ap[c0 * 128:(c0 + 2) * 128, a:b].rearrange("(c p) n -> p c n", p=128),
                                src_b=wbb_)
                else:
                    for cc in range(8):
                        self.ld(wg[gi], wg[gi][:, cc, :], self.w_in[l, cc * 128:(cc + 1) * 128, a:b], q='pool')

            if self.want('C2%d' % l) and l not in self.experts_cast:
                self.precast(l)
                self.experts_cast.add(l)

            def wslice(col, n):
                for gi, (a, b) in enumerate(groups):
                    if a <= col and col + n <= b:
                        return wg[gi], (lambda cc, gi=gi, a=a: wg[gi][:, cc, col - a:col - a + n])
                raise ValueError(col)
            cosT = self.sb(st, 'cosT', [128, T], F32)
            sinT = self.sb(st, 'sinT', [128, T], F32)
            with contextlib.ExitStack() as st2:
                posi = self.sb(st2, 'posi', [128, T], I32)
                ang = self.sb(st2, 'ang', [128, T], F32)
                u = self.sb(st2, 'u', [128, T], F32)
                ki = self.sb(st2, 'ki', [128, T], I32)
                invf = self.sb(st2, 'invf', [128, 1], F32)
                self.ld(invf, invf[:], self.c_invf)
                self.ld(posi, posi[:], self.pos[0, :].partition_broadcast(128))
                self.cp(ang, ang[:], posi, posi[:])
                self.ts(ang, ang[:], ang, ang[:], invf[:, 0:1], None, ALU.mult, extra_reads=[invf])
                for tab, off in ((sinT, 0.0), (cosT, 0.25)):
                    self.ts(u, u[:], ang, ang[:], 1.0 / (2 * math.pi), off, ALU.mult, ALU.add)
                    self.cp(ki, ki[:], u, u[:])
                    self.cp(tab, tab[:], ki, ki[:])
                    self.tt(u, u[:], u, u[:], tab, tab[:], ALU.subtract)
                    self.ts(tab, tab[:], u, u[:], 0.5, None, ALU.is_gt)
                    self.tt(u, u[:], u, u[:], tab, tab[:], ALU.subtract)
                    self.ts(tab, tab[:], u, u[:], -0.5, None, ALU.is_lt)
                    self.tt(u, u[:], u, u[:], tab, tab[:], ALU.add)
                    self.act(tab, tab[:], u, u[:], AF.Sin, scale=2 * math.pi)
                S.barrier()
            bf = self.sb(st, 'bfg', [8, 1], F32)
            self.ld(bf, bf[:], self.b_forget[l])
            self.ts(bf, bf[:], bf, bf[:], -1.0, None, ALU.mult)
            xTt = self.ring(st, 'pxT', 2, [128, 8, TS], BF16)
            pss = self.ring(st, 'pps', 7, [128, 512], F32, psum=True)
            stg = self.ring(st, 'pstg', 8, [128, TS], BF16)
            tmp = self.ring(st, 'ptmp', 4, [128, TS], F32)
            sva = self.ring(st, 'psva', 3, [128, 512], BF16)
            svb = self.ring(st, 'psvb', 2, [128, 8, 65], BF16)
            svc = self.ring(st, 'psvc', 2, [128, 12, 65], BF16)
            for b in svb.bufs + svc.bufs:
                self.memset(b, b[:], 1.0)

            def load_x(t):
                xt = xTt.next()
                self.ld(xt, xt[:], self.xT.ap[:, t * TS:(t + 1) * TS].rearrange("(c p) t -> p c t", p=128),
                        src_b=self.xT)
                return xt

            def proj_fm(xt, col, m=128):
                p = pss.next()
                wb, wf = wslice(col, m)
                for cc in range(8):
                    self.mm(p, p[0:m, :], wb, wf(cc), xt, xt[:, cc, :], cc == 0, cc == 7)
                return p
            nxt = load_x(0)
            for t in range(NT):
                xt = nxt
                if t + 1 < NT:
                    nxt = load_x(t + 1)
                tc_ = slice(t * TS, (t + 1) * TS)
                for (base, dst, npair) in ((0, self.qaT, 2), (512, self.kaT, 2), (3080, self.qcT, 3),
                                           (3848, self.kcT, 3)):
                    for m in range(npair):
                        pa = proj_fm(xt, base + 256 * m)
                        pb = proj_fm(xt, base + 256 * m + 128)
                        t1, t2, t3, t4 = tmp.next(), tmp.next(), tmp.next(), tmp.next()
                        self.tt(t1, t1[:], pa, pa[:], cosT, cosT[:, tc_], ALU.mult)
                        self.tt(t2, t2[:], pb, pb[:], sinT, sinT[:, tc_], ALU.mult)
                        self.tt(t3, t3[:], pb, pb[:], cosT, cosT[:, tc_], ALU.mult)
                        self.tt(t4, t4[:], pa, pa[:], sinT, sinT[:, tc_], ALU.mult)
                        o1, o2 = stg.next(), stg.next()
                        self.tt(o1, o1[:], t1, t1[:], t2, t2[:], ALU.subtract)
                        self.tt(o2, o2[:], t3, t3[:], t4, t4[:], ALU.add)
                        self.stor(dst, dst.ap[(2 * m) * 128:(2 * m + 1) * 128, tc_], o1, o1[:])
                        self.stor(dst, dst.ap[(2 * m + 1) * 128:(2 * m + 2) * 128, tc_], o2, o2[:])
                for (base, dst) in ((1536, self.qbT), (2048, self.kbT)):
                    for m in range(4):
                        p = proj_fm(xt, base + 128 * m)
                        o = stg.next()
                        self.act(o, o[:], p, p[:], AF.Copy)
                        self.stor(dst, dst.ap[m * 128:(m + 1) * 128, tc_], o, o[:])
                p = proj_fm(xt, 3072, 8)
                self.act(fbuf, fbuf[:, tc_], p, p[0:8, :], AF.Exp, extra_reads=[bf], scale=-1.0, bias=bf[:, 0:1])
                for sub in range(4):
                    rows = slice(t * TS + sub * 128, t * TS + (sub + 1) * 128)
                    xs = slice(sub * 128, (sub + 1) * 128)

                    def proj_tm(col, n):
                        p = pss.next()
                        wb, wf = wslice(col, n)
                        for cc in range(8):
                            self.mm(p, p[:, 0:n], xt, xt[:, cc, xs], wb, wf(cc), cc == 0, cc == 7)
                        return p
                    p = proj_tm(1024, 512)
                    o = sva.next()
                    self.act(o, o[:], p, p[:], AF.Copy)
                    self.stor(self.va, self.va.ap[rows, :], o, o[:])
                    p = proj_tm(2560, 512)
                    o = svb.next()
                    self.act(o, o[:, :, 0:64], p, p[:].rearrange("p (h e) -> p h e", h=8), AF.Copy)
                    self.stor(self.vb, self.vb.ap[rows, :], o, o[:].rearrange("p h e -> p (h e)"))
                    p = proj_tm(4616, 512)
                    p2 = proj_tm(4616 + 512, 256)
                    o = svc.next()
                    self.act(o, o[:, 0:8, 0:64], p, p[:].rearrange("p (h e) -> p h e", h=8), AF.Copy)
                    self.act(o, o[:, 8:12, 0:64], p2, p2[:, 0:256].rearrange("p (h e) -> p h e", h=4), AF.Copy)
                    self.stor(self.vc, self.vc.ap[rows, :], o, o[:].rearrange("p h e -> p (h e)"))
            S.barrier()
        with st0 as st:
            lg = fbuf
            self.act(lg, lg[:], fbuf, fbuf[:], AF.Ln, bias=1.0)
            onesf = self.sb(st, 'f1', [8, T], F32)
            self.memset(onesf, onesf[:], 1.0)
            cs = self.sb(st, 'fcs', [8, T], F32)
            self.S.op('dve', lambda e: e.tensor_tensor_scan(out=cs[:], data0=onesf[:], data1=lg[:], initial=0.0,
                                                            op0=ALU.mult, op1=ALU.add),
                      reads=[onesf, lg], writes=[cs])
            self.ts(cs, cs[:], cs, cs[:], 8.0, None, ALU.mult)
            parts = []
            rem = cs
            for i in range(3):
                pb_ = self.sb(st, 'fp%d' % i, [8, T], BF16)
                self.cp(pb_, pb_[:], rem, rem[:])
                parts.append(pb_)
                if i < 2:
                    nr = self.sb(st, 'fr%d' % i, [8, T], F32)
                    self.tt(nr, nr[:], rem, rem[:], pb_, pb_[:], ALU.subtract)
                    rem = nr
            onesb = self.sb(st, 'f1b', [8, T], BF16)
            self.memset(onesb, onesb[:], 1.0)
            for i in range(3):
                ng = self.sb(st, 'fn%d' % i, [8, T], BF16)
                self.ts(ng, ng[:], parts[i], parts[i][:], -1.0, None, ALU.mult)
                self.stor(self.qbaug, self.qbaug.ap[:, i, :], ng, ng[:])
                self.stor(self.qbaug, self.qbaug.ap[:, 3 + i, :], onesb, onesb[:])
                self.stor(self.kbaug, self.kbaug.ap[:, i, :], onesb, onesb[:])
                self.stor(self.kbaug, self.kbaug.ap[:, 3 + i, :], parts[i], parts[i][:])
            S.barrier()

    def load_rope_head(self, dst, rows0, src, j):
        m, jj = j // 4, j % 4
        self.ld(dst, dst[rows0:rows0 + 32, :], src.ap[(2 * m) * 128 + jj * 32:(2 * m) * 128 + jj * 32 + 32, :],
                src_b=src)
        self.ld(dst, dst[rows0 + 32:rows0 + 64, :],
                src.ap[(2 * m + 1) * 128 + jj * 32:(2 * m + 1) * 128 + jj * 32 + 32, :], src_b=src)

    def phaseHA(self, l):
        self.issue_bg('HA%d' % l)
        lam_init = 0.8 - 0.6 * math.exp(-0.3 * l)
        with contextlib.ExitStack() as st:
            c = self.consts(st, need_masks=True)
            dl = self.sb(st, 'dl', [128, 256], F32)
            self.ld(dl, dl[:], self.diff_lambda[l, 0, :].partition_broadcast(128))
            lt = self.sb(st, 'lt', [128, 8], F32)
            pr = self.sb(st, 'lpr', [128, 128], F32)
            self.tt(pr, pr[:, 0:64], dl, dl[:, 0:64], dl, dl[:, 64:128], ALU.mult)
            self.tt(pr, pr[:, 64:128], dl, dl[:, 128:192], dl, dl[:, 192:256], ALU.mult)
            self.S.op('dve', lambda e: e.tensor_reduce(out=lt[:, 0:2], in_=pr[:].rearrange("p (a b) -> p a b", a=2),
                                                      axis=AX.X, op=ALU.add), reads=[pr], writes=[lt])
            self.act(lt, lt[:, 2:4], lt, lt[:, 0:2], AF.Exp)
            self.tt(lt, lt[:, 4:5], lt, lt[:, 2:3], lt, lt[:, 3:4], ALU.subtract)
            self.ts(lt, lt[:, 5:6], lt, lt[:, 4:5], lam_init, -1.0, ALU.add, ALU.mult)
            sub = self.sb(st, 'subln', [128, 1], F32)
            self.ld(sub, sub[:], self.diff_subln[l])
            self.ts(sub, sub[:], sub, sub[:], 1.0 - lam_init, None, ALU.mult)
            onesm = self.sb(st, 'onesm', [128, 128], BF16)
            self.memset(onesm, onesm[:], 1.0 / 128.0)
            V = self.sb(st, 'haV', [128, 32, 512], BF16)
            self.ld(V, V[:], self.va.ap.rearrange("(n p) e -> p n e", p=128), src_b=self.va)
            qTs = self.ring(st, 'haq', 2, [128, T], BF16)
            kTs = self.ring(st, 'hak', 2, [128, T], BF16)
            for b in qTs.bufs + kTs.bufs:
                b.disjoint = True
            psS = self.ring(st, 'haS', 2, [128, 1024], F32, psum=True)
            acc = [self.ps(st, 'haacc%d' % i) for i in range(4)]
            Ps = self.ring(st, 'haP', 3, [128, 1024], BF16)
            fins = Ring([[self.sb(st, 'hafin%d_%d' % (a_, i), [128, 512], F32) for i in range(4)] for a_ in range(2)])
            sqbs = self.ring(st, 'sqb', 2, [128, 512], BF16)
            deferred = []
            ostg = self.ring(st, 'haost', 2, [128, 512], BF16)

            def load_head(h):
                q, k = qTs.next(), kTs.next()
                for rr in range(2):
                    self.load_rope_head(q, rr * 64, self.qaT, 2 * h + rr)
                    self.load_rope_head(k, rr * 64, self.kaT, 2 * h + rr)
                return q, k
            nxt = load_head(0)
            for h in range(4):
                q, k = nxt
                if h + 1 < 4:
                    nxt = load_head(h + 1)
                for j in range(NT):
                    nk = 4 * j + 4
                    qs = slice(j * TS, (j + 1) * TS)
                    state = {}

                    def qk(i, q=q, k=k, j=j, qs=qs, state=state):
                        if i == 3:
                            while deferred:
                                deferred.pop(0)()
                        p = psS.next()
                        diag = i >= 4 * j
                        for mp in range(2):
                            r = slice(mp * 64, mp * 64 + 64)
                            po = p[:, mp * 512:(mp + 1) * 512]
                            self.mm(p, po, k, k[r, i * 128:(i + 1) * 128], q, q[r, qs], True, not diag)
                            if diag:
                                a = i - 4 * j
                                self.mm(p, po, c['ident_b'], c['ident_b'][:], c['masks'],
                                        c['masks'][:, a * 512:(a + 1) * 512], False, True)
                        P = Ps.next()
                        self.act(P, P[:], p, p[:], AF.Exp, scale=0.125)
                        state[i] = P

                    def pv(i, h=h, state=state, nk=nk):
                        P = state.pop(i)
                        first, last = (i == 0), (i == nk - 1)
                        for mp in range(2):
                            Pm = P[:, mp * 512:(mp + 1) * 512]
                            self.mm(acc[2 * mp], acc[2 * mp][:], V, V[:, i, h * 128:(h + 1) * 128], P, Pm, first, last)
                            self.mm(acc[2 * mp + 1], acc[2 * mp + 1][:], c['ones_b'], c['ones_b'][:], P, Pm, first, last)
                    pipeline(nk, qk, pv, depth=1)
                    f0, f1, f2, f3 = fins.next()
                    sqb = sqbs.next()
                    self.act(f0, f0[:], acc[1], acc[1][:], AF.Ln)
                    self.act(f2, f2[:], acc[3], acc[3][:], AF.Ln)
                    self.cp(f1, f1[:], acc[0], acc[0][:])
                    self.cp(f3, f3[:], acc[2], acc[2][:])
                    self.act(f0, f0[:], f0, f0[:], AF.Exp, scale=-1.0)
                    self.act(f2, f2[:], f2, f2[:], AF.Exp, scale=-1.0)
                    self.tt(f1, f1[:], f1, f1[:], f0, f0[:], ALU.mult)
                    self.tt(f3, f3[:], f3, f3[:], f2, f2[:], ALU.mult)
                    self.stt(f1, f1[:], f3, f3[:], lt[:, 5:6], f1, f1[:], ALU.mult, ALU.add, extra_reads=[lt])
                    self.tt(sqb, sqb[:], f1, f1[:], f1, f1[:], ALU.mult)

                    def finb(f1=f1, f2=f2, sqb=sqb, h=h, qs=qs):
                        psMb = psS.next()
                        psM = Buf(psMb.ap[:, 0:512])
                        psM.w, psM.r = psMb.w, psMb.r
                        self.mm(psM, psM[:], onesm, onesm[:], sqb, sqb[:], True, True)
                        self.act(f2, f2[:], psM, psM[:], AF.Ln, bias=EPS)
                        psMb.w, psMb.r = psM.w, psM.r
                        self.act(f2, f2[:], f2, f2[:], AF.Exp, scale=-0.5)
                        self.tt(f1, f1[:], f1, f1[:], f2, f2[:], ALU.mult)
                        o = ostg.next()
                        self.ts(o, o[:], f1, f1[:], sub[:, 0:1], None, ALU.mult, extra_reads=[sub])
                        self.stor(self.oaT, self.oaT.ap[h * 128:(h + 1) * 128, qs], o, o[:])
                    deferred.append(finb)
            while deferred:
                deferred.pop(0)()
            self.S.barrier()

    def fin_norm_a(self, accb, fo, fr):
        self.act(fo, fo[0:65, :], accb, accb[0:65, :], AF.Copy)
        fl, fb = fr['l'], fr['b']
        self.act(fl, fl[64:65, :], fo, fo[64:65, :], AF.Ln)
        self.act(fl, fl[64:65, :], fl, fl[64:65, :], AF.Exp, scale=-1.0)
        self.cp(fb, fb[64:65, :], fl, fl[64:65, :])
        self.tt(fl, fl[64:65, :], fl, fl[64:65, :], fb, fb[64:65, :], ALU.subtract)
        self.cp(fb, fb[96:97, :], fl, fl[64:65, :])

    def fin_norm_b(self, c, fo, fr, psB, ostg, dst, dst_ap):
        fb = fr['b']
        self.mm(psB, psB[0:64, :], c['ones_b'], c['ones_b'][64:97, 0:64], fb, fb[64:97, :], True, True)
        o = ostg.next()
        self.tt(o, o[0:64, :], fo, fo[0:64, :], psB, psB[0:64, :], ALU.mult)
        self.stor(dst, dst_ap, o, o[0:64, :])

    def mk_fr(self, st, name):
        fl = self.sb(st, name + 'l', [128, 512], F32)
        fb = self.sb(st, name + 'b', [128, 512], BF16)
        self.memset(fb, fb[:], 0.0)
        return {'l': fl, 'b': fb}

    def phaseHB(self, l):
        self.issue_bg('HB%d' % l)
        with contextlib.ExitStack() as st:
            c = self.consts(st, need_masks=True)
            V = self.sb(st, 'hbV', [128, 32, 520], BF16)
            self.ld(V, V[:], self.vb.ap.rearrange("(n p) e -> p n e", p=128), src_b=self.vb)
            qTs = self.ring(st, 'hbq', 2, [70, T], BF16)
            kTs = self.ring(st, 'hbk', 2, [70, T], BF16)
            for b in qTs.bufs + kTs.bufs:
                b.disjoint = True
            psS = self.ring(st, 'hbS', 3, [128, 1024], F32, psum=True)
            accs = self.ring(st, 'hbacc', 2, [128, 512], F32, psum=True)
            Ps = self.ring(st, 'hbP', 4, [128, 1024], BF16)
            fos = self.ring(st, 'hbfo', 2, [128, 512], F32)
            frs = Ring([self.mk_fr(st, 'hbfr%d' % i) for i in range(2)])
            deferred = []
            ostg = self.ring(st, 'hbost', 2, [128, 512], BF16)

            def load_head(h):
                q, k = qTs.next(), kTs.next()
                self.ld(q, q[0:64, :], self.qbT.ap[h * 64:(h + 1) * 64, :], src_b=self.qbT)
                self.ld(k, k[0:64, :], self.kbT.ap[h * 64:(h + 1) * 64, :], src_b=self.kbT)
                self.ld(q, q[64:70, :], self.qbaug.ap[h], src_b=self.qbaug)
                self.ld(k, k[64:70, :], self.kbaug.ap[h], src_b=self.kbaug)
                return q, k
            nxt = load_head(0)
            for h in range(8):
                q, k = nxt
                if h + 1 < 8:
                    nxt = load_head(h + 1)
                for j in range(NT):
                    nk = 4 * j + 4
                    qs = slice(j * TS, (j + 1) * TS)
                    state = {}
                    acc = accs.next()

                    def qk(u, q=q, k=k, j=j, qs=qs, state=state, nk=nk):
                        if u == min(3, nk // 2 - 1):
                            while deferred:
                                deferred.pop(0)()
                        p = psS.next()
                        for w in range(2):
                            i = 2 * u + w
                            po = p[:, w * 512:(w + 1) * 512]
                            diag = i >= 4 * j
                            self.mm(p, po, k, k[0:70, i * 128:(i + 1) * 128], q, q[0:70, qs], True, not diag)
                            if diag:
                                a = i - 4 * j
                                self.mm(p, po, c['ident_b'], c['ident_b'][:], c['masks'],
                                        c['masks'][:, a * 512:(a + 1) * 512], False, True)
                        P = Ps.next()
                        self.act(P, P[:], p, p[:], AF.Exp, scale=0.125)
                        state[u] = P

                    def pv(u, h=h, nk=nk, state=state, acc=acc):
                        P = state.pop(u)
                        for w in range(2):
                            i = 2 * u + w
                            self.mm(acc, acc[0:65, :], V, V[:, i, h * 65:(h + 1) * 65], P, P[:, w * 512:(w + 1) * 512],
                                    i == 0, i == nk - 1)
                    pipeline(nk // 2, qk, pv, depth=2)
                    fo, fr = fos.next(), frs.next()
                    self.fin_norm_a(acc, fo, fr)

                    def finb(fo=fo, fr=fr, h=h, qs=qs):
                        psBb = psS.next()
                        psB = Buf(psBb.ap[:, 0:512])
                        psB.w, psB.r = psBb.w, psBb.r
                        self.fin_norm_b(c, fo, fr, psB, ostg, self.obT, self.obT.ap[h * 64:(h + 1) * 64, qs])
                        psBb.w, psBb.r = psB.w, psB.r
                    deferred.append(finb)
            while deferred:
                deferred.pop(0)()
            self.S.barrier()

    def phaseHC(self, l):
        dil = (1, 4, 16)
        with contextlib.ExitStack() as st:
            c = self.consts(st, need_masks=True)
            Vg = []
            for g in range(3):
                d = dil[g]
                v = self.sb(st, 'hcV%d' % g, [128, 32, 260], BF16, disjoint=True)
                src = self.vc.ap[:, g * 260:(g + 1) * 260].rearrange("(b kj cl) e -> kj cl b e", kj=128, cl=d)
                for cl in range(d):
                    nb = 32 // d
                    self.ld(v, v[:, cl * nb:(cl + 1) * nb, :], src[:, cl], src_b=self.vc)
                Vg.append(v)
            qTs = self.ring(st, 'hcq', 2, [64, T], BF16)
            kTs = self.ring(st, 'hck', 2, [64, T], BF16)
            for b in qTs.bufs + kTs.bufs:
                b.disjoint = True
            psC = self.ring(st, 'hcSc', 2, [128, 512], F32, psum=True)
            psP = self.ring(st, 'hcSp', 2, [128, 512], F32, psum=True)
            psO = self.ring(st, 'hcO', 2, [128, 512], F32, psum=True)
            psB = self.ps(st, 'hcB')
            Pc = self.ring(st, 'hcPc', 2, [128, 512], BF16)
            Pp = self.ring(st, 'hcPp', 2, [128, 512], BF16)
            accs = self.ring(st, 'hcacc', 2, [65, T], F32)
            fr = self.mk_fr(st, 'hcfr')
            ostg = self.ring(st, 'hcost', 2, [128, 512], BF16)
            heads = [(s, g) for s in range(4) for g in range(3)]

            def load_head(n):
                s, g = heads[n]
                q, k = qTs.next(), kTs.next()
                self.load_rope_head(q, 0, self.qcT, g * 4 + s)
                self.load_rope_head(k, 0, self.kcT, g * 4 + s)
                return q, k
            nxt = load_head(0)
            for n, (s, g) in enumerate(heads):
                q, k = nxt
                if n + 1 < len(heads):
                    nxt = load_head(n + 1)
                if g == 0:
                    acc = accs.next()
                d = dil[g]
                nb = 32 // d
                V = Vg[g]

                def tsl(cl, b, d=d):
                    start = b * 128 * d + cl
                    return slice(start, start + 127 * d + 1, d)
                state = {}

                def qk(u, q=q, k=k, state=state, nb=nb, tsl=tsl):
                    pc, pp = psC.next(), psP.next()
                    for bb in range(4):
                        L = 4 * u + bb
                        cl, b = L // nb, L % nb
                        cs = slice(bb * 128, (bb + 1) * 128)
                        self.mm(pc, pc[:, cs], k, k[0:64, tsl(cl, b)], q, q[0:64, tsl(cl, b)], True, False)
                        self.mm(pc, pc[:, cs], c['ident_b'], c['ident_b'][:], c['masks'], c['masks'][:, 0:128],
                                False, True)
                        if b > 0:
                            self.mm(pp, pp[:, cs], k, k[0:64, tsl(cl, b - 1)], q, q[0:64, tsl(cl, b)], True, False)
                            self.mm(pp, pp[:, cs], c['ident_b'], c['ident_b'][:], c['mprev'], c['mprev'][:],
                                    False, True)
                        else:
                            self.mm(pp, pp[:, cs], c['ident_b'], c['ident_b'][:], c['masks'], c['masks'][:, 0:128],
                                    True, True)
                    a, b_ = Pc.next(), Pp.next()
                    self.act(a, a[:], pc, pc[:], AF.Exp, scale=0.125)
                    self.act(b_, b_[:], pp, pp[:], AF.Exp, scale=0.125)
                    state[u] = (a, b_)

                def pv(u, s=s, g=g, V=V, state=state, nb=nb, d=d, acc=acc, tsl=tsl):
                    a, b_ = state.pop(u)
                    po = psO.next()
                    for bb in range(4):
                        L = 4 * u + bb
                        cl, b = L // nb, L % nb
                        cs = slice(bb * 128, (bb + 1) * 128)
                        self.mm(po, po[0:65, cs], V, V[:, L, s * 65:(s + 1) * 65], a, a[:, cs], True, b == 0)
                        if b > 0:
                            self.mm(po, po[0:65, cs], V, V[:, L - 1, s * 65:(s + 1) * 65], b_, b_[:, cs], False, True)
                    if d == 16:
                        runs = [(0, 256, (4 * u) // nb), (256, 512, (4 * u) // nb + 1)]
                    else:
                        runs = [(0, 512, (4 * u) // nb)]
                    for (c0, c1, cl) in runs:
                        b0 = (4 * u + c0 // 128) % nb
                        start = b0 * 128 * d + cl
                        cnt = c1 - c0
                        sl = slice(start, start + (cnt - 1) * d + 1, d)
                        if g == 0:
                            self.cp(acc, acc[0:65, sl], po, po[0:65, c0:c1])
                        else:
                            self.tt(acc, acc[0:65, sl], acc, acc[0:65, sl], po, po[0:65, c0:c1], ALU.add)
                pipeline(8, qk, pv)
                if g == 2:
                    for j in range(NT):
                        qs = slice(j * TS, (j + 1) * TS)
                        fl, fb = fr['l'], fr['b']
                        self.act(fl, fl[64:65, :], acc, acc[64:65, qs], AF.Ln)
                        self.act(fl, fl[64:65, :], fl, fl[64:65, :], AF.Exp, scale=-1.0)
                        self.cp(fb, fb[64:65, :], fl, fl[64:65, :])
                        self.tt(fl, fl[64:65, :], fl, fl[64:65, :], fb, fb[64:65, :], ALU.subtract)
                        self.cp(fb, fb[96:97, :], fl, fl[64:65, :])
                        self.mm(psB, psB[0:64, :], c['ones_b'], c['ones_b'][64:97, 0:64], fb, fb[64:97, :], True, True)
                        o = ostg.next()
                        self.tt(o, o[0:64, :], acc, acc[0:64, qs], psB, psB[0:64, :], ALU.mult)
                        self.stor(self.ocT, self.ocT.ap[s * 64:(s + 1) * 64, qs], o, o[0:64, :])
            self.S.barrier()

    def precast_w(self, key, src):
        R, C = src.shape
        sp = 1
        while C // sp > 2048:
            sp *= 2
        dst = Buf(self.nc.dram_tensor("wb_%s_%d" % key, [R, C], BF16, kind="Internal").ap(), disjoint=True)
        sv = src.rearrange("r (s c) -> (r s) c", s=sp)
        dv = dst.ap.rearrange("r (s c) -> (r s) c", s=sp)
        R2 = R * sp
        for r0 in range(0, R2, 1024):
            r1 = min(R2, r0 + 1024)
            self.S.dma('bg', dv[r0:r1].rearrange("(p k) c -> p k c", p=128),
                       sv[r0:r1].rearrange("(p k) c -> p k c", p=128), writes=[dst])
        self.wbf[key] = dst

    def issue_bg(self, phase):
        srcs = {'w_gate': self.w_gate, 'w_ba': self.w_ba, 'w_bb': self.w_bb, 'w_bc': self.w_bc,
                'w_out': self.w_out, 'w_xq': self.w_xq, 'w_xo': self.w_xo, 'w_xk': self.w_xk,
                'w_xv': self.w_xv, 'w_in': self.w_in}
        c1a = ['w_gate', 'w_ba', 'w_bb', 'w_bc', 'w_out']
        c1b = ['w_xq', 'w_xo', 'w_xk', 'w_xv']
        sched = {'HA0': [(n, 0) for n in c1a], 'HB0': [(n, 0) for n in c1b] + [('experts', 0)],
                 'C1a0': [('w_in', 1)], 'C1b0': [(n, 1) for n in c1a],
                 'C20': [(n, 1) for n in c1b] + [('experts', 1)]}
        if True:
            return
        for key in sched.get(phase, []):
            nm, l = key
            if l >= self.layers:
                continue
            if nm == 'experts':
                self.precast(l)
                self.experts_cast.add(l)
            else:
                self.precast_w(key, srcs[nm][l])

    def load_w(self, st, name, src, kc, n, key=None, defer=False):
        w = self.sb(st, name, [128, kc, n], BF16, disjoint=True)
        if defer:
            self._deferred_loads.append(lambda: self._issue_w(w, src, kc, n, key))
            return w
        self._issue_w(w, src, kc, n, key)
        return w

    def _issue_w(self, w, src, kc, n, key):
        if key is not None and key in self.wbf:
            b = self.wbf[key]
            for k0 in range(0, kc, 2):
                k1 = min(kc, k0 + 2)
                self.ld(w, w[:, k0:k1, :], b.ap[k0 * 128:k1 * 128, :].rearrange("(c p) n -> p c n", p=128), src_b=b)
            return w
        step = max(1, 2048 // n)
        for k0 in range(0, kc, step):
            k1 = min(kc, k0 + step)
            if n <= 2048:
                self.ld(w, w[:, k0:k1, :], src[k0 * 128:k1 * 128, :].rearrange("(c p) n -> p c n", p=128), q='pool')
            else:
                for n0 in range(0, n, 1536):
                    n1 = min(n, n0 + 1536)
                    self.ld(w, w[:, k0, n0:n1], src[k0 * 128:(k0 + 1) * 128, n0:n1], q='pool')
        return w

    def ln_tiles(self, st, l, idx):
        g = self.sb(st, 'lng', [128, D], F32)
        b = self.sb(st, 'lnb', [128, D], F32)
        self.ld(g, g[:], self.ln_g[l, idx, :].partition_broadcast(128))
        self.ld(b, b[:], self.ln_b[l, idx, :].partition_broadcast(128))
        sm = {'stats': self.sb(st, 'lnst', [128, 12], F32), 'mv': self.sb(st, 'lnmv', [128, 2], F32),
              'sc': self.sb(st, 'lnsc', [128, 4], F32)}
        return g, b, sm

    def phaseC1a(self, l):
        self.issue_bg('C1a%d' % l)
        xsrc = self.x if l == 0 else self.xres.ap
        xsrc_b = None if l == 0 else self.xres
        with contextlib.ExitStack() as st:
            c = self.consts(st)
            Wg = self.load_w(st, 'wgate', self.w_gate[l], 8, 3072, key=('w_gate', l))
            Wa = self.load_w(st, 'wba', self.w_ba[l], 4, 1024, key=('w_ba', l))
            Wb = self.load_w(st, 'wbb', self.w_bb[l], 4, 1024, key=('w_bb', l))
            Wc = self.load_w(st, 'wbc', self.w_bc[l], 2, 1024, key=('w_bc', l))
            Wo = self.load_w(st, 'wout', self.w_out[l], 8, 1024, key=('w_out', l))
            bg = self.sb(st, 'bgate', [128, 24], F32)
            self.ld(bg, bg[:], self.b_gate[l])
            lg_, lb_, sm = self.ln_tiles(st, l, 0)
            xTt = self.ring(st, 'c1xT', 2, [128, 8, TS], BF16)
            oat = self.ring(st, 'c1oa', 2, [128, 4, TS], BF16)
            obt = self.ring(st, 'c1ob', 2, [128, 4, TS], BF16)
            oct_ = self.ring(st, 'c1oc', 2, [128, 2, TS], BF16)
            xrs = self.ring(st, 'c1xr', 1, [128, 4, D], F32)
            mT = self.sb(st, 'c1mT', [128, 8, TS], BF16)
            sg = self.ring(st, 'c1sg', 3, [128, TS], F32)
            mm_ = self.ring(st, 'c1mm', 3, [128, TS], F32)
            x1Tt = self.ring(st, 'c1x1T', 2, [128, 8, TS], BF16)
            pss = self.ring(st, 'c1ps', 8, [128, 512], F32, psum=True)

            def loads(t):
                tc_ = slice(t * TS, (t + 1) * TS)
                a, b, cc_, xt = oat.next(), obt.next(), oct_.next(), xTt.next()
                self.ld(xt, xt[:], self.xT.ap[:, tc_].rearrange("(c p) t -> p c t", p=128), src_b=self.xT)
                self.ld(a, a[:], self.oaT.ap[:, tc_].rearrange("(c p) t -> p c t", p=128), src_b=self.oaT)
                self.ld(b, b[:], self.obT.ap[:, tc_].rearrange("(c p) t -> p c t", p=128), src_b=self.obT)
                self.ld(cc_, cc_[:], self.ocT.ap[:, tc_].rearrange("(c p) t -> p c t", p=128), src_b=self.ocT)
                return a, b, cc_, xt
            rr4 = self.ring(st, 'c1r4', 4, [128, D], F32)
            tiles = {}

            prea = {}

            def stageA(t):
                a, b, cc_, xt = prea.pop(t)
                for ch in range(8):
                    cs = slice(ch * 128, (ch + 1) * 128)
                    ms = []
                    for bi, (W, o, kc) in enumerate(((Wa, a, 4), (Wb, b, 4), (Wc, cc_, 2))):
                        pg = pss.next()
                        for k in range(8):
                            self.mm(pg, pg[:], Wg, Wg[:, k, bi * 1024 + ch * 128:bi * 1024 + (ch + 1) * 128],
                                    xt, xt[:, k, :], k == 0, k == 7)
                        pb = pss.next()
                        for k in range(kc):
                            self.mm(pb, pb[:], W, W[:, k, cs], o, o[:, k, :], k == 0, k == kc - 1)
                        s_ = sg.next()
                        self.act(s_, s_[:], pg, pg[:], AF.Sigmoid, extra_reads=[bg],
                                 bias=bg[:, bi * 8 + ch:bi * 8 + ch + 1])
                        m = mm_.next()
                        self.tt(m, m[:], s_, s_[:], pb, pb[:], ALU.mult)
                        ms.append(m)
                    self.tt(ms[0], ms[0][:], ms[0], ms[0][:], ms[1], ms[1][:], ALU.add)
                    self.tt(mT, mT[:, ch, :], ms[0], ms[0][:], ms[2], ms[2][:], ALU.add)

            def stageB(t):
                tc_ = slice(t * TS, (t + 1) * TS)
                xr = xrs.next()
                self.ld(xr, xr[:], xsrc[tc_, :].rearrange("(s p) d -> p s d", p=128), src_b=xsrc_b)
                rs = []
                for sub in range(4):
                    r = rr4.next()
                    rs.append(r)
                    for half in range(2):
                        p = pss.next()
                        for k in range(8):
                            self.mm(p, p[:], mT, mT[:, k, sub * 128:(sub + 1) * 128], Wo,
                                    Wo[:, k, half * 512:(half + 1) * 512], k == 0, k == 7)
                        self.stt(r, r[:, half * 512:(half + 1) * 512], xr, xr[:, sub, half * 512:(half + 1) * 512],
                                 ALPHA, p, p[:], ALU.mult, ALU.add)
                for sub in range(4):
                    r = rs[sub]
                    self.layer_norm(sm, r, r[:], lg_, lb_, r, r[:])
                    rows = slice(t * TS + sub * 128, t * TS + (sub + 1) * 128)
                    self.stor(self.x1, self.x1.ap[rows, :], r, r[:])
                tiles[t] = rs

            def stageT(t):
                tc_ = slice(t * TS, (t + 1) * TS)
                rs = tiles.pop(t)
                x1T = x1Tt.next()
                for sub in range(4):
                    self.transpose_to_xT(c, rs[sub], rs[sub][:], pss, x1T, sub)
                self.stor(self.x1T, self.x1T.ap[:, tc_].rearrange("(c p) t -> p c t", p=128), x1T, x1T[:])
            prea[0] = loads(0)
            stageA(0)
            for t in range(NT):
                if t + 1 < NT:
                    prea[t + 1] = loads(t + 1)
                stageB(t)
                if t + 1 < NT:
                    stageA(t + 1)
                stageT(t)
            self.S.barrier()

    def phaseC1b(self, l):
        self.issue_bg('C1b%d' % l)
        BIG = 1.0e4
        with contextlib.ExitStack() as st:
            c = self.consts(st)
            Wq = self.load_w(st, 'wxq', self.w_xq[l], 8, 1024, key=('w_xq', l), defer=True)
            Wo = self.load_w(st, 'wxo', self.w_xo[l], 8, 1024, key=('w_xo', l), defer=True)
            KmT = self.sb(st, 'KmT', [128, 8, 256], BF16)
            Vm = self.sb(st, 'Vm', [128, 2, 1024], BF16)
            pss = self.ring(st, 'cbps', 8, [128, 512], F32, psum=True)
            with contextlib.ExitStack() as st2:
                Wk = self.load_w(st2, 'wxk', self.w_xk[l], 8, 1024, key=('w_xk', l))
                Wv = self.load_w(st2, 'wxv', self.w_xv[l], 8, 1024, key=('w_xv', l))
                memf = self.sb(st2, 'memf', [128, 2, D], F32)
                self.ld(memf, memf[:], self.mem.rearrange("(s p) d -> p s d", p=128))
                while self._deferred_loads:
                    self._deferred_loads.pop(0)()
                memT = self.sb(st2, 'memT', [128, 8, 256], BF16)
                for ks in range(2):
                    for half in range(2):
                        p = pss.next()
                        for k in range(4):
                            cc = half * 4 + k
                            self.S.op('pe', lambda e, p=p, k=k, cc=cc, ks=ks: e.transpose(
                                p[:, k * 128:(k + 1) * 128], memf[:, ks, cc * 128:(cc + 1) * 128], c['ident_f'][:]),
                                reads=[memf, c['ident_f']], writes=[p])
                        self.act(memT, memT[:, half * 4:half * 4 + 4, ks * 128:(ks + 1) * 128], p,
                                 p[:].rearrange("p (k t) -> p k t", k=4), AF.Copy)
                for cc in range(8):
                    p = pss.next()
                    for k in range(8):
                        self.mm(p, p[:, 0:256], Wk, Wk[:, k, cc * 128:(cc + 1) * 128], memT, memT[:, k, :], k == 0, k == 7)
                    self.act(KmT, KmT[:, cc, :], p, p[:, 0:256], AF.Copy)
                for ks in range(2):
                    for half in range(2):
                        p = pss.next()
                        for k in range(8):
                            self.mm(p, p[:], memT, memT[:, k, ks * 128:(ks + 1) * 128], Wv,
                                    Wv[:, k, half * 512:(half + 1) * 512], k == 0, k == 7)
                        self.act(Vm, Vm[:, ks, half * 512:(half + 1) * 512], p, p[:], AF.Copy)
                self.S.barrier()
            lg_, lb_, sm = self.ln_tiles(st, l, 1)
            wr = self.sb(st, 'wr', [128, 8, 20], F32)
            self.ld(wr, wr[:], self.w_r[l].rearrange("(c p) n -> p c n", p=128))
            br = self.sb(st, 'br', [128, 20], F32)
            self.ld(br, br[:], self.b_r[l, 0, :].partition_broadcast(128))
            x1Tt = self.ring(st, 'cbx1T', 2, [128, 8, TS], BF16)
            x1s = self.ring(st, 'cbx1', 2, [128, 4, D], F32)
            qxT = self.sb(st, 'cbqx', [128, 8, TS], BF16)
            PT = self.ring(st, 'cbPT', 4, [128, TS], BF16)
            rden = self.ring(st, 'cbrd', 2, [128, TS], F32)
            oxT = self.sb(st, 'cbox', [128, 8, TS], BF16)
            rr = self.ring(st, 'cbr', 2, [128, D], F32)
            x2o = self.ring(st, 'cbx2', 2, [128, D], F32)
            x2Tt = self.ring(st, 'cbx2T', 2, [128, 8, TS], BF16)
            x2Tf = self.ring(st, 'cbx2Tf', 2, [128, 8, 128], F32)
            rt = self.ring(st, 'cbrt', 2, [128, 128], F32)
            gTt = self.ring(st, 'cbgT', 2, [16, TS], F32)

            def loads(t):
                tc_ = slice(t * TS, (t + 1) * TS)
                xt, x1 = x1Tt.next(), x1s.next()
                self.ld(xt, xt[:], self.x1T.ap[:, tc_].rearrange("(c p) t -> p c t", p=128), src_b=self.x1T)
                self.ld(x1, x1[:], self.x1.ap[tc_, :].rearrange("(s p) d -> p s d", p=128), src_b=self.x1)
                return xt, x1
            rr4 = self.ring(st, 'cbr4', 4, [128, D], F32)
            rt4 = self.ring(st, 'cbrt4', 4, [128, 128], F32)
            PT8 = self.ring(st, 'cbPT8', 4, [128, TS], BF16)
            pending = []
            tl = {}

            pre = {}

            def S1(t):
                xt, x1 = pre.pop(t)
                tl[t] = x1
                for cc in range(8):
                    p = pss.next()
                    for k in range(8):
                        self.mm(p, p[:], Wq, Wq[:, k, cc * 128:(cc + 1) * 128], xt, xt[:, k, :], k == 0, k == 7)
                    self.act(qxT, qxT[:, cc, :], p, p[:], AF.Copy)
                hst = {}

                def sc(hh):
                    Ps = []
                    for ks in range(2):
                        p = pss.next()
                        for c2 in range(2):
                            self.mm(p, p[:], KmT, KmT[:, 2 * hh + c2, ks * 128:(ks + 1) * 128], qxT,
                                    qxT[:, 2 * hh + c2, :], c2 == 0, c2 == 1)
                        P = PT8.next()
                        self.act(P, P[:], p, p[:], AF.Exp, scale=1.0 / 16.0)
                        Ps.append(P)
                    hst[hh] = Ps

                def pvx(hh):
                    Ps = hst.pop(hh)
                    pd = pss.next()
                    for ks in range(2):
                        self.mm(pd, pd[:], c['ones_b'], c['ones_b'][:], Ps[ks], Ps[ks][:], ks == 0, ks == 1)
                    rd = rden.next()
                    self.recip(rd, rd[:], pd, pd[:])
                    for c2 in range(2):
                        pn = pss.next()
                        for ks in range(2):
                            self.mm(pn, pn[:], Vm, Vm[:, ks, (2 * hh + c2) * 128:(2 * hh + c2 + 1) * 128], Ps[ks],
                                    Ps[ks][:], ks == 0, ks == 1)
                        self.tt(oxT, oxT[:, 2 * hh + c2, :], pn, pn[:], rd, rd[:], ALU.mult)
                pipeline(4, sc, pvx, depth=1)

            def S2LN(t):
                x1 = tl.pop(t)
                rs = []
                for sub in range(4):
                    r = rr4.next()
                    rs.append(r)
                    for half in range(2):
                        p = pss.next()
                        for k in range(8):
                            self.mm(p, p[:], oxT, oxT[:, k, sub * 128:(sub + 1) * 128], Wo,
                                    Wo[:, k, half * 512:(half + 1) * 512], k == 0, k == 7)
                        self.stt(r, r[:, half * 512:(half + 1) * 512], x1, x1[:, sub, half * 512:(half + 1) * 512],
                                 ALPHA, p, p[:], ALU.mult, ALU.add)
                for sub in range(4):
                    xo = rs[sub]
                    self.layer_norm(sm, xo, xo[:], lg_, lb_, xo, xo[:])
                    rows = slice(t * TS + sub * 128, t * TS + (sub + 1) * 128)
                    self.stor(self.x2, self.x2.ap[rows, :], xo, xo[:])
                return rs

            def TR(t, rs):
                tc_ = slice(t * TS, (t + 1) * TS)
                x2T = x2Tt.next()
                gT = gTt.next()
                ws = []
                for sub in range(4):
                    xo = rs[sub]
                    xf = x2Tf.next()
                    self.transpose_to_xT(c, xo, xo[:], pss, x2T, sub, f32_b=xf)
                    pl = pss.next()
                    for k in range(8):
                        self.mm(pl, pl[:, 0:20], xf, xf[:, k, :], wr, wr[:, k, :], k == 0, k == 7)
                    w = rt4.next()
                    ws.append(w)
                    self.tt(w, w[:, 0:20], pl, pl[:, 0:20], br, br[:], ALU.add)
                self.stor(self.x2T, self.x2T.ap[:, tc_].rearrange("(c p) t -> p c t", p=128), x2T, x2T[:])
                for sub in range(4):
                    w = ws[sub]
                    self.S.op('dve', lambda e, w=w: e.tensor_reduce(out=w[:, 20:21], in_=w[:, 0:4], axis=AX.X, op=ALU.max),
                              reads=[w], writes=[w])
                    self.ts(w, w[:, 24:28], w, w[:, 0:4], w[:, 20:21], None, ALU.is_equal)
                    self.ts(w, w[:, 21:22], w, w[:, 20:21], -1.0, None, ALU.mult)
                    self.act(w, w[:, 118:122], w, w[:, 0:4], AF.Exp, bias=w[:, 21:22], accum_out=w[:, 22:23])
                    self.recip(w, w[:, 23:24], w, w[:, 22:23])
                    self.ts(w, w[:, 28:32], w, w[:, 24:28], -1.0, BIG, ALU.add, ALU.mult)
                    self.tt(w, w[:, 32:48].rearrange("p (g e) -> p g e", g=4), w,
                            w[:, 4:20].rearrange("p (g e) -> p g e", g=4), w,
                            w[:, 28:32].unsqueeze(2).to_broadcast([128, 4, 4]), ALU.add)
                    self.S.op('dve', lambda e, w=w: e.tensor_reduce(out=w[:, 48:49], in_=w[:, 32:48], axis=AX.X, op=ALU.max),
                              reads=[w], writes=[w])
                    self.ts(w, w[:, 50:66], w, w[:, 32:48], w[:, 48:49], None, ALU.is_equal)
                    self.stt(w, w[:, 66:82], w, w[:, 50:66], -BIG, w, w[:, 32:48], ALU.mult, ALU.add)
                    self.S.op('dve', lambda e, w=w: e.tensor_reduce(out=w[:, 49:50], in_=w[:, 66:82], axis=AX.X, op=ALU.max),
                              reads=[w], writes=[w])
                    self.ts(w, w[:, 82:98], w, w[:, 66:82], w[:, 49:50], None, ALU.is_equal)
                    self.tt(w, w[:, 98:99], w, w[:, 49:50], w, w[:, 48:49], ALU.subtract)
                    self.act(w, w[:, 99:100], w, w[:, 98:99], AF.Exp)
                    self.ts(w, w[:, 100:101], w, w[:, 99:100], 1.0, None, ALU.add)
                    self.recip(w, w[:, 100:101], w, w[:, 100:101])
                    self.tt(w, w[:, 101:102], w, w[:, 99:100], w, w[:, 100:101], ALU.mult)
                    self.ts(w, w[:, 100:102], w, w[:, 100:102], w[:, 23:24], None, ALU.mult)
                    self.ts(w, w[:, 102:118], w, w[:, 50:66], w[:, 100:101], None, ALU.mult)
                    self.stt(w, w[:, 102:118], w, w[:, 82:98], w[:, 101:102], w, w[:, 102:118], ALU.mult, ALU.add)


                def fin(ws=ws, gT=gT, tc_=tc_):
                    for sub in range(4):
                        w = ws[sub]
                        pt = pss.next()
                        self.S.op('pe', lambda e, pt=pt, w=w: e.transpose(pt[0:16, 0:128], w[:, 102:118], c['ident_f'][:]),
                                  reads=[w, c['ident_f']], writes=[pt])
                        self.act(gT, gT[0:16, sub * 128:(sub + 1) * 128], pt, pt[0:16, 0:128], AF.Copy)
                    self.stor(self.gateT, self.gateT.ap[:, tc_], gT, gT[:])
                pending.append(fin)
            pre[0] = loads(0)
            S1(0)
            for t in range(NT):
                if t + 1 < NT:
                    pre[t + 1] = loads(t + 1)
                rs = S2LN(t)
                while pending:
                    pending.pop(0)()
                if t + 1 < NT:
                    S1(t + 1)
                TR(t, rs)
            while pending:
                pending.pop(0)()
            self.S.barrier()

    def precast(self, l):
        for e in range(16):
            for src, dst in ((self.w_eg, self.wegb), (self.w_eu, self.weub), (self.w_ed, self.wedb)):
                self.S.dma('bg', dst.ap[l, e].rearrange("a b -> (a b)").rearrange("(p n) -> p n", p=128),
                           src[l, e].rearrange("a b -> (a b)").rearrange("(p n) -> p n", p=128), writes=[dst])

    def phaseC2(self, l):
        self.issue_bg('C2%d' % l)
        if l not in self.experts_cast:
            self.precast(l)
            self.experts_cast.add(l)
        last = (l == DEPTH - 1)
        with contextlib.ExitStack() as st:
            c = self.consts(st)
            sel = self.sb(st, 'sel', [48, 2048], BF16, disjoint=True)
            self.memset(sel, sel[:], 0.0)
            self.ld(sel, sel[0:16, :], self.c_sel, q='pool')
            self.ld(sel, sel[32:48, :], self.c_sel, q='pool')
            g2s = self.ring(st, 'c2g2', 2, [48, TS], BF16)
            for b_ in g2s.bufs:
                self.memset(b_, b_[:], 0.0)
            grem = self.sb(st, 'c2grem', [16, TS], F32)
            lg_, lb_, sm = self.ln_tiles(st, l, 2)
            x2Tt = self.ring(st, 'c2xT', 2, [128, 8, TS], BF16)
            ys = self.ring(st, 'c2y', 3, [128, 4, D], F32)
            gTt = self.ring(st, 'c2gT', 2, [16, TS], F32)
            Wgs = self.ring(st, 'c2wg', 3, [128, 8, 256], BF16)
            Wus = self.ring(st, 'c2wu', 3, [128, 8, 256], BF16)
            Wds = self.ring(st, 'c2wd', 10, [128, 2, D], BF16)
            hs = self.ring(st, 'c2h', 8, [128, 2, TS], BF16)
            sgs = self.ring(st, 'c2sg', 2, [128, TS], F32)
            tms = self.ring(st, 'c2tm', 2, [128, TS], F32)
            x3Tt = self.ring(st, 'c2x3T', 2, [128, 8, TS], BF16)
            psGU = self.ring(st, 'c2gu', 4, [128, 512], F32, psum=True)
            psG = self.ring(st, 'c2G', 2, [128, 512], F32, psum=True)
            psD = self.ring(st, 'c2D', 2, [128, 512], F32, psum=True)

            def loads(t):
                tc_ = slice(t * TS, (t + 1) * TS)
                xt, y, g = x2Tt.next(), ys.next(), gTt.next()
                self.ld(xt, xt[:], self.x2T.ap[:, tc_].rearrange("(c p) t -> p c t", p=128), src_b=self.x2T)
                self.ld(y, y[:], self.x2.ap[tc_, :].rearrange("(s p) d -> p s d", p=128), src_b=self.x2)
                self.ld(g, g[:], self.gateT.ap[:, tc_], src_b=self.gateT)
                return xt, y, g

            def loadw(e):
                wg_, wu_, wd_ = Wgs.next(), Wus.next(), Wds.next()
                self.ld(wg_, wg_[:], self.wegb.ap[l, e].rearrange("(c p) n -> p c n", p=128), src_b=self.wegb)
                self.ld(wu_, wu_[:], self.weub.ap[l, e].rearrange("(c p) n -> p c n", p=128), src_b=self.weub)
                self.ld(wd_, wd_[:], self.wedb.ap[l, e].rearrange("(c p) n -> p c n", p=128), src_b=self.wedb)
                return wg_, wu_, wd_
            pending = []
            nxt = loads(0)
            wq = [loadw(0), loadw(1)]
            for t in range(NT):
                xt, y, gT = nxt
                if t + 1 < NT:
                    nxt = loads(t + 1)
                tc_ = slice(t * TS, (t + 1) * TS)
                for sub in range(4):
                    self.ts(y, y[:, sub, :], y, y[:, sub, :], ALPHA, None, ALU.mult)
                state = {}
                g2 = g2s.next()
                self.cp(g2, g2[0:16, :], gT, gT[:])
                self.tt(grem, grem[:], gT, gT[:], g2, g2[0:16, :], ALU.subtract)
                self.cp(g2, g2[32:48, :], grem, grem[:])
                gT = g2

                def gu(e, xt=xt, gT=gT, state=state, t=t):
                    W = wq.pop(0)
                    nid = t * 16 + e + 2
                    if nid < NT * 16:
                        wq.append(loadw(nid % 16))
                    wg_, wu_, wd_ = W
                    pG = psG.next()
                    self.mm(pG, pG[:], sel, sel[0:48, e * 128:(e + 1) * 128], gT, gT[0:48, :], True, True)
                    h = hs.next()
                    for fc in range(2):
                        fs = slice(fc * 128, (fc + 1) * 128)
                        pg, pu = psGU.next(), psGU.next()
                        for k in range(8):
                            self.mm(pg, pg[:], wg_, wg_[:, k, fs], xt, xt[:, k, :], k == 0, k == 7)
                        for k in range(8):
                            self.mm(pu, pu[:], wu_, wu_[:, k, fs], xt, xt[:, k, :], k == 0, k == 7)
                        s_ = sgs.next()
                        self.act(s_, s_[:], pg, pg[:], AF.Silu)
                        tm = tms.next()
                        self.tt(tm, tm[:], s_, s_[:], pu, pu[:], ALU.mult)
                        self.tt(h, h[:, fc, :], tm, tm[:], pG, pG[:], ALU.mult)
                    state[e] = (h, wd_)

                GE = 4

                def gug(gi, gu=gu):
                    for e in range(gi * GE, (gi + 1) * GE):
                        gu(e)

                def down(gi, y=y, state=state):
                    while pending:
                        pending.pop(0)()
                    items = [state.pop(e) for e in range(gi * GE, (gi + 1) * GE)]
                    for sub in range(4):
                        for half in range(2):
                            p = psD.next()
                            for idx, (h, wd_) in enumerate(items):
                                for fc in range(2):
                                    self.mm(p, p[:], h, h[:, fc, sub * 128:(sub + 1) * 128], wd_,
                                            wd_[:, fc, half * 512:(half + 1) * 512], idx == 0 and fc == 0,
                                            idx == GE - 1 and fc == 1)
                            ysl = y[:, sub, half * 512:(half + 1) * 512]
                            self.tt(y, ysl, y, ysl, p, p[:], ALU.add)
                pipeline(16 // GE, gug, down)
                for sub in range(4):
                    self.layer_norm(sm, y, y[:, sub, :], lg_, lb_, y, y[:, sub, :], gb_eng='dve')
                    rows = slice(t * TS + sub * 128, t * TS + (sub + 1) * 128)
                    dstb = self.out if last else self.xres
                    self.stor(dstb, dstb.ap[rows, :], y, y[:, sub, :])
                if not last:
                    def trs(y=y, tc_=tc_):
                        x3T = x3Tt.next()
                        for sub in range(4):
                            self.transpose_to_xT(c, y, y[:, sub, :], psGU, x3T, sub)
                        self.stor(self.xT, self.xT.ap[:, tc_].rearrange("(c p) t -> p c t", p=128), x3T, x3T[:])
                    pending.append(trs)
            while pending:
                pending.pop(0)()
            self.S.barrier()

    def build(self):
        self.declare()
        if self.want('0'):
            self.phase0()
        for l in range(self.layers):
            if self.want('P%d' % l):
                self.phaseP(l)
            if self.want('HA%d' % l):
                self.phaseHA(l)
            if self.want('HB%d' % l):
                self.phaseHB(l)
            if self.want('HC%d' % l):
                self.phaseHC(l)
            if self.want('C1a%d' % l):
                self.phaseC1a(l)
            if self.want('C1b%d' % l):
                self.phaseC1b(l)
            if self.want('C2%d' % l):
                self.phaseC2(l)
        self.S.barrier(final=True)


def _rope_perm():
    perm = np.arange(NCOL)
    def blk(base, nheads):
        out = []
        for m in range(nheads // 4):
            a = [base + (4 * m + jj) * 64 + i for jj in range(4) for i in range(32)]
            b = [base + (4 * m + jj) * 64 + 32 + i for jj in range(4) for i in range(32)]
            out += a + b
        return out
    perm[0:512] = blk(0, 8)
    perm[512:1024] = blk(512, 8)
    perm[3080:3848] = blk(3080, 12)
    perm[3848:4616] = blk(3848, 12)
    return perm


def _consts():
    ident = np.eye(128, dtype=np.float32)
    k = np.arange(128)[:, None]
    q = np.arange(512)[None, :]
    masks = np.concatenate([np.where(128 * a + k <= q, 0.0, NEG).astype(np.float32) for a in range(4)], axis=1)
    qq = np.arange(128)[None, :]
    mprev = np.where(k >= qq, 0.0, NEG).astype(np.float32)
    sel = np.zeros((16, 16 * 128), np.float32)
    for e in range(16):
        sel[e, e * 128:(e + 1) * 128] = 1.0
    invf = (10000.0 ** (-(np.arange(32, dtype=np.float32)) / 32.0)).astype(np.float32)
    invf = np.tile(invf, 4).reshape(128, 1)
    return dict(c_ident=ident, c_masks=np.ascontiguousarray(masks), c_mprev=mprev, c_sel=sel, c_invf=invf)


def make_in_maps(inp, cores=range(8)):
    f = lambda a: np.ascontiguousarray(np.asarray(a, dtype=np.float32))
    sh = {}
    sh["w_in"] = np.ascontiguousarray(f(inp["w_in"])[:, :, _rope_perm()])
    sh["b_forget"] = f(inp["b_forget"]).reshape(DEPTH, 8, 1)
    sh["diff_lambda"] = f(inp["diff_lambda"]).reshape(DEPTH, 1, 256)
    sh["diff_subln"] = f(inp["diff_subln"]).reshape(DEPTH, 128, 1)
    for k in ["w_branch_a", "w_branch_b", "w_branch_c", "w_gate", "w_out", "w_xq", "w_xk", "w_xv", "w_xo",
              "ln_g", "ln_b"]:
        sh[k] = f(inp[k])
    sh["b_gate"] = np.ascontiguousarray(f(inp["b_gate"]).reshape(DEPTH, 24, 128).transpose(0, 2, 1))
    wre = f(inp["w_route_expert"]).transpose(0, 2, 1, 3).reshape(DEPTH, D, 16)
    sh["w_r"] = np.ascontiguousarray(np.concatenate([f(inp["w_route_group"]), wre], axis=2))
    sh["b_r"] = np.ascontiguousarray(np.concatenate([f(inp["b_route_group"]),
                                                     f(inp["b_route_expert"]).reshape(DEPTH, 16)], axis=1)
                                     ).reshape(DEPTH, 1, 20)
    sh["w_eg"] = f(inp["w_expert_gate"]).reshape(DEPTH, 16, D, 256)
    sh["w_eu"] = f(inp["w_expert_up"]).reshape(DEPTH, 16, D, 256)
    sh["w_ed"] = f(inp["w_expert_down"]).reshape(DEPTH, 16, 256, D)
    sh.update(_consts())
    x = f(inp["x"])
    mem = f(inp["mem"])
    pos = np.ascontiguousarray(np.asarray(inp["positions"], dtype=np.int32))
    maps = []
    for b in cores:
        m = dict(sh)
        m["x"] = x[b]
        m["mem"] = mem[b]
        m["pos"] = pos[b:b + 1]
        maps.append(m)
    return maps


def build_program(debug=False, layers=DEPTH, phases=None):
    nc = bass.Bass("TRN2", target_bir_lowering=False)
    with contextlib.ExitStack() as es:
        k = K(nc, es, debug=debug, layers=layers, phases=phases)
        k.build()
    return nc, k


def kernel(**inputs):
    nc, k = build_program()
    maps = make_in_maps(inputs)
    used = set(k.dram.keys())
    maps = [{n: v for n, v in m.items() if n in used} for m in maps]
    res = run_bass_kernel_spmd(nc, maps, core_ids=list(range(8)))
    return np.stack([np.asarray(r["out"], dtype=np.float32) for r in res.results], axis=0)
```

```python
import contextlib
import math
import numpy as np
import concourse.bass as bass
import concourse.mybir as mybir
from concourse.bass_utils import run_bass_kernel_spmd

F32 = mybir.dt.float32
BF16 = mybir.dt.bfloat16
I32 = mybir.dt.int32
AF = mybir.ActivationFunctionType
ALU = mybir.AluOpType
AX = mybir.AxisListType

T = 4096
D = 1024
NT = 8
TS = 512
DEPTH = 2
NCOL = 5384
NEG = -30000.0
EPS = 1e-5
ALPHA = (2 * DEPTH) ** 0.25
KDMA = 8


class Buf:
    def __init__(self, ap, disjoint=False):
        self.ap = ap
        self.w = {}
        self.r = {}
        self.disjoint = disjoint

    def __getitem__(self, k):
        return self.ap[k]


class Sched:
    def __init__(self, nc, es):
        self.nc = nc
        self.E = {'pe': nc.tensor, 'act': nc.scalar, 'dve': nc.vector, 'pool': nc.gpsimd, 'sp': nc.sync,
                  'bg': nc.gpsimd}
        self.psem = {}
        self.pcnt = {}
        for e in ['pe', 'act', 'dve', 'pool']:
            self.psem[e] = es.enter_context(nc.semaphore('p_' + e))
            self.pcnt[e] = 0
        self.dsem = {}
        self.dcnt = {}
        self.drr = {}
        for q in ['sp', 'pool', 'bg']:
            self.dsem[q] = [es.enter_context(nc.semaphore('d_%s%d' % (q, i))) for i in range(KDMA)]
            self.dcnt[q] = [0] * KDMA
            self.drr[q] = 0
        self.seen = {}
        self.nops = 0

    def _wait(self, e, deps):
        if e == 'bg':
            e = 'pool'
        for name, (sem, val) in deps.items():
            key = (e, name)
            if self.seen.get(key, 0) >= val:
                continue
            self.E[e].wait_ge(sem, val)
            self.seen[key] = val

    def _deps(self, e, reads, writes):
        deps = {}

        def add(d):
            for name, (sem, val) in d.items():
                if val > deps.get(name, (None, 0))[1]:
                    deps[name] = (sem, val)
        for b in reads:
            add(b.w)
        for b in writes:
            add(b.r)
            if not b.disjoint:
                add(b.w)
        if e == 'pe':
            deps.pop('p_pe', None)
        return deps

    def _record(self, tok, reads, writes):
        name, sem, val = tok
        for b in reads:
            if val > b.r.get(name, (None, 0))[1]:
                b.r[name] = (sem, val)
        for b in writes:
            if b.disjoint:
                if val > b.w.get(name, (None, 0))[1]:
                    b.w[name] = (sem, val)
            else:
                b.w = {name: (sem, val)}
                b.r = {}

    def op(self, e, emit, reads=(), writes=()):
        self._wait(e, self._deps(e, reads, writes))
        inst = emit(self.E[e])
        self.pcnt[e] += 1
        inst.then_inc(self.psem[e], 1)
        self._record(('p_' + e, self.psem[e], self.pcnt[e]), reads, writes)
        self.nops += 1

    def dma(self, q, out, in_, reads=(), writes=()):
        deps = self._deps(q, reads, writes)
        i = self.drr[q]
        self.drr[q] = (i + 1) % KDMA
        sem = self.dsem[q][i]
        name = 'd_%s%d' % (q, i)
        if self.dcnt[q][i] > 0:
            deps[name] = (sem, self.dcnt[q][i])
        self._wait(q, deps)
        self.E[q].dma_start(out=out, in_=in_).then_inc(sem, 16)
        self.dcnt[q][i] += 16
        self._record((name, sem, self.dcnt[q][i]), reads, writes)
        self.nops += 1

    def barrier(self, final=False):
        allt = {}
        for e in self.psem:
            if self.pcnt[e] > 0:
                allt['p_' + e] = (self.psem[e], self.pcnt[e])
        for q in self.dsem:
            if q == 'bg' and not final:
                continue
            for i in range(KDMA):
                if self.dcnt[q][i] > 0:
                    allt['d_%s%d' % (q, i)] = (self.dsem[q][i], self.dcnt[q][i])
        for e in ['pe', 'act', 'dve', 'pool', 'sp']:
            d = dict(allt)
            if e in self.psem:
                d.pop('p_' + e, None)
            self._wait(e, d)


class Ring:
    def __init__(self, bufs):
        self.bufs = bufs
        self.i = 0

    def next(self):
        b = self.bufs[self.i]
        self.i = (self.i + 1) % len(self.bufs)
        return b


def pipeline(n, first, second, depth=1):
    for i in range(n + depth):
        if i < n:
            first(i)
        if i >= depth:
            second(i - depth)


class K:
    def __init__(self, nc, es, debug=False, layers=DEPTH, phases=None):
        self.nc = nc
        self.es = es
        self.S = Sched(nc, es)
        self.debug = debug
        self.layers = layers
        self.phases = phases
        self.dram = {}
        self.wbf = {}
        self.experts_cast = set()
        self._deferred_loads = []

    def din(self, name, shape, dt=F32):
        t = self.nc.dram_tensor(name, list(shape), dt, kind="ExternalInput").ap()
        self.dram[name] = t
        return t

    def dscr(self, name, shape, dt):
        kind = "ExternalOutput" if self.debug else "Internal"
        t = self.nc.dram_tensor(name, list(shape), dt, kind=kind).ap()
        return Buf(t, disjoint=True)

    def sb(self, st, name, shape, dt, disjoint=False):
        self.uid = getattr(self, 'uid', 0) + 1
        t = st.enter_context(self.nc.sbuf_tensor('%s_%d' % (name, self.uid), list(shape), dt))
        return Buf(t, disjoint=disjoint)

    def ps(self, st, name, shape=(128, 512), dt=F32):
        self.uid = getattr(self, 'uid', 0) + 1
        t = st.enter_context(self.nc.psum_tensor('%s_%d' % (name, self.uid), list(shape), dt))
        return Buf(t)

    def ring(self, st, name, n, shape, dt, psum=False):
        return Ring([(self.ps if psum else self.sb)(st, '%s%d' % (name, i), shape, dt) for i in range(n)])

    def mm(self, out_b, out_ap, lhsT_b, lhsT_ap, rhs_b, rhs_ap, start, stop):
        self.S.op('pe', lambda e: e.matmul(out_ap, lhsT=lhsT_ap, rhs=rhs_ap, start=start, stop=stop),
                  reads=[lhsT_b, rhs_b], writes=[out_b])

    def act(self, out_b, out_ap, in_b, in_ap, func, extra_reads=(), **kw):
        self.S.op('act', lambda e: e.activation(out=out_ap, in_=in_ap, func=func, **kw),
                  reads=[in_b] + list(extra_reads), writes=[out_b])

    def tt(self, out_b, out_ap, a_b, a_ap, b_b, b_ap, op, eng='dve'):
        self.S.op(eng, lambda e: e.tensor_tensor(out=out_ap, in0=a_ap, in1=b_ap, op=op),
                  reads=[a_b, b_b], writes=[out_b])

    def ts(self, out_b, out_ap, a_b, a_ap, s1, s2, op0, op1=None, extra_reads=(), eng='dve'):
        if op1 is None:
            f = lambda e: e.tensor_scalar(out=out_ap, in0=a_ap, scalar1=s1, scalar2=None, op0=op0)
        else:
            f = lambda e: e.tensor_scalar(out=out_ap, in0=a_ap, scalar1=s1, scalar2=s2, op0=op0, op1=op1)
        self.S.op(eng, f, reads=[a_b] + list(extra_reads), writes=[out_b])

    def stt(self, out_b, out_ap, a_b, a_ap, scalar, b_b, b_ap, op0, op1, extra_reads=()):
        self.S.op('dve', lambda e: e.scalar_tensor_tensor(out=out_ap, in0=a_ap, scalar=scalar, in1=b_ap,
                                                          op0=op0, op1=op1),
                  reads=[a_b, b_b] + list(extra_reads), writes=[out_b])

    def cp(self, out_b, out_ap, in_b, in_ap, eng='dve'):
        self.S.op(eng, lambda e: e.tensor_copy(out=out_ap, in_=in_ap), reads=[in_b], writes=[out_b])

    def memset(self, b, ap, val, eng='dve'):
        self.S.op(eng, lambda e: e.memset(ap, val), writes=[b])

    def recip(self, out_b, out_ap, in_b, in_ap):
        self.S.op('dve', lambda e: e.reciprocal(out=out_ap, in_=in_ap), reads=[in_b], writes=[out_b])

    def ld(self, dst_b, dst_ap, src, q='sp', src_b=None):
        self.S.dma(q, dst_ap, src, reads=[src_b] if src_b is not None else [], writes=[dst_b])

    def stor(self, dst_b, dst_ap, src_b, src_ap, q='sp'):
        self.S.dma(q, dst_ap, src_ap, reads=[src_b], writes=[dst_b])

    def declare(self):
        L = DEPTH
        d = self.din
        self.x = d("x", [T, D])
        self.mem = d("mem", [256, D])
        self.pos = d("pos", [1, T], I32)
        self.w_in = d("w_in", [L, D, NCOL])
        self.b_forget = d("b_forget", [L, 8, 1])
        self.diff_lambda = d("diff_lambda", [L, 1, 256])
        self.diff_subln = d("diff_subln", [L, 128, 1])
        self.w_ba = d("w_branch_a", [L, 512, D])
        self.w_bb = d("w_branch_b", [L, 512, D])
        self.w_bc = d("w_branch_c", [L, 256, D])
        self.w_gate = d("w_gate", [L, D, 3072])
        self.b_gate = d("b_gate", [L, 128, 24])
        self.w_out = d("w_out", [L, D, D])
        self.w_xq = d("w_xq", [L, D, D])
        self.w_xk = d("w_xk", [L, D, D])
        self.w_xv = d("w_xv", [L, D, D])
        self.w_xo = d("w_xo", [L, D, D])
        self.w_r = d("w_r", [L, D, 20])
        self.b_r = d("b_r", [L, 1, 20])
        self.w_eg = d("w_eg", [L, 16, D, 256])
        self.w_eu = d("w_eu", [L, 16, D, 256])
        self.w_ed = d("w_ed", [L, 16, 256, D])
        self.ln_g = d("ln_g", [L, 3, D])
        self.ln_b = d("ln_b", [L, 3, D])
        self.c_ident = d("c_ident", [128, 128])
        self.c_masks = d("c_masks", [128, 4 * 512])
        self.c_mprev = d("c_mprev", [128, 128])
        self.c_sel = d("c_sel", [16, 2048])
        self.c_invf = d("c_invf", [128, 1])
        self.out = Buf(self.nc.dram_tensor("out", [T, D], F32, kind="ExternalOutput").ap(), disjoint=True)
        s = self.dscr
        self.xT = s("s_xT", [D, T], BF16)
        self.xres = s("s_xres", [T, D], F32)
        self.qaT = s("s_qaT", [512, T], BF16)
        self.kaT = s("s_kaT", [512, T], BF16)
        self.qbT = s("s_qbT", [512, T], BF16)
        self.kbT = s("s_kbT", [512, T], BF16)
        self.qbaug = s("s_qbaug", [8, 6, T], BF16)
        self.kbaug = s("s_kbaug", [8, 6, T], BF16)
        self.qcT = s("s_qcT", [768, T], BF16)
        self.kcT = s("s_kcT", [768, T], BF16)
        self.va = s("s_va", [T, 512], BF16)
        self.vb = s("s_vb", [T, 520], BF16)
        self.vc = s("s_vc", [T, 780], BF16)
        self.oaT = s("s_oaT", [512, T], BF16)
        self.obT = s("s_obT", [512, T], BF16)
        self.ocT = s("s_ocT", [256, T], BF16)
        self.x1 = s("s_x1", [T, D], F32)
        self.x1T = s("s_x1T", [D, T], BF16)
        self.x2 = s("s_x2", [T, D], F32)
        self.x2T = s("s_x2T", [D, T], BF16)
        self.gateT = s("s_gateT", [16, T], F32)
        mk = lambda n, shp: Buf(self.nc.dram_tensor(n, shp, BF16, kind="Internal").ap(), disjoint=True)
        self.wegb = mk("s_wegb", [DEPTH, 16, D, 256])
        self.weub = mk("s_weub", [DEPTH, 16, D, 256])
        self.wedb = mk("s_wedb", [DEPTH, 16, 256, D])

    def want(self, ph):
        return self.phases is None or ph in self.phases

    def consts(self, st, need_masks=False):
        c = {}
        c['ident_f'] = self.sb(st, 'ident_f', [128, 128], F32)
        c['ident_b'] = self.sb(st, 'ident_b', [128, 128], BF16)
        self.ld(c['ident_f'], c['ident_f'][:], self.c_ident)
        self.ld(c['ident_b'], c['ident_b'][:], self.c_ident, q='pool')
        c['ones_b'] = self.sb(st, 'ones_b', [128, 128], BF16)
        self.memset(c['ones_b'], c['ones_b'][:], 1.0)
        c['ones_f'] = self.sb(st, 'ones_f', [128, 128], F32)
        self.memset(c['ones_f'], c['ones_f'][:], 1.0)
        if need_masks:
            c['masks'] = self.sb(st, 'masks', [128, 4 * 512], BF16)
            self.ld(c['masks'], c['masks'][:], self.c_masks, q='pool')
            c['mprev'] = self.sb(st, 'mprev', [128, 128], BF16)
            self.ld(c['mprev'], c['mprev'][:], self.c_mprev, q='pool')
        return c

    def transpose_to_xT(self, c, x_b, x_ap, psT, xT_b, sub, f32_b=None):
        for half in range(2):
            p = psT.next()
            for k in range(4):
                cc = half * 4 + k
                self.S.op('pe', lambda e, cc=cc, k=k, p=p: e.transpose(p[:, k * 128:(k + 1) * 128],
                                                                     x_ap[:, cc * 128:(cc + 1) * 128],
                                                                     c['ident_f'][:]),
                          reads=[x_b, c['ident_f']], writes=[p])
            self.S.op('act', lambda e, p=p, half=half: e.activation(
                out=xT_b[:, half * 4:half * 4 + 4, sub * 128:(sub + 1) * 128],
                in_=p[:].rearrange("p (k t) -> p k t", k=4), func=AF.Copy), reads=[p], writes=[xT_b])
            if f32_b is not None:
                self.S.op('act', lambda e, p=p, half=half: e.activation(
                    out=f32_b[:, half * 4:half * 4 + 4, :],
                    in_=p[:].rearrange("p (k t) -> p k t", k=4), func=AF.Copy), reads=[p], writes=[f32_b])

    def layer_norm(self, st_bufs, r_b, r_ap, g_b, b_b, out_b, out_ap, gb_eng='pool'):
        stats, mv, sc = st_bufs['stats'], st_bufs['mv'], st_bufs['sc']
        for k in range(2):
            self.S.op('dve', lambda e, k=k: e.bn_stats(out=stats[:, k * 6:(k + 1) * 6],
                                                      in_=r_ap[:, k * 512:(k + 1) * 512]),
                      reads=[r_b], writes=[stats])
        self.S.op('dve', lambda e: e.bn_aggr(out=mv[:, 0:2], in_=stats[:, 0:12]), reads=[stats], writes=[mv])
        self.ts(sc, sc[:, 0:1], mv, mv[:, 1:2], EPS, None, ALU.add)
        self.act(sc, sc[:, 1:2], sc, sc[:, 0:1], AF.Ln)
        self.act(sc, sc[:, 2:3], sc, sc[:, 1:2], AF.Exp, scale=-0.5)
        self.ts(sc, sc[:, 3:4], mv, mv[:, 0:1], sc[:, 2:3], -1.0, ALU.mult, ALU.mult, extra_reads=[sc])
        self.act(out_b, out_ap, r_b, r_ap, AF.Identity, extra_reads=[sc], scale=sc[:, 2:3], bias=sc[:, 3:4])
        self.tt(out_b, out_ap, out_b, out_ap, g_b, g_b[:], ALU.mult, eng=gb_eng)
        self.tt(out_b, out_ap, out_b, out_ap, b_b, b_b[:], ALU.add, eng=gb_eng)

    def phase0(self):
        with contextlib.ExitStack() as st:
            c = self.consts(st)
            xin = self.ring(st, 'p0x', 3, [128, D], F32)
            xTt = self.ring(st, 'p0xT', 2, [128, 8, TS], BF16)
            psT = self.ring(st, 'p0ps', 4, [128, 512], F32, psum=True)
            for t in range(NT):
                xt = xTt.next()
                for sub in range(4):
                    xb = xin.next()
                    r0 = t * TS + sub * 128
                    self.ld(xb, xb[:], self.x[r0:r0 + 128, :])
                    self.transpose_to_xT(c, xb, xb[:], psT, xt, sub)
                self.stor(self.xT, self.xT.ap[:, t * TS:(t + 1) * TS].rearrange("(c p) t -> p c t", p=128),
                          xt, xt[:])
            self.S.barrier()

    def phaseP(self, l):
        S = self.S
        st0 = contextlib.ExitStack()
        fbuf = self.sb(st0, 'fbuf', [8, T], F32, disjoint=True)
        with contextlib.ExitStack() as st:
            groups = [(0, 1536), (1536, 3080), (3080, 4616), (4616, 5384)]
            wg = []
            for gi, (a, b) in enumerate(groups):
                wb = self.sb(st, 'win%d' % gi, [128, 8, b - a], BF16, disjoint=True)
                wg.append(wb)
            order = [0, 2, 1, 3]
            for gi in order:
                a, b = groups[gi]
                if ('w_in', l) in self.wbf:
                    wbb_ = self.wbf[('w_in', l)]
                    for c0 in range(0, 8, 2):
                        self.ld(wg[gi], wg[gi][:, c0:c0 + 2, :],
                                wbb_.ap[c0 * 128:(c0 + 2) * 128, a:b].rearrange("(c p) n -> p c n", p=128),
                                src_b=wbb_)
                else:
                    for cc in range(8):
                        self.ld(wg[gi], wg[gi][:, cc, :], self.w_in[l, cc * 128:(cc + 1) * 128, a:b], q='pool')

            if self.want('C2%d' % l) and l not in self.experts_cast:
                self.precast(l)
                self.experts_cast.add(l)

            def wslice(col, n):
                for gi, (a, b) in enumerate(groups):
                    if a <= col and col + n <= b:
                        return wg[gi], (lambda cc, gi=gi, a=a: wg[gi][:, cc, col - a:col - a + n])
                raise ValueError(col)
            cosT = self.sb(st, 'cosT', [128, T], F32)
            sinT = self.sb(st, 'sinT', [128, T], F32)
            with contextlib.ExitStack() as st2:
                posi = self.sb(st2, 'posi', [128, T], I32)
                ang = self.sb(st2, 'ang', [128, T], F32)
                u = self.sb(st2, 'u', [128, T], F32)
                ki = self.sb(st2, 'ki', [128, T], I32)
                invf = self.sb(st2, 'invf', [128, 1], F32)
                self.ld(invf, invf[:], self.c_invf)
                self.ld(posi, posi[:], self.pos[0, :].partition_broadcast(128))
                self.cp(ang, ang[:], posi, posi[:])
                self.ts(ang, ang[:], ang, ang[:], invf[:, 0:1], None, ALU.mult, extra_reads=[invf])
                for tab, off in ((sinT, 0.0), (cosT, 0.25)):
                    self.ts(u, u[:], ang, ang[:], 1.0 / (2 * math.pi), off, ALU.mult, ALU.add)
                    self.cp(ki, ki[:], u, u[:])
                    self.cp(tab, tab[:], ki, ki[:])
                    self.tt(u, u[:], u, u[:], tab, tab[:], ALU.subtract)
                    self.ts(tab, tab[:], u, u[:], 0.5, None, ALU.is_gt)
                    self.tt(u, u[:], u, u[:], tab, tab[:], ALU.subtract)
                    self.ts(tab, tab[:], u, u[:], -0.5, None, ALU.is_lt)
                    self.tt(u, u[:], u, u[:], tab, tab[:], ALU.add)
                    self.act(tab, tab[:], u, u[:], AF.Sin, scale=2 * math.pi)
                S.barrier()
            bf = self.sb(st, 'bfg', [8, 1], F32)
            self.ld(bf, bf[:], self.b_forget[l])
            self.ts(bf, bf[:], bf, bf[:], -1.0, None, ALU.mult)
            xTt = self.ring(st, 'pxT', 2, [128, 8, TS], BF16)
            pss = self.ring(st, 'pps', 7, [128, 512], F32, psum=True)
            stg = self.ring(st, 'pstg', 8, [128, TS], BF16)
            tmp = self.ring(st, 'ptmp', 4, [128, TS], F32)
            sva = self.ring(st, 'psva', 3, [128, 512], BF16)
            svb = self.ring(st, 'psvb', 2, [128, 8, 65], BF16)
            svc = self.ring(st, 'psvc', 2, [128, 12, 65], BF16)
            for b in svb.bufs + svc.bufs:
                self.memset(b, b[:], 1.0)

            def load_x(t):
                xt = xTt.next()
                self.ld(xt, xt[:], self.xT.ap[:, t * TS:(t + 1) * TS].rearrange("(c p) t -> p c t", p=128),
                        src_b=self.xT)
                return xt

            def proj_fm(xt, col, m=128):
                p = pss.next()
                wb, wf = wslice(col, m)
                for cc in range(8):
                    self.mm(p, p[0:m, :], wb, wf(cc), xt, xt[:, cc, :], cc == 0, cc == 7)
                return p
            nxt = load_x(0)
            for t in range(NT):
                xt = nxt
                if t + 1 < NT:
                    nxt = load_x(t + 1)
                tc_ = slice(t * TS, (t + 1) * TS)
                for (base, dst, npair) in ((0, self.qaT, 2), (512, self.kaT, 2), (3080, self.qcT, 3),
                                           (3848, self.kcT, 3)):
                    for m in range(npair):
                        pa = proj_fm(xt, base + 256 * m)
                        pb = proj_fm(xt, base + 256 * m + 128)
                        t1, t2, t3, t4 = tmp.next(), tmp.next(), tmp.next(), tmp.next()
                        self.tt(t1, t1[:], pa, pa[:], cosT, cosT[:, tc_], ALU.mult)
                        self.tt(t2, t2[:], pb, pb[:], sinT, sinT[:, tc_], ALU.mult)
                        self.tt(t3, t3[:], pb, pb[:], cosT, cosT[:, tc_], ALU.mult)
                        self.tt(t4, t4[:], pa, pa[:], sinT, sinT[:, tc_], ALU.mult)
                        o1, o2 = stg.next(), stg.next()
                        self.tt(o1, o1[:], t1, t1[:], t2, t2[:], ALU.subtract)
                        self.tt(o2, o2[:], t3, t3[:], t4, t4[:], ALU.add)
                        self.stor(dst, dst.ap[(2 * m) * 128:(2 * m + 1) * 128, tc_], o1, o1[:])
                        self.stor(dst, dst.ap[(2 * m + 1) * 128:(2 * m + 2) * 128, tc_], o2, o2[:])
                for (base, dst) in ((1536, self.qbT), (2048, self.kbT)):
                    for m in range(4):
                        p = proj_fm(xt, base + 128 * m)
                        o = stg.next()
                        self.act(o, o[:], p, p[:], AF.Copy)
                        self.stor(dst, dst.ap[m * 128:(m + 1) * 128, tc_], o, o[:])
                p = proj_fm(xt, 3072, 8)
                self.act(fbuf, fbuf[:, tc_], p, p[0:8, :], AF.Exp, extra_reads=[bf], scale=-1.0, bias=bf[:, 0:1])
                for sub in range(4):
                    rows = slice(t * TS + sub * 128, t * TS + (sub + 1) * 128)
                    xs = slice(sub * 128, (sub + 1) * 128)

                    def proj_tm(col, n):
                        p = pss.next()
                        wb, wf = wslice(col, n)
                        for cc in range(8):
                            self.mm(p, p[:, 0:n], xt, xt[:, cc, xs], wb, wf(cc), cc == 0, cc == 7)
                        return p
                    p = proj_tm(1024, 512)
                    o = sva.next()
                    self.act(o, o[:], p, p[:], AF.Copy)
                    self.stor(self.va, self.va.ap[rows, :], o, o[:])
                    p = proj_tm(2560, 512)
                    o = svb.next()
                    self.act(o, o[:, :, 0:64], p, p[:].rearrange("p (h e) -> p h e", h=8), AF.Copy)
                    self.stor(self.vb, self.vb.ap[rows, :], o, o[:].rearrange("p h e -> p (h e)"))
                    p = proj_tm(4616, 512)
                    p2 = proj_tm(4616 + 512, 256)
                    o = svc.next()
                    self.act(o, o[:, 0:8, 0:64], p, p[:].rearrange("p (h e) -> p h e", h=8), AF.Copy)
                    self.act(o, o[:, 8:12, 0:64], p2, p2[:, 0:256].rearrange("p (h e) -> p h e", h=4), AF.Copy)
                    self.stor(self.vc, self.vc.ap[rows, :], o, o[:].rearrange("p h e -> p (h e)"))
            S.barrier()
        with st0 as st:
            lg = fbuf
            self.act(lg, lg[:], fbuf, fbuf[:], AF.Ln, bias=1.0)
            onesf = self.sb(st, 'f1', [8, T], F32)
            self.memset(onesf, onesf[:], 1.0)
            cs = self.sb(st, 'fcs', [8, T], F32)
            self.S.op('dve', lambda e: e.tensor_tensor_scan(out=cs[:], data0=onesf[:], data1=lg[:], initial=0.0,
                                                            op0=ALU.mult, op1=ALU.add),
                      reads=[onesf, lg], writes=[cs])
            self.ts(cs, cs[:], cs, cs[:], 8.0, None, ALU.mult)
            parts = []
            rem = cs
            for i in range(3):
                pb_ = self.sb(st, 'fp%d' % i, [8, T], BF16)
                self.cp(pb_, pb_[:], rem, rem[:])
                parts.append(pb_)
                if i < 2:
                    nr = self.sb(st, 'fr%d' % i, [8, T], F32)
                    self.tt(nr, nr[:], rem, rem[:], pb_, pb_[:], ALU.subtract)
                    rem = nr
            onesb = self.sb(st, 'f1b', [8, T], BF16)
            self.memset(onesb, onesb[:], 1.0)
            for i in range(3):
                ng = self.sb(st, 'fn%d' % i, [8, T], BF16)
                self.ts(ng, ng[:], parts[i], parts[i][:], -1.0, None, ALU.mult)
                self.stor(self.qbaug, self.qbaug.ap[:, i, :], ng, ng[:])
                self.stor(self.qbaug, self.qbaug.ap[:, 3 + i, :], onesb, onesb[:])
                self.stor(self.kbaug, self.kbaug.ap[:, i, :], onesb, onesb[:])
                self.stor(self.kbaug, self.kbaug.ap[:, 3 + i, :], parts[i], parts[i][:])
            S.barrier()

    def load_rope_head(self, dst, rows0, src, j):
        m, jj = j // 4, j % 4
        self.ld(dst, dst[rows0:rows0 + 32, :], src.ap[(2 * m) * 128 + jj * 32:(2 * m) * 128 + jj * 32 + 32, :],
                src_b=src)
        self.ld(dst, dst[rows0 + 32:rows0 + 64, :],
                src.ap[(2 * m + 1) * 128 + jj * 32:(2 * m + 1) * 128 + jj * 32 + 32, :], src_b=src)

    def phaseHA(self, l):
        self.issue_bg('HA%d' % l)
        lam_init = 0.8 - 0.6 * math.exp(-0.3 * l)
        with contextlib.ExitStack() as st:
            c = self.consts(st, need_masks=True)
            dl = self.sb(st, 'dl', [128, 256], F32)
            self.ld(dl, dl[:], self.diff_lambda[l, 0, :].partition_broadcast(128))
            lt = self.sb(st, 'lt', [128, 8], F32)
            pr = self.sb(st, 'lpr', [128, 128], F32)
            self.tt(pr, pr[:, 0:64], dl, dl[:, 0:64], dl, dl[:, 64:128], ALU.mult)
            self.tt(pr, pr[:, 64:128], dl, dl[:, 128:192], dl, dl[:, 192:256], ALU.mult)
            self.S.op('dve', lambda e: e.tensor_reduce(out=lt[:, 0:2], in_=pr[:].rearrange("p (a b) -> p a b", a=2),
                                                      axis=AX.X, op=ALU.add), reads=[pr], writes=[lt])
            self.act(lt, lt[:, 2:4], lt, lt[:, 0:2], AF.Exp)
            self.tt(lt, lt[:, 4:5], lt, lt[:, 2:3], lt, lt[:, 3:4], ALU.subtract)
            self.ts(lt, lt[:, 5:6], lt, lt[:, 4:5], lam_init, -1.0, ALU.add, ALU.mult)
            sub = self.sb(st, 'subln', [128, 1], F32)
            self.ld(sub, sub[:], self.diff_subln[l])
            self.ts(sub, sub[:], sub, sub[:], 1.0 - lam_init, None, ALU.mult)
            onesm = self.sb(st, 'onesm', [128, 128], BF16)
            self.memset(onesm, onesm[:], 1.0 / 128.0)
            V = self.sb(st, 'haV', [128, 32, 512], BF16)
            self.ld(V, V[:], self.va.ap.rearrange("(n p) e -> p n e", p=128), src_b=self.va)
            qTs = self.ring(st, 'haq', 2, [128, T], BF16)
            kTs = self.ring(st, 'hak', 2, [128, T], BF16)
            for b in qTs.bufs + kTs.bufs:
                b.disjoint = True
            psS = self.ring(st, 'haS', 2, [128, 1024], F32, psum=True)
            acc = [self.ps(st, 'haacc%d' % i) for i in range(4)]
            Ps = self.ring(st, 'haP', 3, [128, 1024], BF16)
            fins = Ring([[self.sb(st, 'hafin%d_%d' % (a_, i), [128, 512], F32) for i in range(4)] for a_ in range(2)])
            sqbs = self.ring(st, 'sqb', 2, [128, 512], BF16)
            deferred = []
            ostg = self.ring(st, 'haost', 2, [128, 512], BF16)

            def load_head(h):
                q, k = qTs.next(), kTs.next()
                for rr in range(2):
                    self.load_rope_head(q, rr * 64, self.qaT, 2 * h + rr)
                    self.load_rope_head(k, rr * 64, self.kaT, 2 * h + rr)
                return q, k
            nxt = load_head(0)
            for h in range(4):
                q, k = nxt
                if h + 1 < 4:
                    nxt = load_head(h + 1)
                for j in range(NT):
                    nk = 4 * j + 4
                    qs = slice(j * TS, (j + 1) * TS)
                    state = {}

                    def qk(i, q=q, k=k, j=j, qs=qs, state=state):
                        if i == 3:
                            while deferred:
                                deferred.pop(0)()
                        p = psS.next()
                        diag = i >= 4 * j
                        for mp in range(2):
                            r = slice(mp * 64, mp * 64 + 64)
                            po = p[:, mp * 512:(mp + 1) * 512]
                            self.mm(p, po, k, k[r, i * 128:(i + 1) * 128], q, q[r, qs], True, not diag)
                            if diag:
                                a = i - 4 * j
                                self.mm(p, po, c['ident_b'], c['ident_b'][:], c['masks'],
                                        c['masks'][:, a * 512:(a + 1) * 512], False, True)
                        P = Ps.next()
                        self.act(P, P[:], p, p[:], AF.Exp, scale=0.125)
                        state[i] = P

                    def pv(i, h=h, state=state, nk=nk):
                        P = state.pop(i)
                        first, last = (i == 0), (i == nk - 1)
                        for mp in range(2):
                            Pm = P[:, mp * 512:(mp + 1) * 512]
                            self.mm(acc[2 * mp], acc[2 * mp][:], V, V[:, i, h * 128:(h + 1) * 128], P, Pm, first, last)
                            self.mm(acc[2 * mp + 1], acc[2 * mp + 1][:], c['ones_b'], c['ones_b'][:], P, Pm, first, last)
                    pipeline(nk, qk, pv, depth=1)
                    f0, f1, f2, f3 = fins.next()
                    sqb = sqbs.next()
                    self.act(f0, f0[:], acc[1], acc[1][:], AF.Ln)
                    self.act(f2, f2[:], acc[3], acc[3][:], AF.Ln)
                    self.cp(f1, f1[:], acc[0], acc[0][:])
                    self.cp(f3, f3[:], acc[2], acc[2][:])
                    self.act(f0, f0[:], f0, f0[:], AF.Exp, scale=-1.0)
                    self.act(f2, f2[:], f2, f2[:], AF.Exp, scale=-1.0)
                    self.tt(f1, f1[:], f1, f1[:], f0, f0[:], ALU.mult)
                    self.tt(f3, f3[:], f3, f3[:], f2, f2[:], ALU.mult)
                    self.stt(f1, f1[:], f3, f3[:], lt[:, 5:6], f1, f1[:], ALU.mult, ALU.add, extra_reads=[lt])
                    self.tt(sqb, sqb[:], f1, f1[:], f1, f1[:], ALU.mult)

                    def finb(f1=f1, f2=f2, sqb=sqb, h=h, qs=qs):
                        psMb = psS.next()
                        psM = Buf(psMb.ap[:, 0:512])
                        psM.w, psM.r = psMb.w, psMb.r
                        self.mm(psM, psM[:], onesm, onesm[:], sqb, sqb[:], True, True)
                        self.act(f2, f2[:], psM, psM[:], AF.Ln, bias=EPS)
                        psMb.w, psMb.r = psM.w, psM.r
                        self.act(f2, f2[:], f2, f2[:], AF.Exp, scale=-0.5)
                        self.tt(f1, f1[:], f1, f1[:], f2, f2[:], ALU.mult)
                        o = ostg.next()
                        self.ts(o, o[:], f1, f1[:], sub[:, 0:1], None, ALU.mult, extra_reads=[sub])
                        self.stor(self.oaT, self.oaT.ap[h * 128:(h + 1) * 128, qs], o, o[:])
                    deferred.append(finb)
            while deferred:
                deferred.pop(0)()
            self.S.barrier()

    def fin_norm_a(self, accb, fo, fr):
        self.act(fo, fo[0:65, :], accb, accb[0:65, :], AF.Copy)
        fl, fb = fr['l'], fr['b']
        self.act(fl, fl[64:65, :], fo, fo[64:65, :], AF.Ln)
        self.act(fl, fl[64:65, :], fl, fl[64:65, :], AF.Exp, scale=-1.0)
        self.cp(fb, fb[64:65, :], fl, fl[64:65, :])
        self.tt(fl, fl[64:65, :], fl, fl[64:65, :], fb, fb[64:65, :], ALU.subtract)
        self.cp(fb, fb[96:97, :], fl, fl[64:65, :])

    def fin_norm_b(self, c, fo, fr, psB, ostg, dst, dst_ap):
        fb = fr['b']
        self.mm(psB, psB[0:64, :], c['ones_b'], c['ones_b'][64:97, 0:64], fb, fb[64:97, :], True, True)
        o = ostg.next()
        self.tt(o, o[0:64, :], fo, fo[0:64, :], psB, psB[0:64, :], ALU.mult)
        self.stor(dst, dst_ap, o, o[0:64, :])

    def mk_fr(self, st, name):
        fl = self.sb(st, name + 'l', [128, 512], F32)
        fb = self.sb(st, name + 'b', [128, 512], BF16)
        self.memset(fb, fb[:], 0.0)
        return {'l': fl, 'b': fb}

    def phaseHB(self, l):
        self.issue_bg('HB%d' % l)
        with contextlib.ExitStack() as st:
            c = self.consts(st, need_masks=True)
            V = self.sb(st, 'hbV', [128, 32, 520], BF16)
            self.ld(V, V[:], self.vb.ap.rearrange("(n p) e -> p n e", p=128), src_b=self.vb)
            qTs = self.ring(st, 'hbq', 2, [70, T], BF16)
            kTs = self.ring(st, 'hbk', 2, [70, T], BF16)
            for b in qTs.bufs + kTs.bufs:
                b.disjoint = True
            psS = self.ring(st, 'hbS', 3, [128, 1024], F32, psum=True)
            accs = self.ring(st, 'hbacc', 2, [128, 512], F32, psum=True)
            Ps = self.ring(st, 'hbP', 4, [128, 1024], BF16)
            fos = self.ring(st, 'hbfo', 2, [128, 512], F32)
            frs = Ring([self.mk_fr(st, 'hbfr%d' % i) for i in range(2)])
            deferred = []
            ostg = self.ring(st, 'hbost', 2, [128, 512], BF16)

            def load_head(h):
                q, k = qTs.next(), kTs.next()
                self.ld(q, q[0:64, :], self.qbT.ap[h * 64:(h + 1) * 64, :], src_b=self.qbT)
                self.ld(k, k[0:64, :], self.kbT.ap[h * 64:(h + 1) * 64, :], src_b=self.kbT)
                self.ld(q, q[64:70, :], self.qbaug.ap[h], src_b=self.qbaug)
                self.ld(k, k[64:70, :], self.kbaug.ap[h], src_b=self.kbaug)
                return q, k
            nxt = load_head(0)
            for h in range(8):
                q, k = nxt
                if h + 1 < 8:
                    nxt = load_head(h + 1)
                for j in range(NT):
                    nk = 4 * j + 4
                    qs = slice(j * TS, (j + 1) * TS)
                    state = {}
                    acc = accs.next()

                    def qk(u, q=q, k=k, j=j, qs=qs, state=state, nk=nk):
                        if u == min(3, nk // 2 - 1):
                            while deferred:
                                deferred.pop(0)()
                        p = psS.next()
                        for w in range(2):
                            i = 2 * u + w
                            po = p[:, w * 512:(w + 1) * 512]
                            diag = i >= 4 * j
                            self.mm(p, po, k, k[0:70, i * 128:(i + 1) * 128], q, q[0:70, qs], True, not diag)
                            if diag:
                                a = i - 4 * j
                                self.mm(p, po, c['ident_b'], c['ident_b'][:], c['masks'],
                                        c['masks'][:, a * 512:(a + 1) * 512], False, True)
                        P = Ps.next()
                        self.act(P, P[:], p, p[:], AF.Exp, scale=0.125)
                        state[u] = P

                    def pv(u, h=h, nk=nk, state=state, acc=acc):
                        P = state.pop(u)
                        for w in range(2):
                            i = 2 * u + w
                            self.mm(acc, acc[0:65, :], V, V[:, i, h * 65:(h + 1) * 65], P, P[:, w * 512:(w + 1) * 512],
                                    i == 0, i == nk - 1)
                    pipeline(nk // 2, qk, pv, depth=2)
                    fo, fr = fos.next(), frs.next()
                    self.fin_norm_a(acc, fo, fr)

                    def finb(fo=fo, fr=fr, h=h, qs=qs):
                        psBb = psS.next()
                        psB = Buf(psBb.ap[:, 0:512])
                        psB.w, psB.r = psBb.w, psBb.r
                        self.fin_norm_b(c, fo, fr, psB, ostg, self.obT, self.obT.ap[h * 64:(h + 1) * 64, qs])
                        psBb.w, psBb.r = psB.w, psB.r
                    deferred.append(finb)
            while deferred:
                deferred.pop(0)()
            self.S.barrier()

    def phaseHC(self, l):
        dil = (1, 4, 16)
        with contextlib.ExitStack() as st:
            c = self.consts(st, need_masks=True)
            Vg = []
            for g in range(3):
                d = dil[g]
                v = self.sb(st, 'hcV%d' % g, [128, 32, 260], BF16, disjoint=True)
                src = self.vc.ap[:, g * 260:(g + 1) * 260].rearrange("(b kj cl) e -> kj cl b e", kj=128, cl=d)
                for cl in range(d):
                    nb = 32 // d
                    self.ld(v, v[:, cl * nb:(cl + 1) * nb, :], src[:, cl], src_b=self.vc)
                Vg.append(v)
            qTs = self.ring(st, 'hcq', 2, [64, T], BF16)
            kTs = self.ring(st, 'hck', 2, [64, T], BF16)
            for b in qTs.bufs + kTs.bufs:
                b.disjoint = True
            psC = self.ring(st, 'hcSc', 2, [128, 512], F32, psum=True)
            psP = self.ring(st, 'hcSp', 2, [128, 512], F32, psum=True)
            psO = self.ring(st, 'hcO', 2, [128, 512], F32, psum=True)
            psB = self.ps(st, 'hcB')
            Pc = self.ring(st, 'hcPc', 2, [128, 512], BF16)
            Pp = self.ring(st, 'hcPp', 2, [128, 512], BF16)
            accs = self.ring(st, 'hcacc', 2, [65, T], F32)
            fr = self.mk_fr(st, 'hcfr')
            ostg = self.ring(st, 'hcost', 2, [128, 512], BF16)
            heads = [(s, g) for s in range(4) for g in range(3)]

            def load_head(n):
                s, g = heads[n]
                q, k = qTs.next(), kTs.next()
                self.load_rope_head(q, 0, self.qcT, g * 4 + s)
                self.load_rope_head(k, 0, self.kcT, g * 4 + s)
                return q, k
            nxt = load_head(0)
            for n, (s, g) in enumerate(heads):
                q, k = nxt
                if n + 1 < len(heads):
                    nxt = load_head(n + 1)
                if g == 0:
                    acc = accs.next()
                d = dil[g]
                nb = 32 // d
                V = Vg[g]

                def tsl(cl, b, d=d):
                    start = b * 128 * d + cl
                    return slice(start, start + 127 * d + 1, d)
                state = {}

                def qk(u, q=q, k=k, state=state, nb=nb, tsl=tsl):
                    pc, pp = psC.next(), psP.next()
                    for bb in range(4):
                        L = 4 * u + bb
                        cl, b = L // nb, L % nb
                        cs = slice(bb * 128, (bb + 1) * 128)
                        self.mm(pc, pc[:, cs], k, k[0:64, tsl(cl, b)], q, q[0:64, tsl(cl, b)], True, False)
                        self.mm(pc, pc[:, cs], c['ident_b'], c['ident_b'][:], c['masks'], c['masks'][:, 0:128],
                                False, True)
                        if b > 0:
                            self.mm(pp, pp[:, cs], k, k[0:64, tsl(cl, b - 1)], q, q[0:64, tsl(cl, b)], True, False)
                            self.mm(pp, pp[:, cs], c['ident_b'], c['ident_b'][:], c['mprev'], c['mprev'][:],
                                    False, True)
                        else:
                            self.mm(pp, pp[:, cs], c['ident_b'], c['ident_b'][:], c['masks'], c['masks'][:, 0:128],
                                    True, True)
                    a, b_ = Pc.next(), Pp.next()
                    self.act(a, a[:], pc, pc[:], AF.Exp, scale=0.125)
                    self.act(b_, b_[:], pp, pp[:], AF.Exp, scale=0.125)
                    state[u] = (a, b_)

                def pv(u, s=s, g=g, V=V, state=state, nb=nb, d=d, acc=acc, tsl=tsl):
                    a, b_ = state.pop(u)
                    po = psO.next()
                    for bb in range(4):
                        L = 4 * u + bb
                        cl, b = L // nb, L % nb
                        cs = slice(bb * 128, (bb + 1) * 128)
                        self.mm(po, po[0:65, cs], V, V[:, L, s * 65:(s + 1) * 65], a, a[:, cs], True, b == 0)
                        if b > 0:
                            self.mm(po, po[0:65, cs], V, V[:, L - 1, s * 65:(s + 1) * 65], b_, b_[:, cs], False, True)
                    if d == 16:
                        runs = [(0, 256, (4 * u) // nb), (256, 512, (4 * u) // nb + 1)]
                    else:
                        runs = [(0, 512, (4 * u) // nb)]
                    for (c0, c1, cl) in runs:
                        b0 = (4 * u + c0 // 128) % nb
                        start = b0 * 128 * d + cl
                        cnt = c1 - c0
                        sl = slice(start, start + (cnt - 1) * d + 1, d)
                        if g == 0:
                            self.cp(acc, acc[0:65, sl], po, po[0:65, c0:c1])
                        else:
                            self.tt(acc, acc[0:65, sl], acc, acc[0:65, sl], po, po[0:65, c0:c1], ALU.add)
                pipeline(8, qk, pv)
                if g == 2:
                    for j in range(NT):
                        qs = slice(j * TS, (j + 1) * TS)
                        fl, fb = fr['l'], fr['b']
                        self.act(fl, fl[64:65, :], acc, acc[64:65, qs], AF.Ln)
                        self.act(fl, fl[64:65, :], fl, fl[64:65, :], AF.Exp, scale=-1.0)
                        self.cp(fb, fb[64:65, :], fl, fl[64:65, :])
                        self.tt(fl, fl[64:65, :], fl, fl[64:65, :], fb, fb[64:65, :], ALU.subtract)
                        self.cp(fb, fb[96:97, :], fl, fl[64:65, :])
                        self.mm(psB, psB[0:64, :], c['ones_b'], c['ones_b'][64:97, 0:64], fb, fb[64:97, :], True, True)
                        o = ostg.next()
                        self.tt(o, o[0:64, :], acc, acc[0:64, qs], psB, psB[0:64, :], ALU.mult)
                        self.stor(self.ocT, self.ocT.ap[s * 64:(s + 1) * 64, qs], o, o[0:64, :])
            self.S.barrier()

    def precast_w(self, key, src):
        R, C = src.shape
        sp = 1
        while C // sp > 2048:
            sp *= 2
        dst = Buf(self.nc.dram_tensor("wb_%s_%d" % key, [R, C], BF16, kind="Internal").ap(), disjoint=True)
        sv = src.rearrange("r (s c) -> (r s) c", s=sp)
        dv = dst.ap.rearrange("r (s c) -> (r s) c", s=sp)
        R2 = R * sp
        for r0 in range(0, R2, 1024):
            r1 = min(R2, r0 + 1024)
            self.S.dma('bg', dv[r0:r1].rearrange("(p k) c -> p k c", p=128),
                       sv[r0:r1].rearrange("(p k) c -> p k c", p=128), writes=[dst])
        self.wbf[key] = dst

    def issue_bg(self, phase):
        srcs = {'w_gate': self.w_gate, 'w_ba': self.w_ba, 'w_bb': self.w_bb, 'w_bc': self.w_bc,
                'w_out': self.w_out, 'w_xq': self.w_xq, 'w_xo': self.w_xo, 'w_xk': self.w_xk,
                'w_xv': self.w_xv, 'w_in': self.w_in}
        c1a = ['w_gate', 'w_ba', 'w_bb', 'w_bc', 'w_out']
        c1b = ['w_xq', 'w_xo', 'w_xk', 'w_xv']
        sched = {'HA0': [(n, 0) for n in c1a], 'HB0': [(n, 0) for n in c1b] + [('experts', 0)],
                 'C1a0': [('w_in', 1)], 'C1b0': [(n, 1) for n in c1a],
                 'C20': [(n, 1) for n in c1b] + [('experts', 1)]}
        if True:
            return
        for key in sched.get(phase, []):
            nm, l = key
            if l >= self.layers:
                continue
            if nm == 'experts':
                self.precast(l)
                self.experts_cast.add(l)
            else:
                self.precast_w(key, srcs[nm][l])

    def load_w(self, st, name, src, kc, n, key=None, defer=False):
        w = self.sb(st, name, [128, kc, n], BF16, disjoint=True)
        if defer:
            self._deferred_loads.append(lambda: self._issue_w(w, src, kc, n, key))
            return w
        self._issue_w(w, src, kc, n, key)
        return w

    def _issue_w(self, w, src, kc, n, key):
        if key is not None and key in self.wbf:
            b = self.wbf[key]
            for k0 in range(0, kc, 2):
                k1 = min(kc, k0 + 2)
                self.ld(w, w[:, k0:k1, :], b.ap[k0 * 128:k1 * 128, :].rearrange("(c p) n -> p c n", p=128), src_b=b)
            return w
        step = max(1, 2048 // n)
        for k0 in range(0, kc, step):
            k1 = min(kc, k0 + step)
            if n <= 2048:
                self.ld(w, w[:, k0:k1, :], src[k0 * 128:k1 * 128, :].rearrange("(c p) n -> p c n", p=128), q='pool')
            else:
                for n0 in range(0, n, 1536):
                    n1 = min(n, n0 + 1536)
                    self.ld(w, w[:, k0, n0:n1], src[k0 * 128:(k0 + 1) * 128, n0:n1], q='pool')
        return w

    def ln_tiles(self, st, l, idx):
        g = self.sb(st, 'lng', [128, D], F32)
        b = self.sb(st, 'lnb', [128, D], F32)
        self.ld(g, g[:], self.ln_g[l, idx, :].partition_broadcast(128))
        self.ld(b, b[:], self.ln_b[l, idx, :].partition_broadcast(128))
        sm = {'stats': self.sb(st, 'lnst', [128, 12], F32), 'mv': self.sb(st, 'lnmv', [128, 2], F32),
              'sc': self.sb(st, 'lnsc', [128, 4], F32)}
        return g, b, sm

    def phaseC1a(self, l):
        self.issue_bg('C1a%d' % l)
        xsrc = self.x if l == 0 else self.xres.ap
        xsrc_b = None if l == 0 else self.xres
        with contextlib.ExitStack() as st:
            c = self.consts(st)
            Wg = self.load_w(st, 'wgate', self.w_gate[l], 8, 3072, key=('w_gate', l))
            Wa = self.load_w(st, 'wba', self.w_ba[l], 4, 1024, key=('w_ba', l))
            Wb = self.load_w(st, 'wbb', self.w_bb[l], 4, 1024, key=('w_bb', l))
            Wc = self.load_w(st, 'wbc', self.w_bc[l], 2, 1024, key=('w_bc', l))
            Wo = self.load_w(st, 'wout', self.w_out[l], 8, 1024, key=('w_out', l))
            bg = self.sb(st, 'bgate', [128, 24], F32)
            self.ld(bg, bg[:], self.b_gate[l])
            lg_, lb_, sm = self.ln_tiles(st, l, 0)
            xTt = self.ring(st, 'c1xT', 2, [128, 8, TS], BF16)
            oat = self.ring(st, 'c1oa', 2, [128, 4, TS], BF16)
            obt = self.ring(st, 'c1ob', 2, [128, 4, TS], BF16)
            oct_ = self.ring(st, 'c1oc', 2, [128, 2, TS], BF16)
            xrs = self.ring(st, 'c1xr', 1, [128, 4, D], F32)
            mT = self.sb(st, 'c1mT', [128, 8, TS], BF16)
            sg = self.ring(st, 'c1sg', 3, [128, TS], F32)
            mm_ = self.ring(st, 'c1mm', 3, [128, TS], F32)
            x1Tt = self.ring(st, 'c1x1T', 2, [128, 8, TS], BF16)
            pss = self.ring(st, 'c1ps', 8, [128, 512], F32, psum=True)

            def loads(t):
                tc_ = slice(t * TS, (t + 1) * TS)
                a, b, cc_, xt = oat.next(), obt.next(), oct_.next(), xTt.next()
                self.ld(xt, xt[:], self.xT.ap[:, tc_].rearrange("(c p) t -> p c t", p=128), src_b=self.xT)
                self.ld(a, a[:], self.oaT.ap[:, tc_].rearrange("(c p) t -> p c t", p=128), src_b=self.oaT)
                self.ld(b, b[:], self.obT.ap[:, tc_].rearrange("(c p) t -> p c t", p=128), src_b=self.obT)
                self.ld(cc_, cc_[:], self.ocT.ap[:, tc_].rearrange("(c p) t -> p c t", p=128), src_b=self.ocT)
                return a, b, cc_, xt
            rr4 = self.ring(st, 'c1r4', 4, [128, D], F32)
            tiles = {}

            prea = {}

            def stageA(t):
                a, b, cc_, xt = prea.pop(t)
                for ch in range(8):
                    cs = slice(ch * 128, (ch + 1) * 128)
                    ms = []
                    for bi, (W, o, kc) in enumerate(((Wa, a, 4), (Wb, b, 4), (Wc, cc_, 2))):
                        pg = pss.next()
                        for k in range(8):
                            self.mm(pg, pg[:], Wg, Wg[:, k, bi * 1024 + ch * 128:bi * 1024 + (ch + 1) * 128],
                                    xt, xt[:, k, :], k == 0, k == 7)
                        pb = pss.next()
                        for k in range(kc):
                            self.mm(pb, pb[:], W, W[:, k, cs], o, o[:, k, :], k == 0, k == kc - 1)
                        s_ = sg.next()
                        self.act(s_, s_[:], pg, pg[:], AF.Sigmoid, extra_reads=[bg],
                                 bias=bg[:, bi * 8 + ch:bi * 8 + ch + 1])
                        m = mm_.next()
                        self.tt(m, m[:], s_, s_[:], pb, pb[:], ALU.mult)
                        ms.append(m)
                    self.tt(ms[0], ms[0][:], ms[0], ms[0][:], ms[1], ms[1][:], ALU.add)
                    self.tt(mT, mT[:, ch, :], ms[0], ms[0][:], ms[2], ms[2][:], ALU.add)

            def stageB(t):
                tc_ = slice(t * TS, (t + 1) * TS)
                xr = xrs.next()
                self.ld(xr, xr[:], xsrc[tc_, :].rearrange("(s p) d -> p s d", p=128), src_b=xsrc_b)
                rs = []
                for sub in range(4):
                    r = rr4.next()
                    rs.append(r)
                    for half in range(2):
                        p = pss.next()
                        for k in range(8):
                            self.mm(p, p[:], mT, mT[:, k, sub * 128:(sub + 1) * 128], Wo,
                                    Wo[:, k, half * 512:(half + 1) * 512], k == 0, k == 7)
                        self.stt(r, r[:, half * 512:(half + 1) * 512], xr, xr[:, sub, half * 512:(half + 1) * 512],
                                 ALPHA, p, p[:], ALU.mult, ALU.add)
                for sub in range(4):
                    r = rs[sub]
                    self.layer_norm(sm, r, r[:], lg_, lb_, r, r[:])
                    rows = slice(t * TS + sub * 128, t * TS + (sub + 1) * 128)
                    self.stor(self.x1, self.x1.ap[rows, :], r, r[:])
                tiles[t] = rs

            def stageT(t):
                tc_ = slice(t * TS, (t + 1) * TS)
                rs = tiles.pop(t)
                x1T = x1Tt.next()
                for sub in range(4):
                    self.transpose_to_xT(c, rs[sub], rs[sub][:], pss, x1T, sub)
                self.stor(self.x1T, self.x1T.ap[:, tc_].rearrange("(c p) t -> p c t", p=128), x1T, x1T[:])
            prea[0] = loads(0)
            stageA(0)
            for t in range(NT):
                if t + 1 < NT:
                    prea[t + 1] = loads(t + 1)
                stageB(t)
                if t + 1 < NT:
                    stageA(t + 1)
                stageT(t)
            self.S.barrier()

    def phaseC1b(self, l):
        self.issue_bg('C1b%d' % l)
        BIG = 1.0e4
        with contextlib.ExitStack() as st:
            c = self.consts(st)
            Wq = self.load_w(st, 'wxq', self.w_xq[l], 8, 1024, key=('w_xq', l), defer=True)
            Wo = self.load_w(st, 'wxo', self.w_xo[l], 8, 1024, key=('w_xo', l), defer=True)
            KmT = self.sb(st, 'KmT', [128, 8, 256], BF16)
            Vm = self.sb(st, 'Vm', [128, 2, 1024], BF16)
            pss = self.ring(st, 'cbps', 8, [128, 512], F32, psum=True)
            with contextlib.ExitStack() as st2:
                Wk = self.load_w(st2, 'wxk', self.w_xk[l], 8, 1024, key=('w_xk', l))
                Wv = self.load_w(st2, 'wxv', self.w_xv[l], 8, 1024, key=('w_xv', l))
                memf = self.sb(st2, 'memf', [128, 2, D], F32)
                self.ld(memf, memf[:], self.mem.rearrange("(s p) d -> p s d", p=128))
                while self._deferred_loads:
                    self._deferred_loads.pop(0)()
                memT = self.sb(st2, 'memT', [128, 8, 256], BF16)
                for ks in range(2):
                    for half in range(2):
                        p = pss.next()
                        for k in range(4):
                            cc = half * 4 + k
                            self.S.op('pe', lambda e, p=p, k=k, cc=cc, ks=ks: e.transpose(
                                p[:, k * 128:(k + 1) * 128], memf[:, ks, cc * 128:(cc + 1) * 128], c['ident_f'][:]),
                                reads=[memf, c['ident_f']], writes=[p])
                        self.act(memT, memT[:, half * 4:half * 4 + 4, ks * 128:(ks + 1) * 128], p,
                                 p[:].rearrange("p (k t) -> p k t", k=4), AF.Copy)
                for cc in range(8):
                    p = pss.next()
                    for k in range(8):
                        self.mm(p, p[:, 0:256], Wk, Wk[:, k, cc * 128:(cc + 1) * 128], memT, memT[:, k, :], k == 0, k == 7)
                    self.act(KmT, KmT[:, cc, :], p, p[:, 0:256], AF.Copy)
                for ks in range(2):
                    for half in range(2):
                        p = pss.next()
                        for k in range(8):
                            self.mm(p, p[:], memT, memT[:, k, ks * 128:(ks + 1) * 128], Wv,
                                    Wv[:, k, half * 512:(half + 1) * 512], k == 0, k == 7)
                        self.act(Vm, Vm[:, ks, half * 512:(half + 1) * 512], p, p[:], AF.Copy)
                self.S.barrier()
            lg_, lb_, sm = self.ln_tiles(st, l, 1)
            wr = self.sb(st, 'wr', [128, 8, 20], F32)
            self.ld(wr, wr[:], self.w_r[l].rearrange("(c p) n -> p c n", p=128))
            br = self.sb(st, 'br', [128, 20], F32)
            self.ld(br, br[:], self.b_r[l, 0, :].partition_broadcast(128))
            x1Tt = self.ring(st, 'cbx1T', 2, [128, 8, TS], BF16)
            x1s = self.ring(st, 'cbx1', 2, [128, 4, D], F32)
            qxT = self.sb(st, 'cbqx', [128, 8, TS], BF16)
            PT = self.ring(st, 'cbPT', 4, [128, TS], BF16)
            rden = self.ring(st, 'cbrd', 2, [128, TS], F32)
            oxT = self.sb(st, 'cbox', [128, 8, TS], BF16)
            rr = self.ring(st, 'cbr', 2, [128, D], F32)
            x2o = self.ring(st, 'cbx2', 2, [128, D], F32)
            x2Tt = self.ring(st, 'cbx2T', 2, [128, 8, TS], BF16)
            x2Tf = self.ring(st, 'cbx2Tf', 2, [128, 8, 128], F32)
            rt = self.ring(st, 'cbrt', 2, [128, 128], F32)
            gTt = self.ring(st, 'cbgT', 2, [16, TS], F32)

            def loads(t):
                tc_ = slice(t * TS, (t + 1) * TS)
                xt, x1 = x1Tt.next(), x1s.next()
                self.ld(xt, xt[:], self.x1T.ap[:, tc_].rearrange("(c p) t -> p c t", p=128), src_b=self.x1T)
                self.ld(x1, x1[:], self.x1.ap[tc_, :].rearrange("(s p) d -> p s d", p=128), src_b=self.x1)
                return xt, x1
            rr4 = self.ring(st, 'cbr4', 4, [128, D], F32)
            rt4 = self.ring(st, 'cbrt4', 4, [128, 128], F32)
            PT8 = self.ring(st, 'cbPT8', 4, [128, TS], BF16)
            pending = []
            tl = {}

            pre = {}

            def S1(t):
                xt, x1 = pre.pop(t)
                tl[t] = x1
                for cc in range(8):
                    p = pss.next()
                    for k in range(8):
                        self.mm(p, p[:], Wq, Wq[:, k, cc * 128:(cc + 1) * 128], xt, xt[:, k, :], k == 0, k == 7)
                    self.act(qxT, qxT[:, cc, :], p, p[:], AF.Copy)
                hst = {}

                def sc(hh):
                    Ps = []
                    for ks in range(2):
                        p = pss.next()
                        for c2 in range(2):
                            self.mm(p, p[:], KmT, KmT[:, 2 * hh + c2, ks * 128:(ks + 1) * 128], qxT,
                                    qxT[:, 2 * hh + c2, :], c2 == 0, c2 == 1)
                        P = PT8.next()
                        self.act(P, P[:], p, p[:], AF.Exp, scale=1.0 / 16.0)
                        Ps.append(P)
                    hst[hh] = Ps

                def pvx(hh):
                    Ps = hst.pop(hh)
                    pd = pss.next()
                    for ks in range(2):
                        self.mm(pd, pd[:], c['ones_b'], c['ones_b'][:], Ps[ks], Ps[ks][:], ks == 0, ks == 1)
                    rd = rden.next()
                    self.recip(rd, rd[:], pd, pd[:])
                    for c2 in range(2):
                        pn = pss.next()
                        for ks in range(2):
                            self.mm(pn, pn[:], Vm, Vm[:, ks, (2 * hh + c2) * 128:(2 * hh + c2 + 1) * 128], Ps[ks],
                                    Ps[ks][:], ks == 0, ks == 1)
                        self.tt(oxT, oxT[:, 2 * hh + c2, :], pn, pn[:], rd, rd[:], ALU.mult)
                pipeline(4, sc, pvx, depth=1)

            def S2LN(t):
                x1 = tl.pop(t)
                rs = []
                for sub in range(4):
                    r = rr4.next()
                    rs.append(r)
                    for half in range(2):
                        p = pss.next()
                        for k in range(8):
                            self.mm(p, p[:], oxT, oxT[:, k, sub * 128:(sub + 1) * 128], Wo,
                                    Wo[:, k, half * 512:(half + 1) * 512], k == 0, k == 7)
                        self.stt(r, r[:, half * 512:(half + 1) * 512], x1, x1[:, sub, half * 512:(half + 1) * 512],
                                 ALPHA, p, p[:], ALU.mult, ALU.add)
                for sub in range(4):
                    xo = rs[sub]
                    self.layer_norm(sm, xo, xo[:], lg_, lb_, xo, xo[:])
                    rows = slice(t * TS + sub * 128, t * TS + (sub + 1) * 128)
                    self.stor(self.x2, self.x2.ap[rows, :], xo, xo[:])
                return rs

            def TR(t, rs):
                tc_ = slice(t * TS, (t + 1) * TS)
                x2T = x2Tt.next()
                gT = gTt.next()
                ws = []
                for sub in range(4):
                    xo = rs[sub]
                    xf = x2Tf.next()
                    self.transpose_to_xT(c, xo, xo[:], pss, x2T, sub, f32_b=xf)
                    pl = pss.next()
                    for k in range(8):
                        self.mm(pl, pl[:, 0:20], xf, xf[:, k, :], wr, wr[:, k, :], k == 0, k == 7)
                    w = rt4.next()
                    ws.append(w)
                    self.tt(w, w[:, 0:20], pl, pl[:, 0:20], br, br[:], ALU.add)
                self.stor(self.x2T, self.x2T.ap[:, tc_].rearrange("(c p) t -> p c t", p=128), x2T, x2T[:])
                for sub in range(4):
                    w = ws[sub]
                    self.S.op('dve', lambda e, w=w: e.tensor_reduce(out=w[:, 20:21], in_=w[:, 0:4], axis=AX.X, op=ALU.max),
                              reads=[w], writes=[w])
                    self.ts(w, w[:, 24:28], w, w[:, 0:4], w[:, 20:21], None, ALU.is_equal)
                    self.ts(w, w[:, 21:22], w, w[:, 20:21], -1.0, None, ALU.mult)
                    self.act(w, w[:, 118:122], w, w[:, 0:4], AF.Exp, bias=w[:, 21:22], accum_out=w[:, 22:23])
                    self.recip(w, w[:, 23:24], w, w[:, 22:23])
                    self.ts(w, w[:, 28:32], w, w[:, 24:28], -1.0, BIG, ALU.add, ALU.mult)
                    self.tt(w, w[:, 32:48].rearrange("p (g e) -> p g e", g=4), w,
                            w[:, 4:20].rearrange("p (g e) -> p g e", g=4), w,
                            w[:, 28:32].unsqueeze(2).to_broadcast([128, 4, 4]), ALU.add)
                    self.S.op('dve', lambda e, w=w: e.tensor_reduce(out=w[:, 48:49], in_=w[:, 32:48], axis=AX.X, op=ALU.max),
                              reads=[w], writes=[w])
                    self.ts(w, w[:, 50:66], w, w[:, 32:48], w[:, 48:49], None, ALU.is_equal)
                    self.stt(w, w[:, 66:82], w, w[:, 50:66], -BIG, w, w[:, 32:48], ALU.mult, ALU.add)
                    self.S.op('dve', lambda e, w=w: e.tensor_reduce(out=w[:, 49:50], in_=w[:, 66:82], axis=AX.X, op=ALU.max),
                              reads=[w], writes=[w])
                    self.ts(w, w[:, 82:98], w, w[:, 66:82], w[:, 49:50], None, ALU.is_equal)
                    self.tt(w, w[:, 98:99], w, w[:, 49:50], w, w[:, 48:49], ALU.subtract)
                    self.act(w, w[:, 99:100], w, w[:, 98:99], AF.Exp)
                    self.ts(w, w[:, 100:101], w, w[:, 99:100], 1.0, None, ALU.add)
                    self.recip(w, w[:, 100:101], w, w[:, 100:101])
                    self.tt(w, w[:, 101:102], w, w[:, 99:100], w, w[:, 100:101], ALU.mult)
                    self.ts(w, w[:, 100:102], w, w[:, 100:102], w[:, 23:24], None, ALU.mult)
                    self.ts(w, w[:, 102:118], w, w[:, 50:66], w[:, 100:101], None, ALU.mult)
                    self.stt(w, w[:, 102:118], w, w[:, 82:98], w[:, 101:102], w, w[:, 102:118], ALU.mult, ALU.add)


                def fin(ws=ws, gT=gT, tc_=tc_):
                    for sub in range(4):
                        w = ws[sub]
                        pt = pss.next()
                        self.S.op('pe', lambda e, pt=pt, w=w: e.transpose(pt[0:16, 0:128], w[:, 102:118], c['ident_f'][:]),
                                  reads=[w, c['ident_f']], writes=[pt])
                        self.act(gT, gT[0:16, sub * 128:(sub + 1) * 128], pt, pt[0:16, 0:128], AF.Copy)
                    self.stor(self.gateT, self.gateT.ap[:, tc_], gT, gT[:])
                pending.append(fin)
            pre[0] = loads(0)
            S1(0)
            for t in range(NT):
                if t + 1 < NT:
                    pre[t + 1] = loads(t + 1)
                rs = S2LN(t)
                while pending:
                    pending.pop(0)()
                if t + 1 < NT:
                    S1(t + 1)
                TR(t, rs)
            while pending:
                pending.pop(0)()
            self.S.barrier()

    def precast(self, l):
        for e in range(16):
            for src, dst in ((self.w_eg, self.wegb), (self.w_eu, self.weub), (self.w_ed, self.wedb)):
                self.S.dma('bg', dst.ap[l, e].rearrange("a b -> (a b)").rearrange("(p n) -> p n", p=128),
                           src[l, e].rearrange("a b -> (a b)").rearrange("(p n) -> p n", p=128), writes=[dst])

    def phaseC2(self, l):
        self.issue_bg('C2%d' % l)
        if l not in self.experts_cast:
            self.precast(l)
            self.experts_cast.add(l)
        last = (l == DEPTH - 1)
        with contextlib.ExitStack() as st:
            c = self.consts(st)
            sel = self.sb(st, 'sel', [48, 2048], BF16, disjoint=True)
            self.memset(sel, sel[:], 0.0)
            self.ld(sel, sel[0:16, :], self.c_sel, q='pool')
            self.ld(sel, sel[32:48, :], self.c_sel, q='pool')
            g2s = self.ring(st, 'c2g2', 2, [48, TS], BF16)
            for b_ in g2s.bufs:
                self.memset(b_, b_[:], 0.0)
            grem = self.sb(st, 'c2grem', [16, TS], F32)
            lg_, lb_, sm = self.ln_tiles(st, l, 2)
            x2Tt = self.ring(st, 'c2xT', 2, [128, 8, TS], BF16)
            ys = self.ring(st, 'c2y', 3, [128, 4, D], F32)
            gTt = self.ring(st, 'c2gT', 2, [16, TS], F32)
            Wgs = self.ring(st, 'c2wg', 3, [128, 8, 256], BF16)
            Wus = self.ring(st, 'c2wu', 3, [128, 8, 256], BF16)
            Wds = self.ring(st, 'c2wd', 10, [128, 2, D], BF16)
            hs = self.ring(st, 'c2h', 8, [128, 2, TS], BF16)
            sgs = self.ring(st, 'c2sg', 2, [128, TS], F32)
            tms = self.ring(st, 'c2tm', 2, [128, TS], F32)
            x3Tt = self.ring(st, 'c2x3T', 2, [128, 8, TS], BF16)
            psGU = self.ring(st, 'c2gu', 4, [128, 512], F32, psum=True)
            psG = self.ring(st, 'c2G', 2, [128, 512], F32, psum=True)
            psD = self.ring(st, 'c2D', 2, [128, 512], F32, psum=True)

            def loads(t):
                tc_ = slice(t * TS, (t + 1) * TS)
                xt, y, g = x2Tt.next(), ys.next(), gTt.next()
                self.ld(xt, xt[:], self.x2T.ap[:, tc_].rearrange("(c p) t -> p c t", p=128), src_b=self.x2T)
                self.ld(y, y[:], self.x2.ap[tc_, :].rearrange("(s p) d -> p s d", p=128), src_b=self.x2)
                self.ld(g, g[:], self.gateT.ap[:, tc_], src_b=self.gateT)
                return xt, y, g

            def loadw(e):
                wg_, wu_, wd_ = Wgs.next(), Wus.next(), Wds.next()
                self.ld(wg_, wg_[:], self.wegb.ap[l, e].rearrange("(c p) n -> p c n", p=128), src_b=self.wegb)
                self.ld(wu_, wu_[:], self.weub.ap[l, e].rearrange("(c p) n -> p c n", p=128), src_b=self.weub)
                self.ld(wd_, wd_[:], self.wedb.ap[l, e].rearrange("(c p) n -> p c n", p=128), src_b=self.wedb)
                return wg_, wu_, wd_
            pending = []
            nxt = loads(0)
            wq = [loadw(0), loadw(1)]
            for t in range(NT):
                xt, y, gT = nxt
                if t + 1 < NT:
                    nxt = loads(t + 1)
                tc_ = slice(t * TS, (t + 1) * TS)
                for sub in range(4):
                    self.ts(y, y[:, sub, :], y, y[:, sub, :], ALPHA, None, ALU.mult)
                state = {}
                g2 = g2s.next()
                self.cp(g2, g2[0:16, :], gT, gT[:])
                self.tt(grem, grem[:], gT, gT[:], g2, g2[0:16, :], ALU.subtract)
                self.cp(g2, g2[32:48, :], grem, grem[:])
                gT = g2

                def gu(e, xt=xt, gT=gT, state=state, t=t):
                    if pending and e >= 1:
                        pending.pop(0)()
                    W = wq.pop(0)
                    nid = t * 16 + e + 2
                    if nid < NT * 16:
                        wq.append(loadw(nid % 16))
                    wg_, wu_, wd_ = W
                    pG = psG.next()
                    self.mm(pG, pG[:], sel, sel[0:48, e * 128:(e + 1) * 128], gT, gT[0:48, :], True, True)
                    h = hs.next()
                    for fc in range(2):
                        fs = slice(fc * 128, (fc + 1) * 128)
                        pg, pu = psGU.next(), psGU.next()
                        for k in range(8):
                            self.mm(pg, pg[:], wg_, wg_[:, k, fs], xt, xt[:, k, :], k == 0, k == 7)
                        for k in range(8):
                            self.mm(pu, pu[:], wu_, wu_[:, k, fs], xt, xt[:, k, :], k == 0, k == 7)
                        s_ = sgs.next()
                        self.act(s_, s_[:], pg, pg[:], AF.Silu)
                        tm = tms.next()
                        self.tt(tm, tm[:], s_, s_[:], pu, pu[:], ALU.mult)
                        self.tt(h, h[:, fc, :], tm, tm[:], pG, pG[:], ALU.mult)
                    state[e] = (h, wd_)

                GE = 4

                def gug(gi, gu=gu):
                    for e in range(gi * GE, (gi + 1) * GE):
                        gu(e)

                def down(gi, y=y, state=state):
                    items = [state.pop(e) for e in range(gi * GE, (gi + 1) * GE)]
                    for sub in range(4):
                        for half in range(2):
                            p = psD.next()
                            for idx, (h, wd_) in enumerate(items):
                                for fc in range(2):
                                    self.mm(p, p[:], h, h[:, fc, sub * 128:(sub + 1) * 128], wd_,
                                            wd_[:, fc, half * 512:(half + 1) * 512], idx == 0 and fc == 0,
                                            idx == GE - 1 and fc == 1)
                            ysl = y[:, sub, half * 512:(half + 1) * 512]
                            self.tt(y, ysl, y, ysl, p, p[:], ALU.add)
                pipeline(16 // GE, gug, down)
                for sub in range(4):
                    def lnsub(sub=sub, y=y, t=t):
                        self.layer_norm(sm, y, y[:, sub, :], lg_, lb_, y, y[:, sub, :], gb_eng='dve')
                        rows = slice(t * TS + sub * 128, t * TS + (sub + 1) * 128)
                        dstb = self.out if last else self.xres
                        self.stor(dstb, dstb.ap[rows, :], y, y[:, sub, :], q='pool')
                    pending.append(lnsub)
                if not last:
                    def trs(y=y, tc_=tc_):
                        x3T = x3Tt.next()
                        for sub in range(4):
                            self.transpose_to_xT(c, y, y[:, sub, :], psGU, x3T, sub)
                        self.stor(self.xT, self.xT.ap[:, tc_].rearrange("(c p) t -> p c t", p=128), x3T, x3T[:],
                                  q='pool')
                    pending.append(trs)
            while pending:
                pending.pop(0)()
            self.S.barrier()

    def build(self):
        self.declare()
        if self.want('0'):
            self.phase0()
        for l in range(self.layers):
            if self.want('P%d' % l):
                self.phaseP(l)
            if self.want('HA%d' % l):
                self.phaseHA(l)
            if self.want('HB%d' % l):
                self.phaseHB(l)
            if self.want('HC%d' % l):
                self.phaseHC(l)
            if self.want('C1a%d' % l):
                self.phaseC1a(l)
            if self.want('C1b%d' % l):
                self.phaseC1b(l)
            if self.want('C2%d' % l):
                self.phaseC2(l)
        self.S.barrier(final=True)


def _rope_perm():
    perm = np.arange(NCOL)
    def blk(base, nheads):
        out = []
        for m in range(nheads // 4):
            a = [base + (4 * m + jj) * 64 + i for jj in range(4) for i in range(32)]
            b = [base + (4 * m + jj) * 64 + 32 + i for jj in range(4) for i in range(32)]
            out += a + b
        return out
    perm[0:512] = blk(0, 8)
    perm[512:1024] = blk(512, 8)
    perm[3080:3848] = blk(3080, 12)
    perm[3848:4616] = blk(3848, 12)
    return perm


def _consts():
    ident = np.eye(128, dtype=np.float32)
    k = np.arange(128)[:, None]
    q = np.arange(512)[None, :]
    masks = np.concatenate([np.where(128 * a + k <= q, 0.0, NEG).astype(np.float32) for a in range(4)], axis=1)
    qq = np.arange(128)[None, :]
    mprev = np.where(k >= qq, 0.0, NEG).astype(np.float32)
    sel = np.zeros((16, 16 * 128), np.float32)
    for e in range(16):
        sel[e, e * 128:(e + 1) * 128] = 1.0
    invf = (10000.0 ** (-(np.arange(32, dtype=np.float32)) / 32.0)).astype(np.float32)
    invf = np.tile(invf, 4).reshape(128, 1)
    return dict(c_ident=ident, c_masks=np.ascontiguousarray(masks), c_mprev=mprev, c_sel=sel, c_invf=invf)


def make_in_maps(inp, cores=range(8)):
    f = lambda a: np.ascontiguousarray(np.asarray(a, dtype=np.float32))
    sh = {}
    sh["w_in"] = np.ascontiguousarray(f(inp["w_in"])[:, :, _rope_perm()])
    sh["b_forget"] = f(inp["b_forget"]).reshape(DEPTH, 8, 1)
    sh["diff_lambda"] = f(inp["diff_lambda"]).reshape(DEPTH, 1, 256)
    sh["diff_subln"] = f(inp["diff_subln"]).reshape(DEPTH, 128, 1)
    for k in ["w_branch_a", "w_branch_b", "w_branch_c", "w_gate", "w_out", "w_xq", "w_xk", "w_xv", "w_xo",
              "ln_g", "ln_b"]:
        sh[k] = f(inp[k])
    sh["b_gate"] = np.ascontiguousarray(f(inp["b_gate"]).reshape(DEPTH, 24, 128).transpose(0, 2, 1))
    wre = f(inp["w_route_expert"]).transpose(0, 2, 1, 3).reshape(DEPTH, D, 16)
    sh["w_r"] = np.ascontiguousarray(np.concatenate([f(inp["w_route_group"]), wre], axis=2))
    sh["b_r"] = np.ascontiguousarray(np.concatenate([f(inp["b_route_group"]),
                                                     f(inp["b_route_expert"]).reshape(DEPTH, 16)], axis=1)
                                     ).reshape(DEPTH, 1, 20)
    sh["w_eg"] = f(inp["w_expert_gate"]).reshape(DEPTH, 16, D, 256)
    sh["w_eu"] = f(inp["w_expert_up"]).reshape(DEPTH, 16, D, 256)
    sh["w_ed"] = f(inp["w_expert_down"]).reshape(DEPTH, 16, 256, D)
    sh.update(_consts())
    x = f(inp["x"])
    mem = f(inp["mem"])
    pos = np.ascontiguousarray(np.asarray(inp["positions"], dtype=np.int32))
    maps = []
    for b in cores:
        m = dict(sh)
        m["x"] = x[b]
        m["mem"] = mem[b]
        m["pos"] = pos[b:b + 1]
        maps.append(m)
    return maps


def build_program(debug=False, layers=DEPTH, phases=None):
    nc = bass.Bass("TRN2", target_bir_lowering=False)
    with contextlib.ExitStack() as es:
        k = K(nc, es, debug=debug, layers=layers, phases=phases)
        k.build()
    return nc, k


def kernel(**inputs):
    nc, k = build_program()
    maps = make_in_maps(inputs)
    used = set(k.dram.keys())
    maps = [{n: v for n, v in m.items() if n in used} for m in maps]
    res = run_bass_kernel_spmd(nc, maps, core_ids=list(range(8)))
    return np.stack([np.asarray(r["out"], dtype=np.float32) for r in res.results], axis=0)
```

```python
import contextlib
import math
import numpy as np
import concourse.bass as bass
import concourse.mybir as mybir
from concourse.bass_utils import run_bass_kernel_spmd

F32 = mybir.dt.float32
BF16 = mybir.dt.bfloat16
I32 = mybir.dt.int32
AF = mybir.ActivationFunctionType
ALU = mybir.AluOpType
AX = mybir.AxisListType

T = 4096
D = 1024
NT = 8
TS = 512
DEPTH = 2
NCOL = 5384
NEG = -30000.0
EPS = 1e-5
ALPHA = (2 * DEPTH) ** 0.25
KDMA = 8


class Buf:
    def __init__(self, ap, disjoint=False):
        self.ap = ap
        self.w = {}
        self.r = {}
        self.disjoint = disjoint

    def __getitem__(self, k):
        return self.ap[k]


class Sched:
    def __init__(self, nc, es):
        self.nc = nc
        self.E = {'pe': nc.tensor, 'act': nc.scalar, 'dve': nc.vector, 'pool': nc.gpsimd, 'sp': nc.sync,
                  'bg': nc.gpsimd}
        self.psem = {}
        self.pcnt = {}
        for e in ['pe', 'act', 'dve', 'pool']:
            self.psem[e] = es.enter_context(nc.semaphore('p_' + e))
            self.pcnt[e] = 0
        self.dsem = {}
        self.dcnt = {}
        self.drr = {}
        for q in ['sp', 'pool', 'bg']:
            self.dsem[q] = [es.enter_context(nc.semaphore('d_%s%d' % (q, i))) for i in range(KDMA)]
            self.dcnt[q] = [0] * KDMA
            self.drr[q] = 0
        self.seen = {}
        self.nops = 0

    def _wait(self, e, deps):
        if e == 'bg':
            e = 'pool'
        for name, (sem, val) in deps.items():
            key = (e, name)
            if self.seen.get(key, 0) >= val:
                continue
            self.E[e].wait_ge(sem, val)
            self.seen[key] = val

    def _deps(self, e, reads, writes):
        deps = {}

        def add(d):
            for name, (sem, val) in d.items():
                if val > deps.get(name, (None, 0))[1]:
                    deps[name] = (sem, val)
        for b in reads:
            add(b.w)
        for b in writes:
            add(b.r)
            if not b.disjoint:
                add(b.w)
        if e == 'pe':
            deps.pop('p_pe', None)
        return deps

    def _record(self, tok, reads, writes):
        name, sem, val = tok
        for b in reads:
            if val > b.r.get(name, (None, 0))[1]:
                b.r[name] = (sem, val)
        for b in writes:
            if b.disjoint:
                if val > b.w.get(name, (None, 0))[1]:
                    b.w[name] = (sem, val)
            else:
                b.w = {name: (sem, val)}
                b.r = {}

    def op(self, e, emit, reads=(), writes=()):
        self._wait(e, self._deps(e, reads, writes))
        inst = emit(self.E[e])
        self.pcnt[e] += 1
        inst.then_inc(self.psem[e], 1)
        self._record(('p_' + e, self.psem[e], self.pcnt[e]), reads, writes)
        self.nops += 1

    def dma(self, q, out, in_, reads=(), writes=()):
        deps = self._deps(q, reads, writes)
        i = self.drr[q]
        self.drr[q] = (i + 1) % KDMA
        sem = self.dsem[q][i]
        name = 'd_%s%d' % (q, i)
        if self.dcnt[q][i] > 0:
            deps[name] = (sem, self.dcnt[q][i])
        self._wait(q, deps)
        self.E[q].dma_start(out=out, in_=in_).then_inc(sem, 16)
        self.dcnt[q][i] += 16
        self._record((name, sem, self.dcnt[q][i]), reads, writes)
        self.nops += 1

    def barrier(self, final=False):
        allt = {}
        for e in self.psem:
            if self.pcnt[e] > 0:
                allt['p_' + e] = (self.psem[e], self.pcnt[e])
        for q in self.dsem:
            if q == 'bg' and not final:
                continue
            for i in range(KDMA):
                if self.dcnt[q][i] > 0:
                    allt['d_%s%d' % (q, i)] = (self.dsem[q][i], self.dcnt[q][i])
        for e in ['pe', 'act', 'dve', 'pool', 'sp']:
            d = dict(allt)
            if e in self.psem:
                d.pop('p_' + e, None)
            self._wait(e, d)


class Ring:
    def __init__(self, bufs):
        self.bufs = bufs
        self.i = 0

    def next(self):
        b = self.bufs[self.i]
        self.i = (self.i + 1) % len(self.bufs)
        return b


def pipeline(n, first, second, depth=1):
    for i in range(n + depth):
        if i < n:
            first(i)
        if i >= depth:
            second(i - depth)


class K:
    def __init__(self, nc, es, debug=False, layers=DEPTH, phases=None):
        self.nc = nc
        self.es = es
        self.S = Sched(nc, es)
        self.debug = debug
        self.layers = layers
        self.phases = phases
        self.dram = {}
        self.wbf = {}
        self.experts_cast = set()
        self._deferred_loads = []

    def din(self, name, shape, dt=F32):
        t = self.nc.dram_tensor(name, list(shape), dt, kind="ExternalInput").ap()
        self.dram[name] = t
        return t

    def dscr(self, name, shape, dt):
        kind = "ExternalOutput" if self.debug else "Internal"
        t = self.nc.dram_tensor(name, list(shape), dt, kind=kind).ap()
        return Buf(t, disjoint=True)

    def sb(self, st, name, shape, dt, disjoint=False):
        self.uid = getattr(self, 'uid', 0) + 1
        t = st.enter_context(self.nc.sbuf_tensor('%s_%d' % (name, self.uid), list(shape), dt))
        return Buf(t, disjoint=disjoint)

    def ps(self, st, name, shape=(128, 512), dt=F32):
        self.uid = getattr(self, 'uid', 0) + 1
        t = st.enter_context(self.nc.psum_tensor('%s_%d' % (name, self.uid), list(shape), dt))
        return Buf(t)

    def ring(self, st, name, n, shape, dt, psum=False):
        return Ring([(self.ps if psum else self.sb)(st, '%s%d' % (name, i), shape, dt) for i in range(n)])

    def mm(self, out_b, out_ap, lhsT_b, lhsT_ap, rhs_b, rhs_ap, start, stop):
        self.S.op('pe', lambda e: e.matmul(out_ap, lhsT=lhsT_ap, rhs=rhs_ap, start=start, stop=stop),
                  reads=[lhsT_b, rhs_b], writes=[out_b])

    def act(self, out_b, out_ap, in_b, in_ap, func, extra_reads=(), **kw):
        self.S.op('act', lambda e: e.activation(out=out_ap, in_=in_ap, func=func, **kw),
                  reads=[in_b] + list(extra_reads), writes=[out_b])

    def tt(self, out_b, out_ap, a_b, a_ap, b_b, b_ap, op, eng='dve'):
        self.S.op(eng, lambda e: e.tensor_tensor(out=out_ap, in0=a_ap, in1=b_ap, op=op),
                  reads=[a_b, b_b], writes=[out_b])

    def ts(self, out_b, out_ap, a_b, a_ap, s1, s2, op0, op1=None, extra_reads=(), eng='dve'):
        if op1 is None:
            f = lambda e: e.tensor_scalar(out=out_ap, in0=a_ap, scalar1=s1, scalar2=None, op0=op0)
        else:
            f = lambda e: e.tensor_scalar(out=out_ap, in0=a_ap, scalar1=s1, scalar2=s2, op0=op0, op1=op1)
        self.S.op(eng, f, reads=[a_b] + list(extra_reads), writes=[out_b])

    def stt(self, out_b, out_ap, a_b, a_ap, scalar, b_b, b_ap, op0, op1, extra_reads=()):
        self.S.op('dve', lambda e: e.scalar_tensor_tensor(out=out_ap, in0=a_ap, scalar=scalar, in1=b_ap,
                                                          op0=op0, op1=op1),
                  reads=[a_b, b_b] + list(extra_reads), writes=[out_b])

    def cp(self, out_b, out_ap, in_b, in_ap, eng='dve'):
        self.S.op(eng, lambda e: e.tensor_copy(out=out_ap, in_=in_ap), reads=[in_b], writes=[out_b])

    def memset(self, b, ap, val, eng='dve'):
        self.S.op(eng, lambda e: e.memset(ap, val), writes=[b])

    def recip(self, out_b, out_ap, in_b, in_ap):
        self.S.op('dve', lambda e: e.reciprocal(out=out_ap, in_=in_ap), reads=[in_b], writes=[out_b])

    def ld(self, dst_b, dst_ap, src, q='sp', src_b=None):
        self.S.dma(q, dst_ap, src, reads=[src_b] if src_b is not None else [], writes=[dst_b])

    def stor(self, dst_b, dst_ap, src_b, src_ap, q='sp'):
        self.S.dma(q, dst_ap, src_ap, reads=[src_b], writes=[dst_b])

    def declare(self):
        L = DEPTH
        d = self.din
        self.x = d("x", [T, D])
        self.mem = d("mem", [256, D])
        self.pos = d("pos", [1, T], I32)
        self.w_in = d("w_in", [L, D, NCOL])
        self.b_forget = d("b_forget", [L, 8, 1])
        self.diff_lambda = d("diff_lambda", [L, 1, 256])
        self.diff_subln = d("diff_subln", [L, 128, 1])
        self.w_ba = d("w_branch_a", [L, 512, D])
        self.w_bb = d("w_branch_b", [L, 512, D])
        self.w_bc = d("w_branch_c", [L, 256, D])
        self.w_gate = d("w_gate", [L, D, 3072])
        self.b_gate = d("b_gate", [L, 128, 24])
        self.w_out = d("w_out", [L, D, D])
        self.w_xq = d("w_xq", [L, D, D])
        self.w_xk = d("w_xk", [L, D, D])
        self.w_xv = d("w_xv", [L, D, D])
        self.w_xo = d("w_xo", [L, D, D])
        self.w_r = d("w_r", [L, D, 20])
        self.b_r = d("b_r", [L, 1, 20])
        self.w_eg = d("w_eg", [L, 16, D, 256])
        self.w_eu = d("w_eu", [L, 16, D, 256])
        self.w_ed = d("w_ed", [L, 16, 256, D])
        self.ln_g = d("ln_g", [L, 3, D])
        self.ln_b = d("ln_b", [L, 3, D])
        self.c_ident = d("c_ident", [128, 128])
        self.c_masks = d("c_masks", [128, 4 * 512])
        self.c_mprev = d("c_mprev", [128, 128])
        self.c_sel = d("c_sel", [16, 2048])
        self.c_invf = d("c_invf", [128, 1])
        self.out = Buf(self.nc.dram_tensor("out", [T, D], F32, kind="ExternalOutput").ap(), disjoint=True)
        s = self.dscr
        self.xT = s("s_xT", [D, T], BF16)
        self.xres = s("s_xres", [T, D], F32)
        self.qaT = s("s_qaT", [512, T], BF16)
        self.kaT = s("s_kaT", [512, T], BF16)
        self.qbT = s("s_qbT", [512, T], BF16)
        self.kbT = s("s_kbT", [512, T], BF16)
        self.qbaug = s("s_qbaug", [8, 6, T], BF16)
        self.kbaug = s("s_kbaug", [8, 6, T], BF16)
        self.qcT = s("s_qcT", [768, T], BF16)
        self.kcT = s("s_kcT", [768, T], BF16)
        self.va = s("s_va", [T, 512], BF16)
        self.vb = s("s_vb", [T, 520], BF16)
        self.vc = s("s_vc", [T, 780], BF16)
        self.oaT = s("s_oaT", [512, T], BF16)
        self.obT = s("s_obT", [512, T], BF16)
        self.ocT = s("s_ocT", [256, T], BF16)
        self.x1 = s("s_x1", [T, D], F32)
        self.x1T = s("s_x1T", [D, T], BF16)
        self.x2 = s("s_x2", [T, D], F32)
        self.x2T = s("s_x2T", [D, T], BF16)
        self.gateT = s("s_gateT", [16, T], F32)
        mk = lambda n, shp: Buf(self.nc.dram_tensor(n, shp, BF16, kind="Internal").ap(), disjoint=True)
        self.wegb = mk("s_wegb", [DEPTH, 16, D, 256])
        self.weub = mk("s_weub", [DEPTH, 16, D, 256])
        self.wedb = mk("s_wedb", [DEPTH, 16, 256, D])

    def want(self, ph):
        return self.phases is None or ph in self.phases

    def consts(self, st, need_masks=False):
        c = {}
        c['ident_f'] = self.sb(st, 'ident_f', [128, 128], F32)
        c['ident_b'] = self.sb(st, 'ident_b', [128, 128], BF16)
        self.ld(c['ident_f'], c['ident_f'][:], self.c_ident)
        self.ld(c['ident_b'], c['ident_b'][:], self.c_ident, q='pool')
        c['ones_b'] = self.sb(st, 'ones_b', [128, 128], BF16)
        self.memset(c['ones_b'], c['ones_b'][:], 1.0)
        c['ones_f'] = self.sb(st, 'ones_f', [128, 128], F32)
        self.memset(c['ones_f'], c['ones_f'][:], 1.0)
        if need_masks:
            c['masks'] = self.sb(st, 'masks', [128, 4 * 512], BF16)
            self.ld(c['masks'], c['masks'][:], self.c_masks, q='pool')
            c['mprev'] = self.sb(st, 'mprev', [128, 128], BF16)
            self.ld(c['mprev'], c['mprev'][:], self.c_mprev, q='pool')
        return c

    def transpose_to_xT(self, c, x_b, x_ap, psT, xT_b, sub, f32_b=None):
        for half in range(2):
            p = psT.next()
            for k in range(4):
                cc = half * 4 + k
                self.S.op('pe', lambda e, cc=cc, k=k, p=p: e.transpose(p[:, k * 128:(k + 1) * 128],
                                                                     x_ap[:, cc * 128:(cc + 1) * 128],
                                                                     c['ident_f'][:]),
                          reads=[x_b, c['ident_f']], writes=[p])
            self.S.op('act', lambda e, p=p, half=half: e.activation(
                out=xT_b[:, half * 4:half * 4 + 4, sub * 128:(sub + 1) * 128],
                in_=p[:].rearrange("p (k t) -> p k t", k=4), func=AF.Copy), reads=[p], writes=[xT_b])
            if f32_b is not None:
                self.S.op('act', lambda e, p=p, half=half: e.activation(
                    out=f32_b[:, half * 4:half * 4 + 4, :],
                    in_=p[:].rearrange("p (k t) -> p k t", k=4), func=AF.Copy), reads=[p], writes=[f32_b])

    def layer_norm(self, st_bufs, r_b, r_ap, g_b, b_b, out_b, out_ap, gb_eng='pool'):
        stats, mv, sc = st_bufs['stats'], st_bufs['mv'], st_bufs['sc']
        for k in range(2):
            self.S.op('dve', lambda e, k=k: e.bn_stats(out=stats[:, k * 6:(k + 1) * 6],
                                                      in_=r_ap[:, k * 512:(k + 1) * 512]),
                      reads=[r_b], writes=[stats])
        self.S.op('dve', lambda e: e.bn_aggr(out=mv[:, 0:2], in_=stats[:, 0:12]), reads=[stats], writes=[mv])
        self.ts(sc, sc[:, 0:1], mv, mv[:, 1:2], EPS, None, ALU.add)
        self.act(sc, sc[:, 1:2], sc, sc[:, 0:1], AF.Ln)
        self.act(sc, sc[:, 2:3], sc, sc[:, 1:2], AF.Exp, scale=-0.5)
        self.ts(sc, sc[:, 3:4], mv, mv[:, 0:1], sc[:, 2:3], -1.0, ALU.mult, ALU.mult, extra_reads=[sc])
        self.act(out_b, out_ap, r_b, r_ap, AF.Identity, extra_reads=[sc], scale=sc[:, 2:3], bias=sc[:, 3:4])
        self.tt(out_b, out_ap, out_b, out_ap, g_b, g_b[:], ALU.mult, eng=gb_eng)
        self.tt(out_b, out_ap, out_b, out_ap, b_b, b_b[:], ALU.add, eng=gb_eng)

    def phase0(self):
        with contextlib.ExitStack() as st:
            c = self.consts(st)
            xin = self.ring(st, 'p0x', 3, [128, D], F32)
            xTt = self.ring(st, 'p0xT', 2, [128, 8, TS], BF16)
            psT = self.ring(st, 'p0ps', 4, [128, 512], F32, psum=True)
            for t in range(NT):
                xt = xTt.next()
                for sub in range(4):
                    xb = xin.next()
                    r0 = t * TS + sub * 128
                    self.ld(xb, xb[:], self.x[r0:r0 + 128, :])
                    self.transpose_to_xT(c, xb, xb[:], psT, xt, sub)
                self.stor(self.xT, self.xT.ap[:, t * TS:(t + 1) * TS].rearrange("(c p) t -> p c t", p=128),
                          xt, xt[:])
            self.S.barrier()

    def phaseP(self, l):
        S = self.S
        st0 = contextlib.ExitStack()
        fbuf = self.sb(st0, 'fbuf', [8, T], F32, disjoint=True)
        with contextlib.ExitStack() as st:
            groups = [(0, 1536), (1536, 3080), (3080, 4616), (4616, 5384)]
            wg = []
            for gi, (a, b) in enumerate(groups):
                wb = self.sb(st, 'win%d' % gi, [128, 8, b - a], BF16, disjoint=True)
                wg.append(wb)
            order = [0, 2, 1, 3]
            for gi in order:
                a, b = groups[gi]
                if ('w_in', l) in self.wbf:
                    wbb_ = self.wbf[('w_in', l)]
                    for c0 in range(0, 8, 2):
                        self.ld(wg[gi], wg[gi][:, c0:c0 + 2, :],
                                wbb_.ap[c0 * 128:(c0 + 2) * 128, a:b].rearrange("(c p) n -> p c n", p=128),
                                src_b=wbb_)
                else:
                    for cc in range(8):
                        self.ld(wg[gi], wg[gi][:, cc, :], self.w_in[l, cc * 128:(cc + 1) * 128, a:b], q='pool')

            if self.want('C2%d' % l) and l not in self.experts_cast:
                self.precast(l)
                self.experts_cast.add(l)

            def wslice(col, n):
                for gi, (a, b) in enumerate(groups):
                    if a <= col and col + n <= b:
                        return wg[gi], (lambda cc, gi=gi, a=a: wg[gi][:, cc, col - a:col - a + n])
                raise ValueError(col)
            cosT = self.sb(st, 'cosT', [128, T], F32)
            sinT = self.sb(st, 'sinT', [128, T], F32)
            with contextlib.ExitStack() as st2:
                posi = self.sb(st2, 'posi', [128, T], I32)
                ang = self.sb(st2, 'ang', [128, T], F32)
                u = self.sb(st2, 'u', [128, T], F32)
                ki = self.sb(st2, 'ki', [128, T], I32)
                invf = self.sb(st2, 'invf', [128, 1], F32)
                self.ld(invf, invf[:], self.c_invf)
                self.ld(posi, posi[:], self.pos[0, :].partition_broadcast(128))
                self.cp(ang, ang[:], posi, posi[:])
                self.ts(ang, ang[:], ang, ang[:], invf[:, 0:1], None, ALU.mult, extra_reads=[invf])
                for tab, off in ((sinT, 0.0), (cosT, 0.25)):
                    self.ts(u, u[:], ang, ang[:], 1.0 / (2 * math.pi), off, ALU.mult, ALU.add)
                    self.cp(ki, ki[:], u, u[:])
                    self.cp(tab, tab[:], ki, ki[:])
                    self.tt(u, u[:], u, u[:], tab, tab[:], ALU.subtract)
                    self.ts(tab, tab[:], u, u[:], 0.5, None, ALU.is_gt)
                    self.tt(u, u[:], u, u[:], tab, tab[:], ALU.subtract)
                    self.ts(tab, tab[:], u, u[:], -0.5, None, ALU.is_lt)
                    self.tt(u, u[:], u, u[:], tab, tab[:], ALU.add)
                    self.act(tab, tab[:], u, u[:], AF.Sin, scale=2 * math.pi)
                S.barrier()
            bf = self.sb(st, 'bfg', [8, 1], F32)
            self.ld(bf, bf[:], self.b_forget[l])
            self.ts(bf, bf[:], bf, bf[:], -1.0, None, ALU.mult)
            xTt = self.ring(st, 'pxT', 2, [128, 8, TS], BF16)
            pss = self.ring(st, 'pps', 7, [128, 512], F32, psum=True)
            stg = self.ring(st, 'pstg', 8, [128, TS], BF16)
            tmp = self.ring(st, 'ptmp', 4, [128, TS], F32)
            sva = self.ring(st, 'psva', 3, [128, 512], BF16)
            svb = self.ring(st, 'psvb', 2, [128, 8, 65], BF16)
            svc = self.ring(st, 'psvc', 2, [128, 12, 65], BF16)
            for b in svb.bufs + svc.bufs:
                self.memset(b, b[:], 1.0)

            def load_x(t):
                xt = xTt.next()
                self.ld(xt, xt[:], self.xT.ap[:, t * TS:(t + 1) * TS].rearrange("(c p) t -> p c t", p=128),
                        src_b=self.xT)
                return xt

            def proj_fm(xt, col, m=128):
                p = pss.next()
                wb, wf = wslice(col, m)
                for cc in range(8):
                    self.mm(p, p[0:m, :], wb, wf(cc), xt, xt[:, cc, :], cc == 0, cc == 7)
                return p
            nxt = load_x(0)
            for t in range(NT):
                xt = nxt
                if t + 1 < NT:
                    nxt = load_x(t + 1)
                tc_ = slice(t * TS, (t + 1) * TS)
                for (base, dst, npair) in ((0, self.qaT, 2), (512, self.kaT, 2), (3080, self.qcT, 3),
                                           (3848, self.kcT, 3)):
                    for m in range(npair):
                        pa = proj_fm(xt, base + 256 * m)
                        pb = proj_fm(xt, base + 256 * m + 128)
                        t1, t2, t3, t4 = tmp.next(), tmp.next(), tmp.next(), tmp.next()
                        self.tt(t1, t1[:], pa, pa[:], cosT, cosT[:, tc_], ALU.mult)
                        self.tt(t2, t2[:], pb, pb[:], sinT, sinT[:, tc_], ALU.mult)
                        self.tt(t3, t3[:], pb, pb[:], cosT, cosT[:, tc_], ALU.mult)
                        self.tt(t4, t4[:], pa, pa[:], sinT, sinT[:, tc_], ALU.mult)
                        o1, o2 = stg.next(), stg.next()
                        self.tt(o1, o1[:], t1, t1[:], t2, t2[:], ALU.subtract)
                        self.tt(o2, o2[:], t3, t3[:], t4, t4[:], ALU.add)
                        self.stor(dst, dst.ap[(2 * m) * 128:(2 * m + 1) * 128, tc_], o1, o1[:])
                        self.stor(dst, dst.ap[(2 * m + 1) * 128:(2 * m + 2) * 128, tc_], o2, o2[:])
                for (base, dst) in ((1536, self.qbT), (2048, self.kbT)):
                    for m in range(4):
                        p = proj_fm(xt, base + 128 * m)
                        o = stg.next()
                        self.act(o, o[:], p, p[:], AF.Copy)
                        self.stor(dst, dst.ap[m * 128:(m + 1) * 128, tc_], o, o[:])
                p = proj_fm(xt, 3072, 8)
                self.act(fbuf, fbuf[:, tc_], p, p[0:8, :], AF.Exp, extra_reads=[bf], scale=-1.0, bias=bf[:, 0:1])
                for sub in range(4):
                    rows = slice(t * TS + sub * 128, t * TS + (sub + 1) * 128)
                    xs = slice(sub * 128, (sub + 1) * 128)

                    def proj_tm(col, n):
                        p = pss.next()
                        wb, wf = wslice(col, n)
                        for cc in range(8):
                            self.mm(p, p[:, 0:n], xt, xt[:, cc, xs], wb, wf(cc), cc == 0, cc == 7)
                        return p
                    p = proj_tm(1024, 512)
                    o = sva.next()
                    self.act(o, o[:], p, p[:], AF.Copy)
                    self.stor(self.va, self.va.ap[rows, :], o, o[:])
                    p = proj_tm(2560, 512)
                    o = svb.next()
                    self.act(o, o[:, :, 0:64], p, p[:].rearrange("p (h e) -> p h e", h=8), AF.Copy)
                    self.stor(self.vb, self.vb.ap[rows, :], o, o[:].rearrange("p h e -> p (h e)"))
                    p = proj_tm(4616, 512)
                    p2 = proj_tm(4616 + 512, 256)
                    o = svc.next()
                    self.act(o, o[:, 0:8, 0:64], p, p[:].rearrange("p (h e) -> p h e", h=8), AF.Copy)
                    self.act(o, o[:, 8:12, 0:64], p2, p2[:, 0:256].rearrange("p (h e) -> p h e", h=4), AF.Copy)
                    self.stor(self.vc, self.vc.ap[rows, :], o, o[:].rearrange("p h e -> p (h e)"))
            S.barrier()
        with st0 as st:
            lg = fbuf
            self.act(lg, lg[:], fbuf, fbuf[:], AF.Ln, bias=1.0)
            onesf = self.sb(st, 'f1', [8, T], F32)
            self.memset(onesf, onesf[:], 1.0)
            cs = self.sb(st, 'fcs', [8, T], F32)
            self.S.op('dve', lambda e: e.tensor_tensor_scan(out=cs[:], data0=onesf[:], data1=lg[:], initial=0.0,
                                                            op0=ALU.mult, op1=ALU.add),
                      reads=[onesf, lg], writes=[cs])
            self.ts(cs, cs[:], cs, cs[:], 8.0, None, ALU.mult)
            parts = []
            rem = cs
            for i in range(3):
                pb_ = self.sb(st, 'fp%d' % i, [8, T], BF16)
                self.cp(pb_, pb_[:], rem, rem[:])
                parts.append(pb_)
                if i < 2:
                    nr = self.sb(st, 'fr%d' % i, [8, T], F32)
                    self.tt(nr, nr[:], rem, rem[:], pb_, pb_[:], ALU.subtract)
                    rem = nr
            onesb = self.sb(st, 'f1b', [8, T], BF16)
            self.memset(onesb, onesb[:], 1.0)
            for i in range(3):
                ng = self.sb(st, 'fn%d' % i, [8, T], BF16)
                self.ts(ng, ng[:], parts[i], parts[i][:], -1.0, None, ALU.mult)
                self.stor(self.qbaug, self.qbaug.ap[:, i, :], ng, ng[:])
                self.stor(self.qbaug, self.qbaug.ap[:, 3 + i, :], onesb, onesb[:])
                self.stor(self.kbaug, self.kbaug.ap[:, i, :], onesb, onesb[:])
                self.stor(self.kbaug, self.kbaug.ap[:, 3 + i, :], parts[i], parts[i][:])
            S.barrier()

    def load_rope_head(self, dst, rows0, src, j):
        m, jj = j // 4, j % 4
        self.ld(dst, dst[rows0:rows0 + 32, :], src.ap[(2 * m) * 128 + jj * 32:(2 * m) * 128 + jj * 32 + 32, :],
                src_b=src)
        self.ld(dst, dst[rows0 + 32:rows0 + 64, :],
                src.ap[(2 * m + 1) * 128 + jj * 32:(2 * m + 1) * 128 + jj * 32 + 32, :], src_b=src)

    def phaseHA(self, l):
        self.issue_bg('HA%d' % l)
        lam_init = 0.8 - 0.6 * math.exp(-0.3 * l)
        with contextlib.ExitStack() as st:
            c = self.consts(st, need_masks=True)
            dl = self.sb(st, 'dl', [128, 256], F32)
            self.ld(dl, dl[:], self.diff_lambda[l, 0, :].partition_broadcast(128))
            lt = self.sb(st, 'lt', [128, 8], F32)
            pr = self.sb(st, 'lpr', [128, 128], F32)
            self.tt(pr, pr[:, 0:64], dl, dl[:, 0:64], dl, dl[:, 64:128], ALU.mult)
            self.tt(pr, pr[:, 64:128], dl, dl[:, 128:192], dl, dl[:, 192:256], ALU.mult)
            self.S.op('dve', lambda e: e.tensor_reduce(out=lt[:, 0:2], in_=pr[:].rearrange("p (a b) -> p a b", a=2),
                                                      axis=AX.X, op=ALU.add), reads=[pr], writes=[lt])
            self.act(lt, lt[:, 2:4], lt, lt[:, 0:2], AF.Exp)
            self.tt(lt, lt[:, 4:5], lt, lt[:, 2:3], lt, lt[:, 3:4], ALU.subtract)
            self.ts(lt, lt[:, 5:6], lt, lt[:, 4:5], lam_init, -1.0, ALU.add, ALU.mult)
            sub = self.sb(st, 'subln', [128, 1], F32)
            self.ld(sub, sub[:], self.diff_subln[l])
            self.ts(sub, sub[:], sub, sub[:], 1.0 - lam_init, None, ALU.mult)
            onesm = self.sb(st, 'onesm', [128, 128], BF16)
            self.memset(onesm, onesm[:], 1.0 / 128.0)
            V = self.sb(st, 'haV', [128, 32, 512], BF16)
            self.ld(V, V[:], self.va.ap.rearrange("(n p) e -> p n e", p=128), src_b=self.va)
            qTs = self.ring(st, 'haq', 2, [128, T], BF16)
            kTs = self.ring(st, 'hak', 2, [128, T], BF16)
            for b in qTs.bufs + kTs.bufs:
                b.disjoint = True
            psS = self.ring(st, 'haS', 2, [128, 1024], F32, psum=True)
            acc = [self.ps(st, 'haacc%d' % i) for i in range(4)]
            Ps = self.ring(st, 'haP', 3, [128, 1024], BF16)
            fins = Ring([[self.sb(st, 'hafin%d_%d' % (a_, i), [128, 512], F32) for i in range(4)] for a_ in range(2)])
            sqbs = self.ring(st, 'sqb', 2, [128, 512], BF16)
            deferred = []
            ostg = self.ring(st, 'haost', 2, [128, 512], BF16)

            def load_head(h):
                q, k = qTs.next(), kTs.next()
                for rr in range(2):
                    self.load_rope_head(q, rr * 64, self.qaT, 2 * h + rr)
                    self.load_rope_head(k, rr * 64, self.kaT, 2 * h + rr)
                return q, k
            nxt = load_head(0)
            for h in range(4):
                q, k = nxt
                if h + 1 < 4:
                    nxt = load_head(h + 1)
                for j in range(NT):
                    nk = 4 * j + 4
                    qs = slice(j * TS, (j + 1) * TS)
                    state = {}

                    def qk(i, q=q, k=k, j=j, qs=qs, state=state):
                        if i == 3:
                            while deferred:
                                deferred.pop(0)()
                        p = psS.next()
                        diag = i >= 4 * j
                        for mp in range(2):
                            r = slice(mp * 64, mp * 64 + 64)
                            po = p[:, mp * 512:(mp + 1) * 512]
                            self.mm(p, po, k, k[r, i * 128:(i + 1) * 128], q, q[r, qs], True, not diag)
                            if diag:
                                a = i - 4 * j
                                self.mm(p, po, c['ident_b'], c['ident_b'][:], c['masks'],
                                        c['masks'][:, a * 512:(a + 1) * 512], False, True)
                        P = Ps.next()
                        self.act(P, P[:], p, p[:], AF.Exp, scale=0.125)
                        state[i] = P

                    def pv(i, h=h, state=state, nk=nk):
                        P = state.pop(i)
                        first, last = (i == 0), (i == nk - 1)
                        for mp in range(2):
                            Pm = P[:, mp * 512:(mp + 1) * 512]
                            self.mm(acc[2 * mp], acc[2 * mp][:], V, V[:, i, h * 128:(h + 1) * 128], P, Pm, first, last)
                            self.mm(acc[2 * mp + 1], acc[2 * mp + 1][:], c['ones_b'], c['ones_b'][:], P, Pm, first, last)
                    pipeline(nk, qk, pv, depth=1)
                    f0, f1, f2, f3 = fins.next()
                    sqb = sqbs.next()
                    self.act(f0, f0[:], acc[1], acc[1][:], AF.Ln)
                    self.act(f2, f2[:], acc[3], acc[3][:], AF.Ln)
                    self.cp(f1, f1[:], acc[0], acc[0][:])
                    self.cp(f3, f3[:], acc[2], acc[2][:])
                    self.act(f0, f0[:], f0, f0[:], AF.Exp, scale=-1.0)
                    self.act(f2, f2[:], f2, f2[:], AF.Exp, scale=-1.0)
                    self.tt(f1, f1[:], f1, f1[:], f0, f0[:], ALU.mult)
                    self.tt(f3, f3[:], f3, f3[:], f2, f2[:], ALU.mult)
                    self.stt(f1, f1[:], f3, f3[:], lt[:, 5:6], f1, f1[:], ALU.mult, ALU.add, extra_reads=[lt])
                    self.tt(sqb, sqb[:], f1, f1[:], f1, f1[:], ALU.mult)

                    def finb(f1=f1, f2=f2, sqb=sqb, h=h, qs=qs):
                        psMb = psS.next()
                        psM = Buf(psMb.ap[:, 0:512])
                        psM.w, psM.r = psMb.w, psMb.r
                        self.mm(psM, psM[:], onesm, onesm[:], sqb, sqb[:], True, True)
                        self.act(f2, f2[:], psM, psM[:], AF.Ln, bias=EPS)
                        psMb.w, psMb.r = psM.w, psM.r
                        self.act(f2, f2[:], f2, f2[:], AF.Exp, scale=-0.5)
                        self.tt(f1, f1[:], f1, f1[:], f2, f2[:], ALU.mult)
                        o = ostg.next()
                        self.ts(o, o[:], f1, f1[:], sub[:, 0:1], None, ALU.mult, extra_reads=[sub])
                        self.stor(self.oaT, self.oaT.ap[h * 128:(h + 1) * 128, qs], o, o[:])
                    deferred.append(finb)
            while deferred:
                deferred.pop(0)()
            self.S.barrier()

    def fin_norm_a(self, accb, fo, fr):
        self.act(fo, fo[0:65, :], accb, accb[0:65, :], AF.Copy)
        fl, fb = fr['l'], fr['b']
        self.act(fl, fl[64:65, :], fo, fo[64:65, :], AF.Ln)
        self.act(fl, fl[64:65, :], fl, fl[64:65, :], AF.Exp, scale=-1.0)
        self.cp(fb, fb[64:65, :], fl, fl[64:65, :])
        self.tt(fl, fl[64:65, :], fl, fl[64:65, :], fb, fb[64:65, :], ALU.subtract)
        self.cp(fb, fb[96:97, :], fl, fl[64:65, :])

    def fin_norm_b(self, c, fo, fr, psB, ostg, dst, dst_ap):
        fb = fr['b']
        self.mm(psB, psB[0:64, :], c['ones_b'], c['ones_b'][64:97, 0:64], fb, fb[64:97, :], True, True)
        o = ostg.next()
        self.tt(o, o[0:64, :], fo, fo[0:64, :], psB, psB[0:64, :], ALU.mult)
        self.stor(dst, dst_ap, o, o[0:64, :])

    def mk_fr(self, st, name):
        fl = self.sb(st, name + 'l', [128, 512], F32)
        fb = self.sb(st, name + 'b', [128, 512], BF16)
        self.memset(fb, fb[:], 0.0)
        return {'l': fl, 'b': fb}

    def phaseHB(self, l):
        self.issue_bg('HB%d' % l)
        with contextlib.ExitStack() as st:
            c = self.consts(st, need_masks=True)
            V = self.sb(st, 'hbV', [128, 32, 520], BF16)
            self.ld(V, V[:], self.vb.ap.rearrange("(n p) e -> p n e", p=128), src_b=self.vb)
            qTs = self.ring(st, 'hbq', 2, [70, T], BF16)
            kTs = self.ring(st, 'hbk', 2, [70, T], BF16)
            for b in qTs.bufs + kTs.bufs:
                b.disjoint = True
            psS = self.ring(st, 'hbS', 3, [128, 1024], F32, psum=True)
            accs = self.ring(st, 'hbacc', 2, [128, 512], F32, psum=True)
            Ps = self.ring(st, 'hbP', 4, [128, 1024], BF16)
            fos = self.ring(st, 'hbfo', 2, [128, 512], F32)
            frs = Ring([self.mk_fr(st, 'hbfr%d' % i) for i in range(2)])
            deferred = []
            ostg = self.ring(st, 'hbost', 2, [128, 512], BF16)

            def load_head(h):
                q, k = qTs.next(), kTs.next()
                self.ld(q, q[0:64, :], self.qbT.ap[h * 64:(h + 1) * 64, :], src_b=self.qbT)
                self.ld(k, k[0:64, :], self.kbT.ap[h * 64:(h + 1) * 64, :], src_b=self.kbT)
                self.ld(q, q[64:70, :], self.qbaug.ap[h], src_b=self.qbaug)
                self.ld(k, k[64:70, :], self.kbaug.ap[h], src_b=self.kbaug)
                return q, k
            nxt = load_head(0)
            for h in range(8):
                q, k = nxt
                if h + 1 < 8:
                    nxt = load_head(h + 1)
                for j in range(NT):
                    nk = 4 * j + 4
                    qs = slice(j * TS, (j + 1) * TS)
                    state = {}
                    acc = accs.next()

                    def qk(u, q=q, k=k, j=j, qs=qs, state=state, nk=nk):
                        if u == min(3, nk // 2 - 1):
                            while deferred:
                                deferred.pop(0)()
                        p = psS.next()
                        for w in range(2):
                            i = 2 * u + w
                            po = p[:, w * 512:(w + 1) * 512]
                            diag = i >= 4 * j
                            self.mm(p, po, k, k[0:70, i * 128:(i + 1) * 128], q, q[0:70, qs], True, not diag)
                            if diag:
                                a = i - 4 * j
                                self.mm(p, po, c['ident_b'], c['ident_b'][:], c['masks'],
                                        c['masks'][:, a * 512:(a + 1) * 512], False, True)
                        P = Ps.next()
                        self.act(P, P[:], p, p[:], AF.Exp, scale=0.125)
                        state[u] = P

                    def pv(u, h=h, nk=nk, state=state, acc=acc):
                        P = state.pop(u)
                        for w in range(2):
                            i = 2 * u + w
                            self.mm(acc, acc[0:65, :], V, V[:, i, h * 65:(h + 1) * 65], P, P[:, w * 512:(w + 1) * 512],
                                    i == 0, i == nk - 1)
                    pipeline(nk // 2, qk, pv, depth=2)
                    fo, fr = fos.next(), frs.next()
                    self.fin_norm_a(acc, fo, fr)

                    def finb(fo=fo, fr=fr, h=h, qs=qs):
                        psBb = psS.next()
                        psB = Buf(psBb.ap[:, 0:512])
                        psB.w, psB.r = psBb.w, psBb.r
                        self.fin_norm_b(c, fo, fr, psB, ostg, self.obT, self.obT.ap[h * 64:(h + 1) * 64, qs])
                        psBb.w, psBb.r = psB.w, psB.r
                    deferred.append(finb)
            while deferred:
                deferred.pop(0)()
            self.S.barrier()

    def phaseHC(self, l):
        dil = (1, 4, 16)
        with contextlib.ExitStack() as st:
            c = self.consts(st, need_masks=True)
            Vg = []
            for g in range(3):
                d = dil[g]
                v = self.sb(st, 'hcV%d' % g, [128, 32, 260], BF16, disjoint=True)
                src = self.vc.ap[:, g * 260:(g + 1) * 260].rearrange("(b kj cl) e -> kj cl b e", kj=128, cl=d)
                for cl in range(d):
                    nb = 32 // d
                    self.ld(v, v[:, cl * nb:(cl + 1) * nb, :], src[:, cl], src_b=self.vc)
                Vg.append(v)
            qTs = self.ring(st, 'hcq', 2, [64, T], BF16)
            kTs = self.ring(st, 'hck', 2, [64, T], BF16)
            for b in qTs.bufs + kTs.bufs:
                b.disjoint = True
            psC = self.ring(st, 'hcSc', 2, [128, 512], F32, psum=True)
            psP = self.ring(st, 'hcSp', 2, [128, 512], F32, psum=True)
            psO = self.ring(st, 'hcO', 2, [128, 512], F32, psum=True)
            psB = self.ps(st, 'hcB')
            Pc = self.ring(st, 'hcPc', 2, [128, 512], BF16)
            Pp = self.ring(st, 'hcPp', 2, [128, 512], BF16)
            accs = self.ring(st, 'hcacc', 2, [65, T], F32)
            fr = self.mk_fr(st, 'hcfr')
            ostg = self.ring(st, 'hcost', 2, [128, 512], BF16)
            heads = [(s, g) for s in range(4) for g in range(3)]

            def load_head(n):
                s, g = heads[n]
                q, k = qTs.next(), kTs.next()
                self.load_rope_head(q, 0, self.qcT, g * 4 + s)
                self.load_rope_head(k, 0, self.kcT, g * 4 + s)
                return q, k
            nxt = load_head(0)
            for n, (s, g) in enumerate(heads):
                q, k = nxt
                if n + 1 < len(heads):
                    nxt = load_head(n + 1)
                if g == 0:
                    acc = accs.next()
                d = dil[g]
                nb = 32 // d
                V = Vg[g]

                def tsl(cl, b, d=d):
                    start = b * 128 * d + cl
                    return slice(start, start + 127 * d + 1, d)
                state = {}

                def qk(u, q=q, k=k, state=state, nb=nb, tsl=tsl):
                    pc, pp = psC.next(), psP.next()
                    for bb in range(4):
                        L = 4 * u + bb
                        cl, b = L // nb, L % nb
                        cs = slice(bb * 128, (bb + 1) * 128)
                        self.mm(pc, pc[:, cs], k, k[0:64, tsl(cl, b)], q, q[0:64, tsl(cl, b)], True, False)
                        self.mm(pc, pc[:, cs], c['ident_b'], c['ident_b'][:], c['masks'], c['masks'][:, 0:128],
                                False, True)
                        if b > 0:
                            self.mm(pp, pp[:, cs], k, k[0:64, tsl(cl, b - 1)], q, q[0:64, tsl(cl, b)], True, False)
                            self.mm(pp, pp[:, cs], c['ident_b'], c['ident_b'][:], c['mprev'], c['mprev'][:],
                                    False, True)
                        else:
                            self.mm(pp, pp[:, cs], c['ident_b'], c['ident_b'][:], c['masks'], c['masks'][:, 0:128],
                                    True, True)
                    a, b_ = Pc.next(), Pp.next()
                    self.act(a, a[:], pc, pc[:], AF.Exp, scale=0.125)
                    self.act(b_, b_[:], pp, pp[:], AF.Exp, scale=0.125)
                    state[u] = (a, b_)

                def pv(u, s=s, g=g, V=V, state=state, nb=nb, d=d, acc=acc, tsl=tsl):
                    a, b_ = state.pop(u)
                    po = psO.next()
                    for bb in range(4):
                        L = 4 * u + bb
                        cl, b = L // nb, L % nb
                        cs = slice(bb * 128, (bb + 1) * 128)
                        self.mm(po, po[0:65, cs], V, V[:, L, s * 65:(s + 1) * 65], a, a[:, cs], True, b == 0)
                        if b > 0:
                            self.mm(po, po[0:65, cs], V, V[:, L - 1, s * 65:(s + 1) * 65], b_, b_[:, cs], False, True)
                    if d == 16:
                        runs = [(0, 256, (4 * u) // nb), (256, 512, (4 * u) // nb + 1)]
                    else:
                        runs = [(0, 512, (4 * u) // nb)]
                    for (c0, c1, cl) in runs:
                        b0 = (4 * u + c0 // 128) % nb
                        start = b0 * 128 * d + cl
                        cnt = c1 - c0
                        sl = slice(start, start + (cnt - 1) * d + 1, d)
                        if g == 0:
                            self.cp(acc, acc[0:65, sl], po, po[0:65, c0:c1])
                        else:
                            self.tt(acc, acc[0:65, sl], acc, acc[0:65, sl], po, po[0:65, c0:c1], ALU.add)
                pipeline(8, qk, pv)
                if g == 2:
                    for j in range(NT):
                        qs = slice(j * TS, (j + 1) * TS)
                        fl, fb = fr['l'], fr['b']
                        self.act(fl, fl[64:65, :], acc, acc[64:65, qs], AF.Ln)
                        self.act(fl, fl[64:65, :], fl, fl[64:65, :], AF.Exp, scale=-1.0)
                        self.cp(fb, fb[64:65, :], fl, fl[64:65, :])
                        self.tt(fl, fl[64:65, :], fl, fl[64:65, :], fb, fb[64:65, :], ALU.subtract)
                        self.cp(fb, fb[96:97, :], fl, fl[64:65, :])
                        self.mm(psB, psB[0:64, :], c['ones_b'], c['ones_b'][64:97, 0:64], fb, fb[64:97, :], True, True)
                        o = ostg.next()
                        self.tt(o, o[0:64, :], acc, acc[0:64, qs], psB, psB[0:64, :], ALU.mult)
                        self.stor(self.ocT, self.ocT.ap[s * 64:(s + 1) * 64, qs], o, o[0:64, :])
            self.S.barrier()

    def precast_w(self, key, src):
        R, C = src.shape
        sp = 1
        while C // sp > 2048:
            sp *= 2
        dst = Buf(self.nc.dram_tensor("wb_%s_%d" % key, [R, C], BF16, kind="Internal").ap(), disjoint=True)
        sv = src.rearrange("r (s c) -> (r s) c", s=sp)
        dv = dst.ap.rearrange("r (s c) -> (r s) c", s=sp)
        R2 = R * sp
        for r0 in range(0, R2, 1024):
            r1 = min(R2, r0 + 1024)
            self.S.dma('bg', dv[r0:r1].rearrange("(p k) c -> p k c", p=128),
                       sv[r0:r1].rearrange("(p k) c -> p k c", p=128), writes=[dst])
        self.wbf[key] = dst

    def issue_bg(self, phase):
        srcs = {'w_gate': self.w_gate, 'w_ba': self.w_ba, 'w_bb': self.w_bb, 'w_bc': self.w_bc,
                'w_out': self.w_out, 'w_xq': self.w_xq, 'w_xo': self.w_xo, 'w_xk': self.w_xk,
                'w_xv': self.w_xv, 'w_in': self.w_in}
        c1a = ['w_gate', 'w_ba', 'w_bb', 'w_bc', 'w_out']
        c1b = ['w_xq', 'w_xo', 'w_xk', 'w_xv']
        sched = {'HA0': [(n, 0) for n in c1a], 'HB0': [(n, 0) for n in c1b] + [('experts', 0)],
                 'C1a0': [('w_in', 1)], 'C1b0': [(n, 1) for n in c1a],
                 'C20': [(n, 1) for n in c1b] + [('experts', 1)]}
        if True:
            return
        for key in sched.get(phase, []):
            nm, l = key
            if l >= self.layers:
                continue
            if nm == 'experts':
                self.precast(l)
                self.experts_cast.add(l)
            else:
                self.precast_w(key, srcs[nm][l])

    def load_w(self, st, name, src, kc, n, key=None, defer=False):
        w = self.sb(st, name, [128, kc, n], BF16, disjoint=True)
        if defer:
            self._deferred_loads.append(lambda: self._issue_w(w, src, kc, n, key))
            return w
        self._issue_w(w, src, kc, n, key)
        return w

    def _issue_w(self, w, src, kc, n, key):
        if key is not None and key in self.wbf:
            b = self.wbf[key]
            for k0 in range(0, kc, 2):
                k1 = min(kc, k0 + 2)
                self.ld(w, w[:, k0:k1, :], b.ap[k0 * 128:k1 * 128, :].rearrange("(c p) n -> p c n", p=128), src_b=b)
            return w
        step = max(1, 2048 // n)
        for k0 in range(0, kc, step):
            k1 = min(kc, k0 + step)
            if n <= 2048:
                self.ld(w, w[:, k0:k1, :], src[k0 * 128:k1 * 128, :].rearrange("(c p) n -> p c n", p=128), q='pool')
            else:
                for n0 in range(0, n, 1536):
                    n1 = min(n, n0 + 1536)
                    self.ld(w, w[:, k0, n0:n1], src[k0 * 128:(k0 + 1) * 128, n0:n1], q='pool')
        return w

    def ln_tiles(self, st, l, idx):
        g = self.sb(st, 'lng', [128, D], F32)
        b = self.sb(st, 'lnb', [128, D], F32)
        self.ld(g, g[:], self.ln_g[l, idx, :].partition_broadcast(128))
        self.ld(b, b[:], self.ln_b[l, idx, :].partition_broadcast(128))
        sm = {'stats': self.sb(st, 'lnst', [128, 12], F32), 'mv': self.sb(st, 'lnmv', [128, 2], F32),
              'sc': self.sb(st, 'lnsc', [128, 4], F32)}
        return g, b, sm

    def phaseC1a(self, l):
        self.issue_bg('C1a%d' % l)
        xsrc = self.x if l == 0 else self.xres.ap
        xsrc_b = None if l == 0 else self.xres
        with contextlib.ExitStack() as st:
            c = self.consts(st)
            Wg = self.load_w(st, 'wgate', self.w_gate[l], 8, 3072, key=('w_gate', l))
            Wa = self.load_w(st, 'wba', self.w_ba[l], 4, 1024, key=('w_ba', l))
            Wb = self.load_w(st, 'wbb', self.w_bb[l], 4, 1024, key=('w_bb', l))
            Wc = self.load_w(st, 'wbc', self.w_bc[l], 2, 1024, key=('w_bc', l))
            Wo = self.load_w(st, 'wout', self.w_out[l], 8, 1024, key=('w_out', l))
            bg = self.sb(st, 'bgate', [128, 24], F32)
            self.ld(bg, bg[:], self.b_gate[l])
            lg_, lb_, sm = self.ln_tiles(st, l, 0)
            xTt = self.ring(st, 'c1xT', 2, [128, 8, TS], BF16)
            oat = self.ring(st, 'c1oa', 2, [128, 4, TS], BF16)
            obt = self.ring(st, 'c1ob', 2, [128, 4, TS], BF16)
            oct_ = self.ring(st, 'c1oc', 2, [128, 2, TS], BF16)
            xrs = self.ring(st, 'c1xr', 1, [128, 4, D], F32)
            mT = self.sb(st, 'c1mT', [128, 8, TS], BF16)
            sg = self.ring(st, 'c1sg', 3, [128, TS], F32)
            mm_ = self.ring(st, 'c1mm', 3, [128, TS], F32)
            x1Tt = self.ring(st, 'c1x1T', 2, [128, 8, TS], BF16)
            pss = self.ring(st, 'c1ps', 8, [128, 512], F32, psum=True)

            def loads(t):
                tc_ = slice(t * TS, (t + 1) * TS)
                a, b, cc_, xt = oat.next(), obt.next(), oct_.next(), xTt.next()
                self.ld(xt, xt[:], self.xT.ap[:, tc_].rearrange("(c p) t -> p c t", p=128), src_b=self.xT)
                self.ld(a, a[:], self.oaT.ap[:, tc_].rearrange("(c p) t -> p c t", p=128), src_b=self.oaT)
                self.ld(b, b[:], self.obT.ap[:, tc_].rearrange("(c p) t -> p c t", p=128), src_b=self.obT)
                self.ld(cc_, cc_[:], self.ocT.ap[:, tc_].rearrange("(c p) t -> p c t", p=128), src_b=self.ocT)
                return a, b, cc_, xt
            rr4 = self.ring(st, 'c1r4', 4, [128, D], F32)
            tiles = {}

            prea = {}
            lnq = []

            def stageA(t):
                a, b, cc_, xt = prea.pop(t)
                for ch in range(8):
                    cs = slice(ch * 128, (ch + 1) * 128)
                    ms = []
                    for bi, (W, o, kc) in enumerate(((Wa, a, 4), (Wb, b, 4), (Wc, cc_, 2))):
                        pg = pss.next()
                        for k in range(8):
                            self.mm(pg, pg[:], Wg, Wg[:, k, bi * 1024 + ch * 128:bi * 1024 + (ch + 1) * 128],
                                    xt, xt[:, k, :], k == 0, k == 7)
                        pb = pss.next()
                        for k in range(kc):
                            self.mm(pb, pb[:], W, W[:, k, cs], o, o[:, k, :], k == 0, k == kc - 1)
                        s_ = sg.next()
                        self.act(s_, s_[:], pg, pg[:], AF.Sigmoid, extra_reads=[bg],
                                 bias=bg[:, bi * 8 + ch:bi * 8 + ch + 1])
                        m = mm_.next()
                        self.tt(m, m[:], s_, s_[:], pb, pb[:], ALU.mult)
                        ms.append(m)
                    self.tt(ms[0], ms[0][:], ms[0], ms[0][:], ms[1], ms[1][:], ALU.add)
                    self.tt(mT, mT[:, ch, :], ms[0], ms[0][:], ms[2], ms[2][:], ALU.add)
                    if ch % 2 == 1 and lnq:
                        lnq.pop(0)()

            def stageB(t):
                tc_ = slice(t * TS, (t + 1) * TS)
                xr = xrs.next()
                self.ld(xr, xr[:], xsrc[tc_, :].rearrange("(s p) d -> p s d", p=128), src_b=xsrc_b)
                rs = []
                for sub in range(4):
                    r = rr4.next()
                    rs.append(r)
                    for half in range(2):
                        p = pss.next()
                        for k in range(8):
                            self.mm(p, p[:], mT, mT[:, k, sub * 128:(sub + 1) * 128], Wo,
                                    Wo[:, k, half * 512:(half + 1) * 512], k == 0, k == 7)
                        self.stt(r, r[:, half * 512:(half + 1) * 512], xr, xr[:, sub, half * 512:(half + 1) * 512],
                                 ALPHA, p, p[:], ALU.mult, ALU.add)
                for sub in range(4):
                    def lnsub(r=rs[sub], sub=sub, t=t):
                        self.layer_norm(sm, r, r[:], lg_, lb_, r, r[:])
                        rows = slice(t * TS + sub * 128, t * TS + (sub + 1) * 128)
                        self.stor(self.x1, self.x1.ap[rows, :], r, r[:])
                    lnq.append(lnsub)
                tiles[t] = rs

            def stageT(t):
                tc_ = slice(t * TS, (t + 1) * TS)
                rs = tiles.pop(t)
                x1T = x1Tt.next()
                for sub in range(4):
                    self.transpose_to_xT(c, rs[sub], rs[sub][:], pss, x1T, sub)
                self.stor(self.x1T, self.x1T.ap[:, tc_].rearrange("(c p) t -> p c t", p=128), x1T, x1T[:])
            prea[0] = loads(0)
            stageA(0)
            for t in range(NT):
                if t + 1 < NT:
                    prea[t + 1] = loads(t + 1)
                stageB(t)
                if t + 1 < NT:
                    stageA(t + 1)
                while lnq:
                    lnq.pop(0)()
                stageT(t)
            self.S.barrier()

    def phaseC1b(self, l):
        self.issue_bg('C1b%d' % l)
        BIG = 1.0e4
        with contextlib.ExitStack() as st:
            c = self.consts(st)
            Wq = self.load_w(st, 'wxq', self.w_xq[l], 8, 1024, key=('w_xq', l), defer=True)
            Wo = self.load_w(st, 'wxo', self.w_xo[l], 8, 1024, key=('w_xo', l), defer=True)
            KmT = self.sb(st, 'KmT', [128, 8, 256], BF16)
            Vm = self.sb(st, 'Vm', [128, 2, 1024], BF16)
            pss = self.ring(st, 'cbps', 8, [128, 512], F32, psum=True)
            with contextlib.ExitStack() as st2:
                Wk = self.load_w(st2, 'wxk', self.w_xk[l], 8, 1024, key=('w_xk', l))
                Wv = self.load_w(st2, 'wxv', self.w_xv[l], 8, 1024, key=('w_xv', l))
                memf = self.sb(st2, 'memf', [128, 2, D], F32)
                self.ld(memf, memf[:], self.mem.rearrange("(s p) d -> p s d", p=128))
                while self._deferred_loads:
                    self._deferred_loads.pop(0)()
                memT = self.sb(st2, 'memT', [128, 8, 256], BF16)
                for ks in range(2):
                    for half in range(2):
                        p = pss.next()
                        for k in range(4):
                            cc = half * 4 + k
                            self.S.op('pe', lambda e, p=p, k=k, cc=cc, ks=ks: e.transpose(
                                p[:, k * 128:(k + 1) * 128], memf[:, ks, cc * 128:(cc + 1) * 128], c['ident_f'][:]),
                                reads=[memf, c['ident_f']], writes=[p])
                        self.act(memT, memT[:, half * 4:half * 4 + 4, ks * 128:(ks + 1) * 128], p,
                                 p[:].rearrange("p (k t) -> p k t", k=4), AF.Copy)
                for cc in range(8):
                    p = pss.next()
                    for k in range(8):
                        self.mm(p, p[:, 0:256], Wk, Wk[:, k, cc * 128:(cc + 1) * 128], memT, memT[:, k, :], k == 0, k == 7)
                    self.act(KmT, KmT[:, cc, :], p, p[:, 0:256], AF.Copy)
                for ks in range(2):
                    for half in range(2):
                        p = pss.next()
                        for k in range(8):
                            self.mm(p, p[:], memT, memT[:, k, ks * 128:(ks + 1) * 128], Wv,
                                    Wv[:, k, half * 512:(half + 1) * 512], k == 0, k == 7)
                        self.act(Vm, Vm[:, ks, half * 512:(half + 1) * 512], p, p[:], AF.Copy)
                self.S.barrier()
            lg_, lb_, sm = self.ln_tiles(st, l, 1)
            wr = self.sb(st, 'wr', [128, 8, 20], F32)
            self.ld(wr, wr[:], self.w_r[l].rearrange("(c p) n -> p c n", p=128))
            br = self.sb(st, 'br', [128, 20], F32)
            self.ld(br, br[:], self.b_r[l, 0, :].partition_broadcast(128))
            x1Tt = self.ring(st, 'cbx1T', 2, [128, 8, TS], BF16)
            x1s = self.ring(st, 'cbx1', 2, [128, 4, D], F32)
            qxT = self.sb(st, 'cbqx', [128, 8, TS], BF16)
            PT = self.ring(st, 'cbPT', 4, [128, TS], BF16)
            rden = self.ring(st, 'cbrd', 2, [128, TS], F32)
            oxT = self.sb(st, 'cbox', [128, 8, TS], BF16)
            rr = self.ring(st, 'cbr', 2, [128, D], F32)
            x2o = self.ring(st, 'cbx2', 2, [128, D], F32)
            x2Tt = self.ring(st, 'cbx2T', 2, [128, 8, TS], BF16)
            x2Tf = self.ring(st, 'cbx2Tf', 2, [128, 8, 128], F32)
            rt = self.ring(st, 'cbrt', 2, [128, 128], F32)
            gTt = self.ring(st, 'cbgT', 2, [16, TS], F32)

            def loads(t):
                tc_ = slice(t * TS, (t + 1) * TS)
                xt, x1 = x1Tt.next(), x1s.next()
                self.ld(xt, xt[:], self.x1T.ap[:, tc_].rearrange("(c p) t -> p c t", p=128), src_b=self.x1T)
                self.ld(x1, x1[:], self.x1.ap[tc_, :].rearrange("(s p) d -> p s d", p=128), src_b=self.x1)
                return xt, x1
            rr4 = self.ring(st, 'cbr4', 4, [128, D], F32)
            rt4 = self.ring(st, 'cbrt4', 4, [128, 128], F32)
            PT8 = self.ring(st, 'cbPT8', 4, [128, TS], BF16)
            pending = []
            tl = {}

            pre = {}
            lnq2 = []

            def S1(t):
                xt, x1 = pre.pop(t)
                tl[t] = x1
                for cc in range(8):
                    p = pss.next()
                    for k in range(8):
                        self.mm(p, p[:], Wq, Wq[:, k, cc * 128:(cc + 1) * 128], xt, xt[:, k, :], k == 0, k == 7)
                    self.act(qxT, qxT[:, cc, :], p, p[:], AF.Copy)
                    if cc % 2 == 1 and lnq2:
                        lnq2.pop(0)()
                hst = {}

                def sc(hh):
                    Ps = []
                    for ks in range(2):
                        p = pss.next()
                        for c2 in range(2):
                            self.mm(p, p[:], KmT, KmT[:, 2 * hh + c2, ks * 128:(ks + 1) * 128], qxT,
                                    qxT[:, 2 * hh + c2, :], c2 == 0, c2 == 1)
                        P = PT8.next()
                        self.act(P, P[:], p, p[:], AF.Exp, scale=1.0 / 16.0)
                        Ps.append(P)
                    hst[hh] = Ps

                def pvx(hh):
                    Ps = hst.pop(hh)
                    pd = pss.next()
                    for ks in range(2):
                        self.mm(pd, pd[:], c['ones_b'], c['ones_b'][:], Ps[ks], Ps[ks][:], ks == 0, ks == 1)
                    rd = rden.next()
                    self.recip(rd, rd[:], pd, pd[:])
                    for c2 in range(2):
                        pn = pss.next()
                        for ks in range(2):
                            self.mm(pn, pn[:], Vm, Vm[:, ks, (2 * hh + c2) * 128:(2 * hh + c2 + 1) * 128], Ps[ks],
                                    Ps[ks][:], ks == 0, ks == 1)
                        self.tt(oxT, oxT[:, 2 * hh + c2, :], pn, pn[:], rd, rd[:], ALU.mult)
                pipeline(4, sc, pvx, depth=1)

            def S2LN(t):
                x1 = tl.pop(t)
                rs = []
                for sub in range(4):
                    r = rr4.next()
                    rs.append(r)
                    for half in range(2):
                        p = pss.next()
                        for k in range(8):
                            self.mm(p, p[:], oxT, oxT[:, k, sub * 128:(sub + 1) * 128], Wo,
                                    Wo[:, k, half * 512:(half + 1) * 512], k == 0, k == 7)
                        self.stt(r, r[:, half * 512:(half + 1) * 512], x1, x1[:, sub, half * 512:(half + 1) * 512],
                                 ALPHA, p, p[:], ALU.mult, ALU.add)
                for sub in range(4):
                    def lnsub(xo=rs[sub], sub=sub, t=t):
                        self.layer_norm(sm, xo, xo[:], lg_, lb_, xo, xo[:])
                        rows = slice(t * TS + sub * 128, t * TS + (sub + 1) * 128)
                        self.stor(self.x2, self.x2.ap[rows, :], xo, xo[:])
                    lnq2.append(lnsub)
                return rs

            def TR(t, rs):
                tc_ = slice(t * TS, (t + 1) * TS)
                x2T = x2Tt.next()
                gT = gTt.next()
                ws = []
                for sub in range(4):
                    xo = rs[sub]
                    xf = x2Tf.next()
                    self.transpose_to_xT(c, xo, xo[:], pss, x2T, sub, f32_b=xf)
                    pl = pss.next()
                    for k in range(8):
                        self.mm(pl, pl[:, 0:20], xf, xf[:, k, :], wr, wr[:, k, :], k == 0, k == 7)
                    w = rt4.next()
                    ws.append(w)
                    self.tt(w, w[:, 0:20], pl, pl[:, 0:20], br, br[:], ALU.add)
                self.stor(self.x2T, self.x2T.ap[:, tc_].rearrange("(c p) t -> p c t", p=128), x2T, x2T[:])
                for sub in range(4):
                    w = ws[sub]
                    self.S.op('dve', lambda e, w=w: e.tensor_reduce(out=w[:, 20:21], in_=w[:, 0:4], axis=AX.X, op=ALU.max),
                              reads=[w], writes=[w])
                    self.ts(w, w[:, 24:28], w, w[:, 0:4], w[:, 20:21], None, ALU.is_equal)
                    self.ts(w, w[:, 21:22], w, w[:, 20:21], -1.0, None, ALU.mult)
                    self.act(w, w[:, 118:122], w, w[:, 0:4], AF.Exp, bias=w[:, 21:22], accum_out=w[:, 22:23])
                    self.recip(w, w[:, 23:24], w, w[:, 22:23])
                    self.ts(w, w[:, 28:32], w, w[:, 24:28], -1.0, BIG, ALU.add, ALU.mult)
                    self.tt(w, w[:, 32:48].rearrange("p (g e) -> p g e", g=4), w,
                            w[:, 4:20].rearrange("p (g e) -> p g e", g=4), w,
                            w[:, 28:32].unsqueeze(2).to_broadcast([128, 4, 4]), ALU.add)
                    self.S.op('dve', lambda e, w=w: e.tensor_reduce(out=w[:, 48:49], in_=w[:, 32:48], axis=AX.X, op=ALU.max),
                              reads=[w], writes=[w])
                    self.ts(w, w[:, 50:66], w, w[:, 32:48], w[:, 48:49], None, ALU.is_equal)
                    self.stt(w, w[:, 66:82], w, w[:, 50:66], -BIG, w, w[:, 32:48], ALU.mult, ALU.add)
                    self.S.op('dve', lambda e, w=w: e.tensor_reduce(out=w[:, 49:50], in_=w[:, 66:82], axis=AX.X, op=ALU.max),
                              reads=[w], writes=[w])
                    self.ts(w, w[:, 82:98], w, w[:, 66:82], w[:, 49:50], None, ALU.is_equal)
                    self.tt(w, w[:, 98:99], w, w[:, 49:50], w, w[:, 48:49], ALU.subtract)
                    self.act(w, w[:, 99:100], w, w[:, 98:99], AF.Exp)
                    self.ts(w, w[:, 100:101], w, w[:, 99:100], 1.0, None, ALU.add)
                    self.recip(w, w[:, 100:101], w, w[:, 100:101])
                    self.tt(w, w[:, 101:102], w, w[:, 99:100], w, w[:, 100:101], ALU.mult)
                    self.ts(w, w[:, 100:102], w, w[:, 100:102], w[:, 23:24], None, ALU.mult)
                    self.ts(w, w[:, 102:118], w, w[:, 50:66], w[:, 100:101], None, ALU.mult)
                    self.stt(w, w[:, 102:118], w, w[:, 82:98], w[:, 101:102], w, w[:, 102:118], ALU.mult, ALU.add)


                def fin(ws=ws, gT=gT, tc_=tc_):
                    for sub in range(4):
                        w = ws[sub]
                        pt = pss.next()
                        self.S.op('pe', lambda e, pt=pt, w=w: e.transpose(pt[0:16, 0:128], w[:, 102:118], c['ident_f'][:]),
                                  reads=[w, c['ident_f']], writes=[pt])
                        self.act(gT, gT[0:16, sub * 128:(sub + 1) * 128], pt, pt[0:16, 0:128], AF.Copy)
                    self.stor(self.gateT, self.gateT.ap[:, tc_], gT, gT[:])
                pending.append(fin)
            pre[0] = loads(0)
            S1(0)
            for t in range(NT):
                if t + 1 < NT:
                    pre[t + 1] = loads(t + 1)
                rs = S2LN(t)
                while pending:
                    pending.pop(0)()
                if t + 1 < NT:
                    S1(t + 1)
                while lnq2:
                    lnq2.pop(0)()
                TR(t, rs)
            while pending:
                pending.pop(0)()
            self.S.barrier()

    def precast(self, l):
        for e in range(16):
            for src, dst in ((self.w_eg, self.wegb), (self.w_eu, self.weub), (self.w_ed, self.wedb)):
                self.S.dma('bg', dst.ap[l, e].rearrange("a b -> (a b)").rearrange("(p n) -> p n", p=128),
                           src[l, e].rearrange("a b -> (a b)").rearrange("(p n) -> p n", p=128), writes=[dst])

    def phaseC2(self, l):
        self.issue_bg('C2%d' % l)
        if l not in self.experts_cast:
            self.precast(l)
            self.experts_cast.add(l)
        last = (l == DEPTH - 1)
        with contextlib.ExitStack() as st:
            c = self.consts(st)
            sel = self.sb(st, 'sel', [48, 2048], BF16, disjoint=True)
            self.memset(sel, sel[:], 0.0)
            self.ld(sel, sel[0:16, :], self.c_sel, q='pool')
            self.ld(sel, sel[32:48, :], self.c_sel, q='pool')
            g2s = self.ring(st, 'c2g2', 2, [48, TS], BF16)
            for b_ in g2s.bufs:
                self.memset(b_, b_[:], 0.0)
            grem = self.sb(st, 'c2grem', [16, TS], F32)
            lg_, lb_, sm = self.ln_tiles(st, l, 2)
            x2Tt = self.ring(st, 'c2xT', 2, [128, 8, TS], BF16)
            ys = self.ring(st, 'c2y', 3, [128, 4, D], F32)
            gTt = self.ring(st, 'c2gT', 2, [16, TS], F32)
            Wgs = self.ring(st, 'c2wg', 3, [128, 8, 256], BF16)
            Wus = self.ring(st, 'c2wu', 3, [128, 8, 256], BF16)
            Wds = self.ring(st, 'c2wd', 10, [128, 2, D], BF16)
            hs = self.ring(st, 'c2h', 8, [128, 2, TS], BF16)
            sgs = self.ring(st, 'c2sg', 2, [128, TS], F32)
            tms = self.ring(st, 'c2tm', 2, [128, TS], F32)
            x3Tt = self.ring(st, 'c2x3T', 2, [128, 8, TS], BF16)
            psGU = self.ring(st, 'c2gu', 4, [128, 512], F32, psum=True)
            psG = self.ring(st, 'c2G', 2, [128, 512], F32, psum=True)
            psD = self.ring(st, 'c2D', 2, [128, 512], F32, psum=True)

            def loads(t):
                tc_ = slice(t * TS, (t + 1) * TS)
                xt, y, g = x2Tt.next(), ys.next(), gTt.next()
                self.ld(xt, xt[:], self.x2T.ap[:, tc_].rearrange("(c p) t -> p c t", p=128), src_b=self.x2T)
                self.ld(y, y[:], self.x2.ap[tc_, :].rearrange("(s p) d -> p s d", p=128), src_b=self.x2)
                self.ld(g, g[:], self.gateT.ap[:, tc_], src_b=self.gateT)
                return xt, y, g

            def loadw(e):
                wg_, wu_, wd_ = Wgs.next(), Wus.next(), Wds.next()
                self.ld(wg_, wg_[:], self.wegb.ap[l, e].rearrange("(c p) n -> p c n", p=128), src_b=self.wegb)
                self.ld(wu_, wu_[:], self.weub.ap[l, e].rearrange("(c p) n -> p c n", p=128), src_b=self.weub)
                self.ld(wd_, wd_[:], self.wedb.ap[l, e].rearrange("(c p) n -> p c n", p=128), src_b=self.wedb)
                return wg_, wu_, wd_
            pending = []
            nxt = loads(0)
            wq = [loadw(0), loadw(1)]
            for t in range(NT):
                xt, y, gT = nxt
                if t + 1 < NT:
                    nxt = loads(t + 1)
                tc_ = slice(t * TS, (t + 1) * TS)
                for sub in range(4):
                    self.ts(y, y[:, sub, :], y, y[:, sub, :], ALPHA, None, ALU.mult)
                state = {}
                g2 = g2s.next()
                self.cp(g2, g2[0:16, :], gT, gT[:])
                self.tt(grem, grem[:], gT, gT[:], g2, g2[0:16, :], ALU.subtract)
                self.cp(g2, g2[32:48, :], grem, grem[:])
                gT = g2

                def gu(e, xt=xt, gT=gT, state=state, t=t):
                    if pending and e >= 1:
                        pending.pop(0)()
                    W = wq.pop(0)
                    nid = t * 16 + e + 2
                    if nid < NT * 16:
                        wq.append(loadw(nid % 16))
                    wg_, wu_, wd_ = W
                    pG = psG.next()
                    self.mm(pG, pG[:], sel, sel[0:48, e * 128:(e + 1) * 128], gT, gT[0:48, :], True, True)
                    h = hs.next()
                    for fc in range(2):
                        fs = slice(fc * 128, (fc + 1) * 128)
                        pg, pu = psGU.next(), psGU.next()
                        for k in range(8):
                            self.mm(pg, pg[:], wg_, wg_[:, k, fs], xt, xt[:, k, :], k == 0, k == 7)
                        for k in range(8):
                            self.mm(pu, pu[:], wu_, wu_[:, k, fs], xt, xt[:, k, :], k == 0, k == 7)
                        s_ = sgs.next()
                        self.act(s_, s_[:], pg, pg[:], AF.Silu)
                        tm = tms.next()
                        self.tt(tm, tm[:], s_, s_[:], pu, pu[:], ALU.mult)
                        self.tt(h, h[:, fc, :], tm, tm[:], pG, pG[:], ALU.mult)
                    state[e] = (h, wd_)

                GE = 4

                def gug(gi, gu=gu):
                    for e in range(gi * GE, (gi + 1) * GE):
                        gu(e)

                def down(gi, y=y, state=state):
                    items = [state.pop(e) for e in range(gi * GE, (gi + 1) * GE)]
                    for sub in range(4):
                        for half in range(2):
                            p = psD.next()
                            for idx, (h, wd_) in enumerate(items):
                                for fc in range(2):
                                    self.mm(p, p[:], h, h[:, fc, sub * 128:(sub + 1) * 128], wd_,
                                            wd_[:, fc, half * 512:(half + 1) * 512], idx == 0 and fc == 0,
                                            idx == GE - 1 and fc == 1)
                            ysl = y[:, sub, half * 512:(half + 1) * 512]
                            self.tt(y, ysl, y, ysl, p, p[:], ALU.add)
                pipeline(16 // GE, gug, down)
                for sub in range(4):
                    def lnsub(sub=sub, y=y, t=t):
                        self.layer_norm(sm, y, y[:, sub, :], lg_, lb_, y, y[:, sub, :], gb_eng='dve')
                        rows = slice(t * TS + sub * 128, t * TS + (sub + 1) * 128)
                        dstb = self.out if last else self.xres
                        self.stor(dstb, dstb.ap[rows, :], y, y[:, sub, :], q='pool')
                    pending.append(lnsub)
                if not last:
                    def trs(y=y, tc_=tc_):
                        x3T = x3Tt.next()
                        for sub in range(4):
                            self.transpose_to_xT(c, y, y[:, sub, :], psGU, x3T, sub)
                        self.stor(self.xT, self.xT.ap[:, tc_].rearrange("(c p) t -> p c t", p=128), x3T, x3T[:],
                                  q='pool')
                    pending.append(trs)
            while pending:
                pending.pop(0)()
            self.S.barrier()

    def build(self):
        self.declare()
        if self.want('0'):
            self.phase0()
        for l in range(self.layers):
            if self.want('P%d' % l):
                self.phaseP(l)
            if self.want('HA%d' % l):
                self.phaseHA(l)
            if self.want('HB%d' % l):
                self.phaseHB(l)
            if self.want('HC%d' % l):
                self.phaseHC(l)
            if self.want('C1a%d' % l):
                self.phaseC1a(l)
            if self.want('C1b%d' % l):
                self.phaseC1b(l)
            if self.want('C2%d' % l):
                self.phaseC2(l)
        self.S.barrier(final=True)


def _rope_perm():
    perm = np.arange(NCOL)
    def blk(base, nheads):
        out = []
        for m in range(nheads // 4):
            a = [base + (4 * m + jj) * 64 + i for jj in range(4) for i in range(32)]
            b = [base + (4 * m + jj) * 64 + 32 + i for jj in range(4) for i in range(32)]
            out += a + b
        return out
    perm[0:512] = blk(0, 8)
    perm[512:1024] = blk(512, 8)
    perm[3080:3848] = blk(3080, 12)
    perm[3848:4616] = blk(3848, 12)
    return perm


def _consts():
    ident = np.eye(128, dtype=np.float32)
    k = np.arange(128)[:, None]
    q = np.arange(512)[None, :]
    masks = np.concatenate([np.where(128 * a + k <= q, 0.0, NEG).astype(np.float32) for a in range(4)], axis=1)
    qq = np.arange(128)[None, :]
    mprev = np.where(k >= qq, 0.0, NEG).astype(np.float32)
    sel = np.zeros((16, 16 * 128), np.float32)
    for e in range(16):
        sel[e, e * 128:(e + 1) * 128] = 1.0
    invf = (10000.0 ** (-(np.arange(32, dtype=np.float32)) / 32.0)).astype(np.float32)
    invf = np.tile(invf, 4).reshape(128, 1)
    return dict(c_ident=ident, c_masks=np.ascontiguousarray(masks), c_mprev=mprev, c_sel=sel, c_invf=invf)


def make_in_maps(inp, cores=range(8)):
    f = lambda a: np.ascontiguousarray(np.asarray(a, dtype=np.float32))
    sh = {}
    sh["w_in"] = np.ascontiguousarray(f(inp["w_in"])[:, :, _rope_perm()])
    sh["b_forget"] = f(inp["b_forget"]).reshape(DEPTH, 8, 1)
    sh["diff_lambda"] = f(inp["diff_lambda"]).reshape(DEPTH, 1, 256)
    sh["diff_subln"] = f(inp["diff_subln"]).reshape(DEPTH, 128, 1)
    for k in ["w_branch_a", "w_branch_b", "w_branch_c", "w_gate", "w_out", "w_xq", "w_xk", "w_xv", "w_xo",
              "ln_g", "ln_b"]:
        sh[k] = f(inp[k])
    sh["b_gate"] = np.ascontiguousarray(f(inp["b_gate"]).reshape(DEPTH, 24, 128).transpose(0, 2, 1))
    wre = f(inp["w_route_expert"]).transpose(0, 2, 1, 3).reshape(DEPTH, D, 16)
    sh["w_r"] = np.ascontiguousarray(np.concatenate([f(inp["w_route_group"]), wre], axis=2))
    sh["b_r"] = np.ascontiguousarray(np.concatenate([f(inp["b_route_group"]),
                                                     f(inp["b_route_expert"]).reshape(DEPTH, 16)], axis=1)
                                     ).reshape(DEPTH, 1, 20)
    sh["w_eg"] = f(inp["w_expert_gate"]).reshape(DEPTH, 16, D, 256)
    sh["w_eu"] = f(inp["w_expert_up"]).reshape(DEPTH, 16, D, 256)
    sh["w_ed"] = f(inp["w_expert_down"]).reshape(DEPTH, 16, 256, D)
    sh.update(_consts())
    x = f(inp["x"])
    mem = f(inp["mem"])
    pos = np.ascontiguousarray(np.asarray(inp["positions"], dtype=np.int32))
    maps = []
    for b in cores:
        m = dict(sh)
        m["x"] = x[b]
        m["mem"] = mem[b]
        m["pos"] = pos[b:b + 1]
        maps.append(m)
    return maps


def build_program(debug=False, layers=DEPTH, phases=None):
    nc = bass.Bass("TRN2", target_bir_lowering=False)
    with contextlib.ExitStack() as es:
        k = K(nc, es, debug=debug, layers=layers, phases=phases)
        k.build()
    return nc, k


def kernel(**inputs):
    nc, k = build_program()
    maps = make_in_maps(inputs)
    used = set(k.dram.keys())
    maps = [{n: v for n, v in m.items() if n in used} for m in maps]
    res = run_bass_kernel_spmd(nc, maps, core_ids=list(range(8)))
    return np.stack([np.asarray(r["out"], dtype=np.float32) for r in res.results], axis=0)
```

```python
import contextlib
import math
import numpy as np
import concourse.bass as bass
import concourse.mybir as mybir
from concourse.bass_utils import run_bass_kernel_spmd

F32 = mybir.dt.float32
BF16 = mybir.dt.bfloat16
I32 = mybir.dt.int32
AF = mybir.ActivationFunctionType
ALU = mybir.AluOpType
AX = mybir.AxisListType

T = 4096
D = 1024
NT = 8
TS = 512
DEPTH = 2
NCOL = 5384
NEG = -30000.0
EPS = 1e-5
ALPHA = (2 * DEPTH) ** 0.25
KDMA = 8


class Buf:
    def __init__(self, ap, disjoint=False):
        self.ap = ap
        self.w = {}
        self.r = {}
        self.disjoint = disjoint

    def __getitem__(self, k):
        return self.ap[k]


class Sched:
    def __init__(self, nc, es):
        self.nc = nc
        self.E = {'pe': nc.tensor, 'act': nc.scalar, 'dve': nc.vector, 'pool': nc.gpsimd, 'sp': nc.sync,
                  'bg': nc.gpsimd}
        self.psem = {}
        self.pcnt = {}
        for e in ['pe', 'act', 'dve', 'pool']:
            self.psem[e] = es.enter_context(nc.semaphore('p_' + e))
            self.pcnt[e] = 0
        self.dsem = {}
        self.dcnt = {}
        self.drr = {}
        for q in ['sp', 'pool', 'bg']:
            self.dsem[q] = [es.enter_context(nc.semaphore('d_%s%d' % (q, i))) for i in range(KDMA)]
            self.dcnt[q] = [0] * KDMA
            self.drr[q] = 0
        self.seen = {}
        self.nops = 0

    def _wait(self, e, deps):
        if e == 'bg':
            e = 'pool'
        for name, (sem, val) in deps.items():
            key = (e, name)
            if self.seen.get(key, 0) >= val:
                continue
            self.E[e].wait_ge(sem, val)
            self.seen[key] = val

    def _deps(self, e, reads, writes):
        deps = {}

        def add(d):
            for name, (sem, val) in d.items():
                if val > deps.get(name, (None, 0))[1]:
                    deps[name] = (sem, val)
        for b in reads:
            add(b.w)
        for b in writes:
            add(b.r)
            if not b.disjoint:
                add(b.w)
        if e == 'pe':
            deps.pop('p_pe', None)
        return deps

    def _record(self, tok, reads, writes):
        name, sem, val = tok
        for b in reads:
            if val > b.r.get(name, (None, 0))[1]:
                b.r[name] = (sem, val)
        for b in writes:
            if b.disjoint:
                if val > b.w.get(name, (None, 0))[1]:
                    b.w[name] = (sem, val)
            else:
                b.w = {name: (sem, val)}
                b.r = {}

    def op(self, e, emit, reads=(), writes=()):
        self._wait(e, self._deps(e, reads, writes))
        inst = emit(self.E[e])
        self.pcnt[e] += 1
        inst.then_inc(self.psem[e], 1)
        self._record(('p_' + e, self.psem[e], self.pcnt[e]), reads, writes)
        self.nops += 1

    def dma(self, q, out, in_, reads=(), writes=()):
        deps = self._deps(q, reads, writes)
        i = self.drr[q]
        self.drr[q] = (i + 1) % KDMA
        sem = self.dsem[q][i]
        name = 'd_%s%d' % (q, i)
        if self.dcnt[q][i] > 0:
            deps[name] = (sem, self.dcnt[q][i])
        self._wait(q, deps)
        self.E[q].dma_start(out=out, in_=in_).then_inc(sem, 16)
        self.dcnt[q][i] += 16
        self._record((name, sem, self.dcnt[q][i]), reads, writes)
        self.nops += 1

    def barrier(self, final=False):
        allt = {}
        for e in self.psem:
            if self.pcnt[e] > 0:
                allt['p_' + e] = (self.psem[e], self.pcnt[e])
        for q in self.dsem:
            if q == 'bg' and not final:
                continue
            for i in range(KDMA):
                if self.dcnt[q][i] > 0:
                    allt['d_%s%d' % (q, i)] = (self.dsem[q][i], self.dcnt[q][i])
        for e in ['pe', 'act', 'dve', 'pool', 'sp']:
            d = dict(allt)
            if e in self.psem:
                d.pop('p_' + e, None)
            self._wait(e, d)


class Ring:
    def __init__(self, bufs):
        self.bufs = bufs
        self.i = 0

    def next(self):
        b = self.bufs[self.i]
        self.i = (self.i + 1) % len(self.bufs)
        return b


def pipeline(n, first, second, depth=1):
    for i in range(n + depth):
        if i < n:
            first(i)
        if i >= depth:
            second(i - depth)


class K:
    def __init__(self, nc, es, debug=False, layers=DEPTH, phases=None):
        self.nc = nc
        self.es = es
        self.S = Sched(nc, es)
        self.debug = debug
        self.layers = layers
        self.phases = phases
        self.dram = {}
        self.wbf = {}
        self.experts_cast = set()
        self._deferred_loads = []

    def din(self, name, shape, dt=F32):
        t = self.nc.dram_tensor(name, list(shape), dt, kind="ExternalInput").ap()
        self.dram[name] = t
        return t

    def dscr(self, name, shape, dt):
        kind = "ExternalOutput" if self.debug else "Internal"
        t = self.nc.dram_tensor(name, list(shape), dt, kind=kind).ap()
        return Buf(t, disjoint=True)

    def sb(self, st, name, shape, dt, disjoint=False):
        self.uid = getattr(self, 'uid', 0) + 1
        t = st.enter_context(self.nc.sbuf_tensor('%s_%d' % (name, self.uid), list(shape), dt))
        return Buf(t, disjoint=disjoint)

    def ps(self, st, name, shape=(128, 512), dt=F32):
        self.uid = getattr(self, 'uid', 0) + 1
        t = st.enter_context(self.nc.psum_tensor('%s_%d' % (name, self.uid), list(shape), dt))
        return Buf(t)

    def ring(self, st, name, n, shape, dt, psum=False):
        return Ring([(self.ps if psum else self.sb)(st, '%s%d' % (name, i), shape, dt) for i in range(n)])

    def mm(self, out_b, out_ap, lhsT_b, lhsT_ap, rhs_b, rhs_ap, start, stop):
        self.S.op('pe', lambda e: e.matmul(out_ap, lhsT=lhsT_ap, rhs=rhs_ap, start=start, stop=stop),
                  reads=[lhsT_b, rhs_b], writes=[out_b])

    def act(self, out_b, out_ap, in_b, in_ap, func, extra_reads=(), **kw):
        self.S.op('act', lambda e: e.activation(out=out_ap, in_=in_ap, func=func, **kw),
                  reads=[in_b] + list(extra_reads), writes=[out_b])

    def tt(self, out_b, out_ap, a_b, a_ap, b_b, b_ap, op, eng='dve'):
        self.S.op(eng, lambda e: e.tensor_tensor(out=out_ap, in0=a_ap, in1=b_ap, op=op),
                  reads=[a_b, b_b], writes=[out_b])

    def ts(self, out_b, out_ap, a_b, a_ap, s1, s2, op0, op1=None, extra_reads=(), eng='dve'):
        if op1 is None:
            f = lambda e: e.tensor_scalar(out=out_ap, in0=a_ap, scalar1=s1, scalar2=None, op0=op0)
        else:
            f = lambda e: e.tensor_scalar(out=out_ap, in0=a_ap, scalar1=s1, scalar2=s2, op0=op0, op1=op1)
        self.S.op(eng, f, reads=[a_b] + list(extra_reads), writes=[out_b])

    def stt(self, out_b, out_ap, a_b, a_ap, scalar, b_b, b_ap, op0, op1, extra_reads=()):
        self.S.op('dve', lambda e: e.scalar_tensor_tensor(out=out_ap, in0=a_ap, scalar=scalar, in1=b_ap,
                                                          op0=op0, op1=op1),
                  reads=[a_b, b_b] + list(extra_reads), writes=[out_b])

    def cp(self, out_b, out_ap, in_b, in_ap, eng='dve'):
        self.S.op(eng, lambda e: e.tensor_copy(out=out_ap, in_=in_ap), reads=[in_b], writes=[out_b])

    def memset(self, b, ap, val, eng='dve'):
        self.S.op(eng, lambda e: e.memset(ap, val), writes=[b])

    def recip(self, out_b, out_ap, in_b, in_ap):
        self.S.op('dve', lambda e: e.reciprocal(out=out_ap, in_=in_ap), reads=[in_b], writes=[out_b])

    def ld(self, dst_b, dst_ap, src, q='sp', src_b=None):
        self.S.dma(q, dst_ap, src, reads=[src_b] if src_b is not None else [], writes=[dst_b])

    def stor(self, dst_b, dst_ap, src_b, src_ap, q='sp'):
        self.S.dma(q, dst_ap, src_ap, reads=[src_b], writes=[dst_b])

    def declare(self):
        L = DEPTH
        d = self.din
        self.x = d("x", [T, D])
        self.mem = d("mem", [256, D])
        self.pos = d("pos", [1, T], I32)
        self.w_in = d("w_in", [L, D, NCOL])
        self.b_forget = d("b_forget", [L, 8, 1])
        self.diff_lambda = d("diff_lambda", [L, 1, 256])
        self.diff_subln = d("diff_subln", [L, 128, 1])
        self.w_ba = d("w_branch_a", [L, 512, D])
        self.w_bb = d("w_branch_b", [L, 512, D])
        self.w_bc = d("w_branch_c", [L, 256, D])
        self.w_gate = d("w_gate", [L, D, 3072])
        self.b_gate = d("b_gate", [L, 128, 24])
        self.w_out = d("w_out", [L, D, D])
        self.w_xq = d("w_xq", [L, D, D])
        self.w_xk = d("w_xk", [L, D, D])
        self.w_xv = d("w_xv", [L, D, D])
        self.w_xo = d("w_xo", [L, D, D])
        self.w_r = d("w_r", [L, D, 20])
        self.b_r = d("b_r", [L, 1, 20])
        self.w_eg = d("w_eg", [L, 16, D, 256])
        self.w_eu = d("w_eu", [L, 16, D, 256])
        self.w_ed = d("w_ed", [L, 16, 256, D])
        self.ln_g = d("ln_g", [L, 3, D])
        self.ln_b = d("ln_b", [L, 3, D])
        self.c_ident = d("c_ident", [128, 128])
        self.c_masks = d("c_masks", [128, 4 * 512])
        self.c_mprev = d("c_mprev", [128, 128])
        self.c_sel = d("c_sel", [16, 2048])
        self.c_invf = d("c_invf", [128, 1])
        self.out = Buf(self.nc.dram_tensor("out", [T, D], F32, kind="ExternalOutput").ap(), disjoint=True)
        s = self.dscr
        self.xT = s("s_xT", [D, T], BF16)
        self.xres = s("s_xres", [T, D], F32)
        self.qaT = s("s_qaT", [512, T], BF16)
        self.kaT = s("s_kaT", [512, T], BF16)
        self.qbT = s("s_qbT", [512, T], BF16)
        self.kbT = s("s_kbT", [512, T], BF16)
        self.qbaug = s("s_qbaug", [8, 6, T], BF16)
        self.kbaug = s("s_kbaug", [8, 6, T], BF16)
        self.qcT = s("s_qcT", [768, T], BF16)
        self.kcT = s("s_kcT", [768, T], BF16)
        self.va = s("s_va", [T, 512], BF16)
        self.vb = s("s_vb", [T, 520], BF16)
        self.vc = s("s_vc", [T, 780], BF16)
        self.oaT = s("s_oaT", [512, T], BF16)
        self.obT = s("s_obT", [512, T], BF16)
        self.ocT = s("s_ocT", [256, T], BF16)
        self.x1 = s("s_x1", [T, D], F32)
        self.x1T = s("s_x1T", [D, T], BF16)
        self.x2 = s("s_x2", [T, D], F32)
        self.x2T = s("s_x2T", [D, T], BF16)
        self.gateT = s("s_gateT", [16, T], F32)
        mk = lambda n, shp: Buf(self.nc.dram_tensor(n, shp, BF16, kind="Internal").ap(), disjoint=True)
        self.wegb = mk("s_wegb", [DEPTH, 16, D, 256])
        self.weub = mk("s_weub", [DEPTH, 16, D, 256])
        self.wedb = mk("s_wedb", [DEPTH, 16, 256, D])

    def want(self, ph):
        return self.phases is None or ph in self.phases

    def consts(self, st, need_masks=False):
        c = {}
        c['ident_f'] = self.sb(st, 'ident_f', [128, 128], F32)
        c['ident_b'] = self.sb(st, 'ident_b', [128, 128], BF16)
        self.ld(c['ident_f'], c['ident_f'][:], self.c_ident)
        self.ld(c['ident_b'], c['ident_b'][:], self.c_ident, q='pool')
        c['ones_b'] = self.sb(st, 'ones_b', [128, 128], BF16)
        self.memset(c['ones_b'], c['ones_b'][:], 1.0)
        c['ones_f'] = self.sb(st, 'ones_f', [128, 128], F32)
        self.memset(c['ones_f'], c['ones_f'][:], 1.0)
        if need_masks:
            c['masks'] = self.sb(st, 'masks', [128, 4 * 512], BF16)
            self.ld(c['masks'], c['masks'][:], self.c_masks, q='pool')
            c['mprev'] = self.sb(st, 'mprev', [128, 128], BF16)
            self.ld(c['mprev'], c['mprev'][:], self.c_mprev, q='pool')
        return c

    def transpose_to_xT(self, c, x_b, x_ap, psT, xT_b, sub, f32_b=None):
        for half in range(2):
            p = psT.next()
            for k in range(4):
                cc = half * 4 + k
                self.S.op('pe', lambda e, cc=cc, k=k, p=p: e.transpose(p[:, k * 128:(k + 1) * 128],
                                                                     x_ap[:, cc * 128:(cc + 1) * 128],
                                                                     c['ident_f'][:]),
                          reads=[x_b, c['ident_f']], writes=[p])
            self.S.op('act', lambda e, p=p, half=half: e.activation(
                out=xT_b[:, half * 4:half * 4 + 4, sub * 128:(sub + 1) * 128],
                in_=p[:].rearrange("p (k t) -> p k t", k=4), func=AF.Copy), reads=[p], writes=[xT_b])
            if f32_b is not None:
                self.S.op('act', lambda e, p=p, half=half: e.activation(
                    out=f32_b[:, half * 4:half * 4 + 4, :],
                    in_=p[:].rearrange("p (k t) -> p k t", k=4), func=AF.Copy), reads=[p], writes=[f32_b])

    def layer_norm(self, st_bufs, r_b, r_ap, g_b, b_b, out_b, out_ap, gb_eng='pool'):
        stats, mv, sc = st_bufs['stats'], st_bufs['mv'], st_bufs['sc']
        for k in range(2):
            self.S.op('dve', lambda e, k=k: e.bn_stats(out=stats[:, k * 6:(k + 1) * 6],
                                                      in_=r_ap[:, k * 512:(k + 1) * 512]),
                      reads=[r_b], writes=[stats])
        self.S.op('dve', lambda e: e.bn_aggr(out=mv[:, 0:2], in_=stats[:, 0:12]), reads=[stats], writes=[mv])
        self.ts(sc, sc[:, 0:1], mv, mv[:, 1:2], EPS, None, ALU.add)
        self.act(sc, sc[:, 1:2], sc, sc[:, 0:1], AF.Ln)
        self.act(sc, sc[:, 2:3], sc, sc[:, 1:2], AF.Exp, scale=-0.5)
        self.ts(sc, sc[:, 3:4], mv, mv[:, 0:1], sc[:, 2:3], -1.0, ALU.mult, ALU.mult, extra_reads=[sc])
        self.act(out_b, out_ap, r_b, r_ap, AF.Identity, extra_reads=[sc], scale=sc[:, 2:3], bias=sc[:, 3:4])
        self.tt(out_b, out_ap, out_b, out_ap, g_b, g_b[:], ALU.mult, eng=gb_eng)
        self.tt(out_b, out_ap, out_b, out_ap, b_b, b_b[:], ALU.add, eng=gb_eng)

    def phase0(self):
        with contextlib.ExitStack() as st:
            c = self.consts(st)
            xin = self.ring(st, 'p0x', 3, [128, D], F32)
            xTt = self.ring(st, 'p0xT', 2, [128, 8, TS], BF16)
            psT = self.ring(st, 'p0ps', 4, [128, 512], F32, psum=True)
            for t in range(NT):
                xt = xTt.next()
                for sub in range(4):
                    xb = xin.next()
                    r0 = t * TS + sub * 128
                    self.ld(xb, xb[:], self.x[r0:r0 + 128, :])
                    self.transpose_to_xT(c, xb, xb[:], psT, xt, sub)
                self.stor(self.xT, self.xT.ap[:, t * TS:(t + 1) * TS].rearrange("(c p) t -> p c t", p=128),
                          xt, xt[:])
            self.S.barrier()

    def phaseP(self, l):
        S = self.S
        st0 = contextlib.ExitStack()
        fbuf = self.sb(st0, 'fbuf', [8, T], F32, disjoint=True)
        with contextlib.ExitStack() as st:
            groups = [(0, 1536), (1536, 3080), (3080, 4616), (4616, 5384)]
            wg = []
            for gi, (a, b) in enumerate(groups):
                wb = self.sb(st, 'win%d' % gi, [128, 8, b - a], BF16, disjoint=True)
                wg.append(wb)
            order = [0, 2, 1, 3]
            for gi in order:
                a, b = groups[gi]
                if ('w_in', l) in self.wbf:
                    wbb_ = self.wbf[('w_in', l)]
                    for c0 in range(0, 8, 2):
                        self.ld(wg[gi], wg[gi][:, c0:c0 + 2, :],
                                wbb_.ap[c0 * 128:(c0 + 2) * 128, a:b].rearrange("(c p) n -> p c n", p=128),
                                src_b=wbb_)
                else:
                    for cc in range(8):
                        self.ld(wg[gi], wg[gi][:, cc, :], self.w_in[l, cc * 128:(cc + 1) * 128, a:b], q='pool')

            if self.want('C2%d' % l) and l not in self.experts_cast:
                self.precast(l)
                self.experts_cast.add(l)

            def wslice(col, n):
                for gi, (a, b) in enumerate(groups):
                    if a <= col and col + n <= b:
                        return wg[gi], (lambda cc, gi=gi, a=a: wg[gi][:, cc, col - a:col - a + n])
                raise ValueError(col)
            cosT = self.sb(st, 'cosT', [128, T], F32)
            sinT = self.sb(st, 'sinT', [128, T], F32)
            with contextlib.ExitStack() as st2:
                posi = self.sb(st2, 'posi', [128, T], I32)
                ang = self.sb(st2, 'ang', [128, T], F32)
                u = self.sb(st2, 'u', [128, T], F32)
                ki = self.sb(st2, 'ki', [128, T], I32)
                invf = self.sb(st2, 'invf', [128, 1], F32)
                self.ld(invf, invf[:], self.c_invf)
                self.ld(posi, posi[:], self.pos[0, :].partition_broadcast(128))
                self.cp(ang, ang[:], posi, posi[:])
                self.ts(ang, ang[:], ang, ang[:], invf[:, 0:1], None, ALU.mult, extra_reads=[invf])
                for tab, off in ((sinT, 0.0), (cosT, 0.25)):
                    self.ts(u, u[:], ang, ang[:], 1.0 / (2 * math.pi), off, ALU.mult, ALU.add)
                    self.cp(ki, ki[:], u, u[:])
                    self.cp(tab, tab[:], ki, ki[:])
                    self.tt(u, u[:], u, u[:], tab, tab[:], ALU.subtract)
                    self.ts(tab, tab[:], u, u[:], 0.5, None, ALU.is_gt)
                    self.tt(u, u[:], u, u[:], tab, tab[:], ALU.subtract)
                    self.ts(tab, tab[:], u, u[:], -0.5, None, ALU.is_lt)
                    self.tt(u, u[:], u, u[:], tab, tab[:], ALU.add)
                    self.act(tab, tab[:], u, u[:], AF.Sin, scale=2 * math.pi)
                S.barrier()
            bf = self.sb(st, 'bfg', [8, 1], F32)
            self.ld(bf, bf[:], self.b_forget[l])
            self.ts(bf, bf[:], bf, bf[:], -1.0, None, ALU.mult)
            xTt = self.ring(st, 'pxT', 2, [128, 8, TS], BF16)
            pss = self.ring(st, 'pps', 7, [128, 512], F32, psum=True)
            stg = self.ring(st, 'pstg', 8, [128, TS], BF16)
            tmp = self.ring(st, 'ptmp', 4, [128, TS], F32)
            sva = self.ring(st, 'psva', 3, [128, 512], BF16)
            svb = self.ring(st, 'psvb', 2, [128, 8, 65], BF16)
            svc = self.ring(st, 'psvc', 2, [128, 12, 65], BF16)
            for b in svb.bufs + svc.bufs:
                self.memset(b, b[:], 1.0)

            def load_x(t):
                xt = xTt.next()
                self.ld(xt, xt[:], self.xT.ap[:, t * TS:(t + 1) * TS].rearrange("(c p) t -> p c t", p=128),
                        src_b=self.xT)
                return xt

            def proj_fm(xt, col, m=128):
                p = pss.next()
                wb, wf = wslice(col, m)
                for cc in range(8):
                    self.mm(p, p[0:m, :], wb, wf(cc), xt, xt[:, cc, :], cc == 0, cc == 7)
                return p
            nxt = load_x(0)
            for t in range(NT):
                xt = nxt
                if t + 1 < NT:
                    nxt = load_x(t + 1)
                tc_ = slice(t * TS, (t + 1) * TS)
                for (base, dst, npair) in ((0, self.qaT, 2), (512, self.kaT, 2), (3080, self.qcT, 3),
                                           (3848, self.kcT, 3)):
                    for m in range(npair):
                        pa = proj_fm(xt, base + 256 * m)
                        pb = proj_fm(xt, base + 256 * m + 128)
                        t1, t2, t3, t4 = tmp.next(), tmp.next(), tmp.next(), tmp.next()
                        self.tt(t1, t1[:], pa, pa[:], cosT, cosT[:, tc_], ALU.mult)
                        self.tt(t2, t2[:], pb, pb[:], sinT, sinT[:, tc_], ALU.mult)
                        self.tt(t3, t3[:], pb, pb[:], cosT, cosT[:, tc_], ALU.mult)
                        self.tt(t4, t4[:], pa, pa[:], sinT, sinT[:, tc_], ALU.mult)
                        o1, o2 = stg.next(), stg.next()
                        self.tt(o1, o1[:], t1, t1[:], t2, t2[:], ALU.subtract)
                        self.tt(o2, o2[:], t3, t3[:], t4, t4[:], ALU.add)
                        self.stor(dst, dst.ap[(2 * m) * 128:(2 * m + 1) * 128, tc_], o1, o1[:])
                        self.stor(dst, dst.ap[(2 * m + 1) * 128:(2 * m + 2) * 128, tc_], o2, o2[:])
                for (base, dst) in ((1536, self.qbT), (2048, self.kbT)):
                    for m in range(4):
                        p = proj_fm(xt, base + 128 * m)
                        o = stg.next()
                        self.act(o, o[:], p, p[:], AF.Copy)
                        self.stor(dst, dst.ap[m * 128:(m + 1) * 128, tc_], o, o[:])
                p = proj_fm(xt, 3072, 8)
                self.act(fbuf, fbuf[:, tc_], p, p[0:8, :], AF.Exp, extra_reads=[bf], scale=-1.0, bias=bf[:, 0:1])
                for sub in range(4):
                    rows = slice(t * TS + sub * 128, t * TS + (sub + 1) * 128)
                    xs = slice(sub * 128, (sub + 1) * 128)

                    def proj_tm(col, n):
                        p = pss.next()
                        wb, wf = wslice(col, n)
                        for cc in range(8):
                            self.mm(p, p[:, 0:n], xt, xt[:, cc, xs], wb, wf(cc), cc == 0, cc == 7)
                        return p
                    p = proj_tm(1024, 512)
                    o = sva.next()
                    self.act(o, o[:], p, p[:], AF.Copy)
                    self.stor(self.va, self.va.ap[rows, :], o, o[:])
                    p = proj_tm(2560, 512)
                    o = svb.next()
                    self.act(o, o[:, :, 0:64], p, p[:].rearrange("p (h e) -> p h e", h=8), AF.Copy)
                    self.stor(self.vb, self.vb.ap[rows, :], o, o[:].rearrange("p h e -> p (h e)"))
                    p = proj_tm(4616, 512)
                    p2 = proj_tm(4616 + 512, 256)
                    o = svc.next()
                    self.act(o, o[:, 0:8, 0:64], p, p[:].rearrange("p (h e) -> p h e", h=8), AF.Copy)
                    self.act(o, o[:, 8:12, 0:64], p2, p2[:, 0:256].rearrange("p (h e) -> p h e", h=4), AF.Copy)
                    self.stor(self.vc, self.vc.ap[rows, :], o, o[:].rearrange("p h e -> p (h e)"))
            S.barrier()
        with st0 as st:
            lg = fbuf
            self.act(lg, lg[:], fbuf, fbuf[:], AF.Ln, bias=1.0)
            onesf = self.sb(st, 'f1', [8, T], F32)
            self.memset(onesf, onesf[:], 1.0)
            cs = self.sb(st, 'fcs', [8, T], F32)
            self.S.op('dve', lambda e: e.tensor_tensor_scan(out=cs[:], data0=onesf[:], data1=lg[:], initial=0.0,
                                                            op0=ALU.mult, op1=ALU.add),
                      reads=[onesf, lg], writes=[cs])
            self.ts(cs, cs[:], cs, cs[:], 8.0, None, ALU.mult)
            parts = []
            rem = cs
            for i in range(3):
                pb_ = self.sb(st, 'fp%d' % i, [8, T], BF16)
                self.cp(pb_, pb_[:], rem, rem[:])
                parts.append(pb_)
                if i < 2:
                    nr = self.sb(st, 'fr%d' % i, [8, T], F32)
                    self.tt(nr, nr[:], rem, rem[:], pb_, pb_[:], ALU.subtract)
                    rem = nr
            onesb = self.sb(st, 'f1b', [8, T], BF16)
            self.memset(onesb, onesb[:], 1.0)
            for i in range(3):
                ng = self.sb(st, 'fn%d' % i, [8, T], BF16)
                self.ts(ng, ng[:], parts[i], parts[i][:], -1.0, None, ALU.mult)
                self.stor(self.qbaug, self.qbaug.ap[:, i, :], ng, ng[:])
                self.stor(self.qbaug, self.qbaug.ap[:, 3 + i, :], onesb, onesb[:])
                self.stor(self.kbaug, self.kbaug.ap[:, i, :], onesb, onesb[:])
                self.stor(self.kbaug, self.kbaug.ap[:, 3 + i, :], parts[i], parts[i][:])
            S.barrier()

    def load_rope_head(self, dst, rows0, src, j):
        m, jj = j // 4, j % 4
        self.ld(dst, dst[rows0:rows0 + 32, :], src.ap[(2 * m) * 128 + jj * 32:(2 * m) * 128 + jj * 32 + 32, :],
                src_b=src)
        self.ld(dst, dst[rows0 + 32:rows0 + 64, :],
                src.ap[(2 * m + 1) * 128 + jj * 32:(2 * m + 1) * 128 + jj * 32 + 32, :], src_b=src)

    def phaseHA(self, l):
        self.issue_bg('HA%d' % l)
        lam_init = 0.8 - 0.6 * math.exp(-0.3 * l)
        with contextlib.ExitStack() as st:
            c = self.consts(st, need_masks=True)
            dl = self.sb(st, 'dl', [128, 256], F32)
            self.ld(dl, dl[:], self.diff_lambda[l, 0, :].partition_broadcast(128))
            lt = self.sb(st, 'lt', [128, 8], F32)
            pr = self.sb(st, 'lpr', [128, 128], F32)
            self.tt(pr, pr[:, 0:64], dl, dl[:, 0:64], dl, dl[:, 64:128], ALU.mult)
            self.tt(pr, pr[:, 64:128], dl, dl[:, 128:192], dl, dl[:, 192:256], ALU.mult)
            self.S.op('dve', lambda e: e.tensor_reduce(out=lt[:, 0:2], in_=pr[:].rearrange("p (a b) -> p a b", a=2),
                                                      axis=AX.X, op=ALU.add), reads=[pr], writes=[lt])
            self.act(lt, lt[:, 2:4], lt, lt[:, 0:2], AF.Exp)
            self.tt(lt, lt[:, 4:5], lt, lt[:, 2:3], lt, lt[:, 3:4], ALU.subtract)
            self.ts(lt, lt[:, 5:6], lt, lt[:, 4:5], lam_init, -1.0, ALU.add, ALU.mult)
            sub = self.sb(st, 'subln', [128, 1], F32)
            self.ld(sub, sub[:], self.diff_subln[l])
            self.ts(sub, sub[:], sub, sub[:], 1.0 - lam_init, None, ALU.mult)
            onesm = self.sb(st, 'onesm', [128, 128], BF16)
            self.memset(onesm, onesm[:], 1.0 / 128.0)
            V = self.sb(st, 'haV', [128, 32, 512], BF16)
            self.ld(V, V[:], self.va.ap.rearrange("(n p) e -> p n e", p=128), src_b=self.va)
            qTs = self.ring(st, 'haq', 2, [128, T], BF16)
            kTs = self.ring(st, 'hak', 2, [128, T], BF16)
            for b in qTs.bufs + kTs.bufs:
                b.disjoint = True
            psS = self.ring(st, 'haS', 2, [128, 1024], F32, psum=True)
            acc = [self.ps(st, 'haacc%d' % i) for i in range(4)]
            Ps = self.ring(st, 'haP', 3, [128, 1024], BF16)
            fins = Ring([[self.sb(st, 'hafin%d_%d' % (a_, i), [128, 512], F32) for i in range(4)] for a_ in range(2)])
            sqbs = self.ring(st, 'sqb', 2, [128, 512], BF16)
            deferred = []
            ostg = self.ring(st, 'haost', 2, [128, 512], BF16)

            def load_head(h):
                q, k = qTs.next(), kTs.next()
                for rr in range(2):
                    self.load_rope_head(q, rr * 64, self.qaT, 2 * h + rr)
                    self.load_rope_head(k, rr * 64, self.kaT, 2 * h + rr)
                return q, k
            nxt = load_head(0)
            for h in range(4):
                q, k = nxt
                if h + 1 < 4:
                    nxt = load_head(h + 1)
                for j in range(NT):
                    nk = 4 * j + 4
                    qs = slice(j * TS, (j + 1) * TS)
                    state = {}

                    def qk(i, q=q, k=k, j=j, qs=qs, state=state):
                        if i == 3:
                            while deferred:
                                deferred.pop(0)()
                        p = psS.next()
                        diag = i >= 4 * j
                        for mp in range(2):
                            r = slice(mp * 64, mp * 64 + 64)
                            po = p[:, mp * 512:(mp + 1) * 512]
                            self.mm(p, po, k, k[r, i * 128:(i + 1) * 128], q, q[r, qs], True, not diag)
                            if diag:
                                a = i - 4 * j
                                self.mm(p, po, c['ident_b'], c['ident_b'][:], c['masks'],
                                        c['masks'][:, a * 512:(a + 1) * 512], False, True)
                        P = Ps.next()
                        self.act(P, P[:], p, p[:], AF.Exp, scale=0.125)
                        state[i] = P

                    def pv(i, h=h, state=state, nk=nk):
                        P = state.pop(i)
                        first, last = (i == 0), (i == nk - 1)
                        for mp in range(2):
                            Pm = P[:, mp * 512:(mp + 1) * 512]
                            self.mm(acc[2 * mp], acc[2 * mp][:], V, V[:, i, h * 128:(h + 1) * 128], P, Pm, first, last)
                            self.mm(acc[2 * mp + 1], acc[2 * mp + 1][:], c['ones_b'], c['ones_b'][:], P, Pm, first, last)
                    pipeline(nk, qk, pv, depth=1)
                    f0, f1, f2, f3 = fins.next()
                    sqb = sqbs.next()
                    self.act(f0, f0[:], acc[1], acc[1][:], AF.Ln)
                    self.act(f2, f2[:], acc[3], acc[3][:], AF.Ln)
                    self.cp(f1, f1[:], acc[0], acc[0][:])
                    self.cp(f3, f3[:], acc[2], acc[2][:])
                    self.act(f0, f0[:], f0, f0[:], AF.Exp, scale=-1.0)
                    self.act(f2, f2[:], f2, f2[:], AF.Exp, scale=-1.0)
                    self.tt(f1, f1[:], f1, f1[:], f0, f0[:], ALU.mult)
                    self.tt(f3, f3[:], f3, f3[:], f2, f2[:], ALU.mult)
                    self.stt(f1, f1[:], f3, f3[:], lt[:, 5:6], f1, f1[:], ALU.mult, ALU.add, extra_reads=[lt])
                    self.tt(sqb, sqb[:], f1, f1[:], f1, f1[:], ALU.mult)

                    def finb(f1=f1, f2=f2, sqb=sqb, h=h, qs=qs):
                        psMb = psS.next()
                        psM = Buf(psMb.ap[:, 0:512])
                        psM.w, psM.r = psMb.w, psMb.r
                        self.mm(psM, psM[:], onesm, onesm[:], sqb, sqb[:], True, True)
                        self.act(f2, f2[:], psM, psM[:], AF.Ln, bias=EPS)
                        psMb.w, psMb.r = psM.w, psM.r
                        self.act(f2, f2[:], f2, f2[:], AF.Exp, scale=-0.5)
                        self.tt(f1, f1[:], f1, f1[:], f2, f2[:], ALU.mult)
                        o = ostg.next()
                        self.ts(o, o[:], f1, f1[:], sub[:, 0:1], None, ALU.mult, extra_reads=[sub])
                        self.stor(self.oaT, self.oaT.ap[h * 128:(h + 1) * 128, qs], o, o[:])
                    deferred.append(finb)
            while deferred:
                deferred.pop(0)()
            self.S.barrier()

    def fin_norm_a(self, accb, fo, fr):
        self.act(fo, fo[0:65, :], accb, accb[0:65, :], AF.Copy)
        fl, fb = fr['l'], fr['b']
        self.act(fl, fl[64:65, :], fo, fo[64:65, :], AF.Ln)
        self.act(fl, fl[64:65, :], fl, fl[64:65, :], AF.Exp, scale=-1.0)
        self.cp(fb, fb[64:65, :], fl, fl[64:65, :])
        self.tt(fl, fl[64:65, :], fl, fl[64:65, :], fb, fb[64:65, :], ALU.subtract)
        self.cp(fb, fb[96:97, :], fl, fl[64:65, :])

    def fin_norm_b(self, c, fo, fr, psB, ostg, dst, dst_ap):
        fb = fr['b']
        self.mm(psB, psB[0:64, :], c['ones_b'], c['ones_b'][64:97, 0:64], fb, fb[64:97, :], True, True)
        o = ostg.next()
        self.tt(o, o[0:64, :], fo, fo[0:64, :], psB, psB[0:64, :], ALU.mult)
        self.stor(dst, dst_ap, o, o[0:64, :])

    def mk_fr(self, st, name):
        fl = self.sb(st, name + 'l', [128, 512], F32)
        fb = self.sb(st, name + 'b', [128, 512], BF16)
        self.memset(fb, fb[:], 0.0)
        return {'l': fl, 'b': fb}

    def phaseHB(self, l):
        self.issue_bg('HB%d' % l)
        with contextlib.ExitStack() as st:
            c = self.consts(st, need_masks=True)
            V = self.sb(st, 'hbV', [128, 32, 520], BF16)
            self.ld(V, V[:], self.vb.ap.rearrange("(n p) e -> p n e", p=128), src_b=self.vb)
            qTs = self.ring(st, 'hbq', 2, [70, T], BF16)
            kTs = self.ring(st, 'hbk', 2, [70, T], BF16)
            for b in qTs.bufs + kTs.bufs:
                b.disjoint = True
            psS = self.ring(st, 'hbS', 3, [128, 1024], F32, psum=True)
            accs = self.ring(st, 'hbacc', 2, [128, 512], F32, psum=True)
            Ps = self.ring(st, 'hbP', 4, [128, 1024], BF16)
            fos = self.ring(st, 'hbfo', 2, [128, 512], F32)
            frs = Ring([self.mk_fr(st, 'hbfr%d' % i) for i in range(2)])
            deferred = []
            ostg = self.ring(st, 'hbost', 2, [128, 512], BF16)

            def load_head(h):
                q, k = qTs.next(), kTs.next()
                self.ld(q, q[0:64, :], self.qbT.ap[h * 64:(h + 1) * 64, :], src_b=self.qbT)
                self.ld(k, k[0:64, :], self.kbT.ap[h * 64:(h + 1) * 64, :], src_b=self.kbT)
                self.ld(q, q[64:70, :], self.qbaug.ap[h], src_b=self.qbaug)
                self.ld(k, k[64:70, :], self.kbaug.ap[h], src_b=self.kbaug)
                return q, k
            nxt = load_head(0)
            for h in range(8):
                q, k = nxt
                if h + 1 < 8:
                    nxt = load_head(h + 1)
                for j in range(NT):
                    nk = 4 * j + 4
                    qs = slice(j * TS, (j + 1) * TS)
                    state = {}
                    acc = accs.next()

                    def qk(u, q=q, k=k, j=j, qs=qs, state=state, nk=nk):
                        if u == min(3, nk // 2 - 1):
                            while deferred:
                                deferred.pop(0)()
                        p = psS.next()
                        for w in range(2):
                            i = 2 * u + w
                            po = p[:, w * 512:(w + 1) * 512]
                            diag = i >= 4 * j
                            self.mm(p, po, k, k[0:70, i * 128:(i + 1) * 128], q, q[0:70, qs], True, not diag)
                            if diag:
                                a = i - 4 * j
                                self.mm(p, po, c['ident_b'], c['ident_b'][:], c['masks'],
                                        c['masks'][:, a * 512:(a + 1) * 512], False, True)
                        P = Ps.next()
                        self.act(P, P[:], p, p[:], AF.Exp, scale=0.125)
                        state[u] = P

                    def pv(u, h=h, nk=nk, state=state, acc=acc):
                        P = state.pop(u)
                        for w in range(2):
                            i = 2 * u + w
                            self.mm(acc, acc[0:65, :], V, V[:, i, h * 65:(h + 1) * 65], P, P[:, w * 512:(w + 1) * 512],
                                    i == 0, i == nk - 1)
                    pipeline(nk // 2, qk, pv, depth=2)
                    fo, fr = fos.next(), frs.next()
                    self.fin_norm_a(acc, fo, fr)

                    def finb(fo=fo, fr=fr, h=h, qs=qs):
                        psBb = psS.next()
                        psB = Buf(psBb.ap[:, 0:512])
                        psB.w, psB.r = psBb.w, psBb.r
                        self.fin_norm_b(c, fo, fr, psB, ostg, self.obT, self.obT.ap[h * 64:(h + 1) * 64, qs])
                        psBb.w, psBb.r = psB.w, psB.r
                    deferred.append(finb)
            while deferred:
                deferred.pop(0)()
            self.S.barrier()

    def phaseHC(self, l):
        dil = (1, 4, 16)
        with contextlib.ExitStack() as st:
            c = self.consts(st, need_masks=True)
            Vg = []
            for g in range(3):
                d = dil[g]
                v = self.sb(st, 'hcV%d' % g, [128, 32, 260], BF16, disjoint=True)
                src = self.vc.ap[:, g * 260:(g + 1) * 260].rearrange("(b kj cl) e -> kj cl b e", kj=128, cl=d)
                for cl in range(d):
                    nb = 32 // d
                    self.ld(v, v[:, cl * nb:(cl + 1) * nb, :], src[:, cl], src_b=self.vc)
                Vg.append(v)
            cur4 = self.sb(st, 'hccur4', [128, 512], BF16, disjoint=True)
            prev4 = self.sb(st, 'hcprev4', [128, 512], BF16, disjoint=True)
            for bb in range(4):
                self.ld(cur4, cur4[:, bb * 128:(bb + 1) * 128], self.c_masks[:, 0:128], q='pool')
                self.ld(prev4, prev4[:, bb * 128:(bb + 1) * 128], self.c_mprev, q='pool')
            qTs = self.ring(st, 'hcq', 2, [64, T], BF16)
            kTs = self.ring(st, 'hck', 2, [64, T], BF16)
            for b in qTs.bufs + kTs.bufs:
                b.disjoint = True
            psC = self.ring(st, 'hcSc', 2, [128, 512], F32, psum=True)
            psP = self.ring(st, 'hcSp', 2, [128, 512], F32, psum=True)
            psO = self.ring(st, 'hcO', 2, [128, 512], F32, psum=True)
            psB = self.ps(st, 'hcB')
            Pc = self.ring(st, 'hcPc', 2, [128, 512], BF16)
            Pp = self.ring(st, 'hcPp', 2, [128, 512], BF16)
            accs = self.ring(st, 'hcacc', 2, [65, T], F32)
            fr = self.mk_fr(st, 'hcfr')
            ostg = self.ring(st, 'hcost', 2, [128, 512], BF16)
            heads = [(s, g) for s in range(4) for g in range(3)]

            def load_head(n):
                s, g = heads[n]
                q, k = qTs.next(), kTs.next()
                self.load_rope_head(q, 0, self.qcT, g * 4 + s)
                self.load_rope_head(k, 0, self.kcT, g * 4 + s)
                return q, k
            nxt = load_head(0)
            for n, (s, g) in enumerate(heads):
                q, k = nxt
                if n + 1 < len(heads):
                    nxt = load_head(n + 1)
                if g == 0:
                    acc = accs.next()
                d = dil[g]
                nb = 32 // d
                V = Vg[g]

                def tsl(cl, b, d=d):
                    start = b * 128 * d + cl
                    return slice(start, start + 127 * d + 1, d)
                state = {}

                def qk(u, q=q, k=k, state=state, nb=nb, tsl=tsl):
                    pc, pp = psC.next(), psP.next()
                    self.mm(pc, pc[:], c['ident_b'], c['ident_b'][:], cur4, cur4[:], True, False)
                    self.mm(pp, pp[:], c['ident_b'], c['ident_b'][:], prev4, prev4[:], True, False)
                    for bb in range(4):
                        L = 4 * u + bb
                        cl, b = L // nb, L % nb
                        cs = slice(bb * 128, (bb + 1) * 128)
                        self.mm(pc, pc[:, cs], k, k[0:64, tsl(cl, b)], q, q[0:64, tsl(cl, b)], False, True)
                        if b > 0:
                            self.mm(pp, pp[:, cs], k, k[0:64, tsl(cl, b - 1)], q, q[0:64, tsl(cl, b)], False, True)
                    a, b_ = Pc.next(), Pp.next()
                    self.act(a, a[:], pc, pc[:], AF.Exp, scale=0.125)
                    self.act(b_, b_[:], pp, pp[:], AF.Exp, scale=0.125)
                    state[u] = (a, b_)

                def pv(u, s=s, g=g, V=V, state=state, nb=nb, d=d, acc=acc, tsl=tsl):
                    a, b_ = state.pop(u)
                    po = psO.next()
                    for bb in range(4):
                        L = 4 * u + bb
                        cl, b = L // nb, L % nb
                        cs = slice(bb * 128, (bb + 1) * 128)
                        self.mm(po, po[0:65, cs], V, V[:, L, s * 65:(s + 1) * 65], a, a[:, cs], True, b == 0)
                        if b > 0:
                            self.mm(po, po[0:65, cs], V, V[:, L - 1, s * 65:(s + 1) * 65], b_, b_[:, cs], False, True)
                    if d == 16:
                        runs = [(0, 256, (4 * u) // nb), (256, 512, (4 * u) // nb + 1)]
                    else:
                        runs = [(0, 512, (4 * u) // nb)]
                    for (c0, c1, cl) in runs:
                        b0 = (4 * u + c0 // 128) % nb
                        start = b0 * 128 * d + cl
                        cnt = c1 - c0
                        sl = slice(start, start + (cnt - 1) * d + 1, d)
                        if g == 0:
                            self.cp(acc, acc[0:65, sl], po, po[0:65, c0:c1])
                        else:
                            self.tt(acc, acc[0:65, sl], acc, acc[0:65, sl], po, po[0:65, c0:c1], ALU.add)
                pipeline(8, qk, pv)
                if g == 2:
                    for j in range(NT):
                        qs = slice(j * TS, (j + 1) * TS)
                        fl, fb = fr['l'], fr['b']
                        self.act(fl, fl[64:65, :], acc, acc[64:65, qs], AF.Ln)
                        self.act(fl, fl[64:65, :], fl, fl[64:65, :], AF.Exp, scale=-1.0)
                        self.cp(fb, fb[64:65, :], fl, fl[64:65, :])
                        self.tt(fl, fl[64:65, :], fl, fl[64:65, :], fb, fb[64:65, :], ALU.subtract)
                        self.cp(fb, fb[96:97, :], fl, fl[64:65, :])
                        self.mm(psB, psB[0:64, :], c['ones_b'], c['ones_b'][64:97, 0:64], fb, fb[64:97, :], True, True)
                        o = ostg.next()
                        self.tt(o, o[0:64, :], acc, acc[0:64, qs], psB, psB[0:64, :], ALU.mult)
                        self.stor(self.ocT, self.ocT.ap[s * 64:(s + 1) * 64, qs], o, o[0:64, :])
            self.S.barrier()

    def precast_w(self, key, src):
        R, C = src.shape
        sp = 1
        while C // sp > 2048:
            sp *= 2
        dst = Buf(self.nc.dram_tensor("wb_%s_%d" % key, [R, C], BF16, kind="Internal").ap(), disjoint=True)
        sv = src.rearrange("r (s c) -> (r s) c", s=sp)
        dv = dst.ap.rearrange("r (s c) -> (r s) c", s=sp)
        R2 = R * sp
        for r0 in range(0, R2, 1024):
            r1 = min(R2, r0 + 1024)
            self.S.dma('bg', dv[r0:r1].rearrange("(p k) c -> p k c", p=128),
                       sv[r0:r1].rearrange("(p k) c -> p k c", p=128), writes=[dst])
        self.wbf[key] = dst

    def issue_bg(self, phase):
        srcs = {'w_gate': self.w_gate, 'w_ba': self.w_ba, 'w_bb': self.w_bb, 'w_bc': self.w_bc,
                'w_out': self.w_out, 'w_xq': self.w_xq, 'w_xo': self.w_xo, 'w_xk': self.w_xk,
                'w_xv': self.w_xv, 'w_in': self.w_in}
        c1a = ['w_gate', 'w_ba', 'w_bb', 'w_bc', 'w_out']
        c1b = ['w_xq', 'w_xo', 'w_xk', 'w_xv']
        sched = {'HA0': [(n, 0) for n in c1a], 'HB0': [(n, 0) for n in c1b] + [('experts', 0)],
                 'C1a0': [('w_in', 1)], 'C1b0': [(n, 1) for n in c1a],
                 'C20': [(n, 1) for n in c1b] + [('experts', 1)]}
        if True:
            return
        for key in sched.get(phase, []):
            nm, l = key
            if l >= self.layers:
                continue
            if nm == 'experts':
                self.precast(l)
                self.experts_cast.add(l)
            else:
                self.precast_w(key, srcs[nm][l])

    def load_w(self, st, name, src, kc, n, key=None, defer=False):
        w = self.sb(st, name, [128, kc, n], BF16, disjoint=True)
        if defer:
            self._deferred_loads.append(lambda: self._issue_w(w, src, kc, n, key))
            return w
        self._issue_w(w, src, kc, n, key)
        return w

    def _issue_w(self, w, src, kc, n, key):
        if key is not None and key in self.wbf:
            b = self.wbf[key]
            for k0 in range(0, kc, 2):
                k1 = min(kc, k0 + 2)
                self.ld(w, w[:, k0:k1, :], b.ap[k0 * 128:k1 * 128, :].rearrange("(c p) n -> p c n", p=128), src_b=b)
            return w
        step = max(1, 2048 // n)
        for k0 in range(0, kc, step):
            k1 = min(kc, k0 + step)
            if n <= 2048:
                self.ld(w, w[:, k0:k1, :], src[k0 * 128:k1 * 128, :].rearrange("(c p) n -> p c n", p=128), q='pool')
            else:
                for n0 in range(0, n, 1536):
                    n1 = min(n, n0 + 1536)
                    self.ld(w, w[:, k0, n0:n1], src[k0 * 128:(k0 + 1) * 128, n0:n1], q='pool')
        return w

    def ln_tiles(self, st, l, idx):
        g = self.sb(st, 'lng', [128, D], F32)
        b = self.sb(st, 'lnb', [128, D], F32)
        self.ld(g, g[:], self.ln_g[l, idx, :].partition_broadcast(128))
        self.ld(b, b[:], self.ln_b[l, idx, :].partition_broadcast(128))
        sm = {'stats': self.sb(st, 'lnst', [128, 12], F32), 'mv': self.sb(st, 'lnmv', [128, 2], F32),
              'sc': self.sb(st, 'lnsc', [128, 4], F32)}
        return g, b, sm

    def phaseC1a(self, l):
        self.issue_bg('C1a%d' % l)
        xsrc = self.x if l == 0 else self.xres.ap
        xsrc_b = None if l == 0 else self.xres
        with contextlib.ExitStack() as st:
            c = self.consts(st)
            Wg = self.load_w(st, 'wgate', self.w_gate[l], 8, 3072, key=('w_gate', l))
            Wa = self.load_w(st, 'wba', self.w_ba[l], 4, 1024, key=('w_ba', l))
            Wb = self.load_w(st, 'wbb', self.w_bb[l], 4, 1024, key=('w_bb', l))
            Wc = self.load_w(st, 'wbc', self.w_bc[l], 2, 1024, key=('w_bc', l))
            Wo = self.load_w(st, 'wout', self.w_out[l], 8, 1024, key=('w_out', l))
            bg = self.sb(st, 'bgate', [128, 24], F32)
            self.ld(bg, bg[:], self.b_gate[l])
            lg_, lb_, sm = self.ln_tiles(st, l, 0)
            xTt = self.ring(st, 'c1xT', 2, [128, 8, TS], BF16)
            oat = self.ring(st, 'c1oa', 2, [128, 4, TS], BF16)
            obt = self.ring(st, 'c1ob', 2, [128, 4, TS], BF16)
            oct_ = self.ring(st, 'c1oc', 2, [128, 2, TS], BF16)
            xrs = self.ring(st, 'c1xr', 1, [128, 4, D], F32)
            mT = self.sb(st, 'c1mT', [128, 8, TS], BF16)
            sg = self.ring(st, 'c1sg', 3, [128, TS], F32)
            mm_ = self.ring(st, 'c1mm', 3, [128, TS], F32)
            x1Tt = self.ring(st, 'c1x1T', 2, [128, 8, TS], BF16)
            pss = self.ring(st, 'c1ps', 8, [128, 512], F32, psum=True)

            def loads(t):
                tc_ = slice(t * TS, (t + 1) * TS)
                a, b, cc_, xt = oat.next(), obt.next(), oct_.next(), xTt.next()
                self.ld(xt, xt[:], self.xT.ap[:, tc_].rearrange("(c p) t -> p c t", p=128), src_b=self.xT)
                self.ld(a, a[:], self.oaT.ap[:, tc_].rearrange("(c p) t -> p c t", p=128), src_b=self.oaT)
                self.ld(b, b[:], self.obT.ap[:, tc_].rearrange("(c p) t -> p c t", p=128), src_b=self.obT)
                self.ld(cc_, cc_[:], self.ocT.ap[:, tc_].rearrange("(c p) t -> p c t", p=128), src_b=self.ocT)
                return a, b, cc_, xt
            rr4 = self.ring(st, 'c1r4', 4, [128, D], F32)
            tiles = {}

            prea = {}
            lnq = []

            def stageA(t):
                a, b, cc_, xt = prea.pop(t)
                for ch in range(8):
                    cs = slice(ch * 128, (ch + 1) * 128)
                    ms = []
                    for bi, (W, o, kc) in enumerate(((Wa, a, 4), (Wb, b, 4), (Wc, cc_, 2))):
                        pg = pss.next()
                        for k in range(8):
                            self.mm(pg, pg[:], Wg, Wg[:, k, bi * 1024 + ch * 128:bi * 1024 + (ch + 1) * 128],
                                    xt, xt[:, k, :], k == 0, k == 7)
                        pb = pss.next()
                        for k in range(kc):
                            self.mm(pb, pb[:], W, W[:, k, cs], o, o[:, k, :], k == 0, k == kc - 1)
                        s_ = sg.next()
                        self.act(s_, s_[:], pg, pg[:], AF.Sigmoid, extra_reads=[bg],
                                 bias=bg[:, bi * 8 + ch:bi * 8 + ch + 1])
                        m = mm_.next()
                        self.tt(m, m[:], s_, s_[:], pb, pb[:], ALU.mult)
                        ms.append(m)
                    self.tt(ms[0], ms[0][:], ms[0], ms[0][:], ms[1], ms[1][:], ALU.add)
                    self.tt(mT, mT[:, ch, :], ms[0], ms[0][:], ms[2], ms[2][:], ALU.add)
                    if ch % 2 == 1 and lnq:
                        lnq.pop(0)()

            def stageB(t):
                tc_ = slice(t * TS, (t + 1) * TS)
                xr = xrs.next()
                self.ld(xr, xr[:], xsrc[tc_, :].rearrange("(s p) d -> p s d", p=128), src_b=xsrc_b)
                rs = []
                for sub in range(4):
                    r = rr4.next()
                    rs.append(r)
                    for half in range(2):
                        p = pss.next()
                        for k in range(8):
                            self.mm(p, p[:], mT, mT[:, k, sub * 128:(sub + 1) * 128], Wo,
                                    Wo[:, k, half * 512:(half + 1) * 512], k == 0, k == 7)
                        self.stt(r, r[:, half * 512:(half + 1) * 512], xr, xr[:, sub, half * 512:(half + 1) * 512],
                                 ALPHA, p, p[:], ALU.mult, ALU.add)
                for sub in range(4):
                    def lnsub(r=rs[sub], sub=sub, t=t):
                        self.layer_norm(sm, r, r[:], lg_, lb_, r, r[:])
                        rows = slice(t * TS + sub * 128, t * TS + (sub + 1) * 128)
                        self.stor(self.x1, self.x1.ap[rows, :], r, r[:])
                    lnq.append(lnsub)
                tiles[t] = rs

            def stageT(t):
                tc_ = slice(t * TS, (t + 1) * TS)
                rs = tiles.pop(t)
                x1T = x1Tt.next()
                for sub in range(4):
                    self.transpose_to_xT(c, rs[sub], rs[sub][:], pss, x1T, sub)
                self.stor(self.x1T, self.x1T.ap[:, tc_].rearrange("(c p) t -> p c t", p=128), x1T, x1T[:])
            prea[0] = loads(0)
            stageA(0)
            for t in range(NT):
                if t + 1 < NT:
                    prea[t + 1] = loads(t + 1)
                stageB(t)
                if t + 1 < NT:
                    stageA(t + 1)
                while lnq:
                    lnq.pop(0)()
                stageT(t)
            self.S.barrier()

    def phaseC1b(self, l):
        self.issue_bg('C1b%d' % l)
        BIG = 1.0e4
        with contextlib.ExitStack() as st:
            c = self.consts(st)
            Wq = self.load_w(st, 'wxq', self.w_xq[l], 8, 1024, key=('w_xq', l), defer=True)
            Wo = self.load_w(st, 'wxo', self.w_xo[l], 8, 1024, key=('w_xo', l), defer=True)
            KmT = self.sb(st, 'KmT', [128, 8, 256], BF16)
            Vm = self.sb(st, 'Vm', [128, 2, 1024], BF16)
            pss = self.ring(st, 'cbps', 8, [128, 512], F32, psum=True)
            with contextlib.ExitStack() as st2:
                Wk = self.load_w(st2, 'wxk', self.w_xk[l], 8, 1024, key=('w_xk', l))
                Wv = self.load_w(st2, 'wxv', self.w_xv[l], 8, 1024, key=('w_xv', l))
                memf = self.sb(st2, 'memf', [128, 2, D], F32)
                self.ld(memf, memf[:], self.mem.rearrange("(s p) d -> p s d", p=128))
                while self._deferred_loads:
                    self._deferred_loads.pop(0)()
                memT = self.sb(st2, 'memT', [128, 8, 256], BF16)
                for ks in range(2):
                    for half in range(2):
                        p = pss.next()
                        for k in range(4):
                            cc = half * 4 + k
                            self.S.op('pe', lambda e, p=p, k=k, cc=cc, ks=ks: e.transpose(
                                p[:, k * 128:(k + 1) * 128], memf[:, ks, cc * 128:(cc + 1) * 128], c['ident_f'][:]),
                                reads=[memf, c['ident_f']], writes=[p])
                        self.act(memT, memT[:, half * 4:half * 4 + 4, ks * 128:(ks + 1) * 128], p,
                                 p[:].rearrange("p (k t) -> p k t", k=4), AF.Copy)
                for cc in range(8):
                    p = pss.next()
                    for k in range(8):
                        self.mm(p, p[:, 0:256], Wk, Wk[:, k, cc * 128:(cc + 1) * 128], memT, memT[:, k, :], k == 0, k == 7)
                    self.act(KmT, KmT[:, cc, :], p, p[:, 0:256], AF.Copy)
                for ks in range(2):
                    for half in range(2):
                        p = pss.next()
                        for k in range(8):
                            self.mm(p, p[:], memT, memT[:, k, ks * 128:(ks + 1) * 128], Wv,
                                    Wv[:, k, half * 512:(half + 1) * 512], k == 0, k == 7)
                        self.act(Vm, Vm[:, ks, half * 512:(half + 1) * 512], p, p[:], AF.Copy)
                self.S.barrier()
            lg_, lb_, sm = self.ln_tiles(st, l, 1)
            wr = self.sb(st, 'wr', [128, 8, 20], F32)
            self.ld(wr, wr[:], self.w_r[l].rearrange("(c p) n -> p c n", p=128))
            br = self.sb(st, 'br', [128, 20], F32)
            self.ld(br, br[:], self.b_r[l, 0, :].partition_broadcast(128))
            x1Tt = self.ring(st, 'cbx1T', 2, [128, 8, TS], BF16)
            x1s = self.ring(st, 'cbx1', 2, [128, 4, D], F32)
            qxT = self.sb(st, 'cbqx', [128, 8, TS], BF16)
            PT = self.ring(st, 'cbPT', 4, [128, TS], BF16)
            rden = self.ring(st, 'cbrd', 2, [128, TS], F32)
            oxT = self.sb(st, 'cbox', [128, 8, TS], BF16)
            rr = self.ring(st, 'cbr', 2, [128, D], F32)
            x2o = self.ring(st, 'cbx2', 2, [128, D], F32)
            x2Tt = self.ring(st, 'cbx2T', 2, [128, 8, TS], BF16)
            x2Tf = self.ring(st, 'cbx2Tf', 2, [128, 8, 128], F32)
            rt = self.ring(st, 'cbrt', 2, [128, 128], F32)
            gTt = self.ring(st, 'cbgT', 2, [16, TS], F32)

            def loads(t):
                tc_ = slice(t * TS, (t + 1) * TS)
                xt, x1 = x1Tt.next(), x1s.next()
                self.ld(xt, xt[:], self.x1T.ap[:, tc_].rearrange("(c p) t -> p c t", p=128), src_b=self.x1T)
                self.ld(x1, x1[:], self.x1.ap[tc_, :].rearrange("(s p) d -> p s d", p=128), src_b=self.x1)
                return xt, x1
            rr4 = self.ring(st, 'cbr4', 4, [128, D], F32)
            rt4 = self.ring(st, 'cbrt4', 4, [128, 128], F32)
            PT8 = self.ring(st, 'cbPT8', 4, [128, TS], BF16)
            pending = []
            tl = {}

            pre = {}
            lnq2 = []

            def S1(t):
                xt, x1 = pre.pop(t)
                tl[t] = x1
                for cc in range(8):
                    p = pss.next()
                    for k in range(8):
                        self.mm(p, p[:], Wq, Wq[:, k, cc * 128:(cc + 1) * 128], xt, xt[:, k, :], k == 0, k == 7)
                    self.act(qxT, qxT[:, cc, :], p, p[:], AF.Copy)
                    if cc % 2 == 1 and lnq2:
                        lnq2.pop(0)()
                hst = {}

                def sc(hh):
                    Ps = []
                    for ks in range(2):
                        p = pss.next()
                        for c2 in range(2):
                            self.mm(p, p[:], KmT, KmT[:, 2 * hh + c2, ks * 128:(ks + 1) * 128], qxT,
                                    qxT[:, 2 * hh + c2, :], c2 == 0, c2 == 1)
                        P = PT8.next()
                        self.act(P, P[:], p, p[:], AF.Exp, scale=1.0 / 16.0)
                        Ps.append(P)
                    hst[hh] = Ps

                def pvx(hh):
                    Ps = hst.pop(hh)
                    pd = pss.next()
                    for ks in range(2):
                        self.mm(pd, pd[:], c['ones_b'], c['ones_b'][:], Ps[ks], Ps[ks][:], ks == 0, ks == 1)
                    rd = rden.next()
                    self.recip(rd, rd[:], pd, pd[:])
                    for c2 in range(2):
                        pn = pss.next()
                        for ks in range(2):
                            self.mm(pn, pn[:], Vm, Vm[:, ks, (2 * hh + c2) * 128:(2 * hh + c2 + 1) * 128], Ps[ks],
                                    Ps[ks][:], ks == 0, ks == 1)
                        self.tt(oxT, oxT[:, 2 * hh + c2, :], pn, pn[:], rd, rd[:], ALU.mult)
                pipeline(4, sc, pvx, depth=1)

            def S2LN(t):
                x1 = tl.pop(t)
                rs = []
                for sub in range(4):
                    r = rr4.next()
                    rs.append(r)
                    for half in range(2):
                        p = pss.next()
                        for k in range(8):
                            self.mm(p, p[:], oxT, oxT[:, k, sub * 128:(sub + 1) * 128], Wo,
                                    Wo[:, k, half * 512:(half + 1) * 512], k == 0, k == 7)
                        self.stt(r, r[:, half * 512:(half + 1) * 512], x1, x1[:, sub, half * 512:(half + 1) * 512],
                                 ALPHA, p, p[:], ALU.mult, ALU.add)
                for sub in range(4):
                    def lnsub(xo=rs[sub], sub=sub, t=t):
                        self.layer_norm(sm, xo, xo[:], lg_, lb_, xo, xo[:])
                        rows = slice(t * TS + sub * 128, t * TS + (sub + 1) * 128)
                        self.stor(self.x2, self.x2.ap[rows, :], xo, xo[:])
                    lnq2.append(lnsub)
                return rs

            def TR(t, rs):
                tc_ = slice(t * TS, (t + 1) * TS)
                x2T = x2Tt.next()
                gT = gTt.next()
                ws = []
                for sub in range(4):
                    xo = rs[sub]
                    xf = x2Tf.next()
                    self.transpose_to_xT(c, xo, xo[:], pss, x2T, sub, f32_b=xf)
                    pl = pss.next()
                    for k in range(8):
                        self.mm(pl, pl[:, 0:20], xf, xf[:, k, :], wr, wr[:, k, :], k == 0, k == 7)
                    w = rt4.next()
                    ws.append(w)
                    self.tt(w, w[:, 0:20], pl, pl[:, 0:20], br, br[:], ALU.add)
                self.stor(self.x2T, self.x2T.ap[:, tc_].rearrange("(c p) t -> p c t", p=128), x2T, x2T[:])
                for sub in range(4):
                    w = ws[sub]
                    self.S.op('dve', lambda e, w=w: e.tensor_reduce(out=w[:, 20:21], in_=w[:, 0:4], axis=AX.X, op=ALU.max),
                              reads=[w], writes=[w])
                    self.ts(w, w[:, 24:28], w, w[:, 0:4], w[:, 20:21], None, ALU.is_equal)
                    self.ts(w, w[:, 21:22], w, w[:, 20:21], -1.0, None, ALU.mult)
                    self.act(w, w[:, 118:122], w, w[:, 0:4], AF.Exp, bias=w[:, 21:22], accum_out=w[:, 22:23])
                    self.recip(w, w[:, 23:24], w, w[:, 22:23])
                    self.ts(w, w[:, 28:32], w, w[:, 24:28], -1.0, BIG, ALU.add, ALU.mult)
                    self.tt(w, w[:, 32:48].rearrange("p (g e) -> p g e", g=4), w,
                            w[:, 4:20].rearrange("p (g e) -> p g e", g=4), w,
                            w[:, 28:32].unsqueeze(2).to_broadcast([128, 4, 4]), ALU.add)
                    self.S.op('dve', lambda e, w=w: e.tensor_reduce(out=w[:, 48:49], in_=w[:, 32:48], axis=AX.X, op=ALU.max),
                              reads=[w], writes=[w])
                    self.ts(w, w[:, 50:66], w, w[:, 32:48], w[:, 48:49], None, ALU.is_equal)
                    self.stt(w, w[:, 66:82], w, w[:, 50:66], -BIG, w, w[:, 32:48], ALU.mult, ALU.add)
                    self.S.op('dve', lambda e, w=w: e.tensor_reduce(out=w[:, 49:50], in_=w[:, 66:82], axis=AX.X, op=ALU.max),
                              reads=[w], writes=[w])
                    self.ts(w, w[:, 82:98], w, w[:, 66:82], w[:, 49:50], None, ALU.is_equal)
                    self.tt(w, w[:, 98:99], w, w[:, 49:50], w, w[:, 48:49], ALU.subtract)
                    self.act(w, w[:, 99:100], w, w[:, 98:99], AF.Exp)
                    self.ts(w, w[:, 100:101], w, w[:, 99:100], 1.0, None, ALU.add)
                    self.recip(w, w[:, 100:101], w, w[:, 100:101])
                    self.tt(w, w[:, 101:102], w, w[:, 99:100], w, w[:, 100:101], ALU.mult)
                    self.ts(w, w[:, 100:102], w, w[:, 100:102], w[:, 23:24], None, ALU.mult)
                    self.ts(w, w[:, 102:118], w, w[:, 50:66], w[:, 100:101], None, ALU.mult)
                    self.stt(w, w[:, 102:118], w, w[:, 82:98], w[:, 101:102], w, w[:, 102:118], ALU.mult, ALU.add)


                def fin(ws=ws, gT=gT, tc_=tc_):
                    for sub in range(4):
                        w = ws[sub]
                        pt = pss.next()
                        self.S.op('pe', lambda e, pt=pt, w=w: e.transpose(pt[0:16, 0:128], w[:, 102:118], c['ident_f'][:]),
                                  reads=[w, c['ident_f']], writes=[pt])
                        self.act(gT, gT[0:16, sub * 128:(sub + 1) * 128], pt, pt[0:16, 0:128], AF.Copy)
                    self.stor(self.gateT, self.gateT.ap[:, tc_], gT, gT[:])
                pending.append(fin)
            pre[0] = loads(0)
            S1(0)
            for t in range(NT):
                if t + 1 < NT:
                    pre[t + 1] = loads(t + 1)
                rs = S2LN(t)
                while pending:
                    pending.pop(0)()
                if t + 1 < NT:
                    S1(t + 1)
                while lnq2:
                    lnq2.pop(0)()
                TR(t, rs)
            while pending:
                pending.pop(0)()
            self.S.barrier()

    def precast(self, l):
        for e in range(16):
            for src, dst in ((self.w_eg, self.wegb), (self.w_eu, self.weub), (self.w_ed, self.wedb)):
                self.S.dma('bg', dst.ap[l, e].rearrange("a b -> (a b)").rearrange("(p n) -> p n", p=128),
                           src[l, e].rearrange("a b -> (a b)").rearrange("(p n) -> p n", p=128), writes=[dst])

    def phaseC2(self, l):
        self.issue_bg('C2%d' % l)
        if l not in self.experts_cast:
            self.precast(l)
            self.experts_cast.add(l)
        last = (l == DEPTH - 1)
        with contextlib.ExitStack() as st:
            c = self.consts(st)
            sel = self.sb(st, 'sel', [48, 2048], BF16, disjoint=True)
            self.memset(sel, sel[:], 0.0)
            self.ld(sel, sel[0:16, :], self.c_sel, q='pool')
            self.ld(sel, sel[32:48, :], self.c_sel, q='pool')
            g2s = self.ring(st, 'c2g2', 2, [48, TS], BF16)
            for b_ in g2s.bufs:
                self.memset(b_, b_[:], 0.0)
            grem = self.sb(st, 'c2grem', [16, TS], F32)
            lg_, lb_, sm = self.ln_tiles(st, l, 2)
            x2Tt = self.ring(st, 'c2xT', 2, [128, 8, TS], BF16)
            ys = self.ring(st, 'c2y', 3, [128, 4, D], F32)
            gTt = self.ring(st, 'c2gT', 2, [16, TS], F32)
            Wgs = self.ring(st, 'c2wg', 3, [128, 8, 256], BF16)
            Wus = self.ring(st, 'c2wu', 3, [128, 8, 256], BF16)
            Wds = self.ring(st, 'c2wd', 10, [128, 2, D], BF16)
            hs = self.ring(st, 'c2h', 8, [128, 2, TS], BF16)
            sgs = self.ring(st, 'c2sg', 2, [128, TS], F32)
            tms = self.ring(st, 'c2tm', 2, [128, TS], F32)
            x3Tt = self.ring(st, 'c2x3T', 2, [128, 8, TS], BF16)
            psGU = self.ring(st, 'c2gu', 4, [128, 512], F32, psum=True)
            psG = self.ring(st, 'c2G', 2, [128, 512], F32, psum=True)
            psD = self.ring(st, 'c2D', 2, [128, 512], F32, psum=True)

            def loads(t):
                tc_ = slice(t * TS, (t + 1) * TS)
                xt, y, g = x2Tt.next(), ys.next(), gTt.next()
                self.ld(xt, xt[:], self.x2T.ap[:, tc_].rearrange("(c p) t -> p c t", p=128), src_b=self.x2T)
                self.ld(y, y[:], self.x2.ap[tc_, :].rearrange("(s p) d -> p s d", p=128), src_b=self.x2)
                self.ld(g, g[:], self.gateT.ap[:, tc_], src_b=self.gateT)
                return xt, y, g

            def loadw(e):
                wg_, wu_, wd_ = Wgs.next(), Wus.next(), Wds.next()
                self.ld(wg_, wg_[:], self.wegb.ap[l, e].rearrange("(c p) n -> p c n", p=128), src_b=self.wegb)
                self.ld(wu_, wu_[:], self.weub.ap[l, e].rearrange("(c p) n -> p c n", p=128), src_b=self.weub)
                self.ld(wd_, wd_[:], self.wedb.ap[l, e].rearrange("(c p) n -> p c n", p=128), src_b=self.wedb)
                return wg_, wu_, wd_
            pending = []
            nxt = loads(0)
            wq = [loadw(0), loadw(1)]
            for t in range(NT):
                xt, y, gT = nxt
                if t + 1 < NT:
                    nxt = loads(t + 1)
                tc_ = slice(t * TS, (t + 1) * TS)
                for sub in range(4):
                    self.ts(y, y[:, sub, :], y, y[:, sub, :], ALPHA, None, ALU.mult)
                state = {}
                g2 = g2s.next()
                self.cp(g2, g2[0:16, :], gT, gT[:])
                self.tt(grem, grem[:], gT, gT[:], g2, g2[0:16, :], ALU.subtract)
                self.cp(g2, g2[32:48, :], grem, grem[:])
                gT = g2

                def gu(e, xt=xt, gT=gT, state=state, t=t):
                    if pending and e >= 1:
                        pending.pop(0)()
                    W = wq.pop(0)
                    nid = t * 16 + e + 2
                    if nid < NT * 16:
                        wq.append(loadw(nid % 16))
                    wg_, wu_, wd_ = W
                    pG = psG.next()
                    self.mm(pG, pG[:], sel, sel[0:48, e * 128:(e + 1) * 128], gT, gT[0:48, :], True, True)
                    h = hs.next()
                    for fc in range(2):
                        fs = slice(fc * 128, (fc + 1) * 128)
                        pg, pu = psGU.next(), psGU.next()
                        for k in range(8):
                            self.mm(pg, pg[:], wg_, wg_[:, k, fs], xt, xt[:, k, :], k == 0, k == 7)
                        for k in range(8):
                            self.mm(pu, pu[:], wu_, wu_[:, k, fs], xt, xt[:, k, :], k == 0, k == 7)
                        s_ = sgs.next()
                        self.act(s_, s_[:], pg, pg[:], AF.Silu)
                        tm = tms.next()
                        self.tt(tm, tm[:], s_, s_[:], pu, pu[:], ALU.mult)
                        self.tt(h, h[:, fc, :], tm, tm[:], pG, pG[:], ALU.mult)
                    state[e] = (h, wd_)

                GE = 4

                def gug(gi, gu=gu):
                    for e in range(gi * GE, (gi + 1) * GE):
                        gu(e)

                def down(gi, y=y, state=state):
                    items = [state.pop(e) for e in range(gi * GE, (gi + 1) * GE)]
                    for sub in range(4):
                        for half in range(2):
                            p = psD.next()
                            for idx, (h, wd_) in enumerate(items):
                                for fc in range(2):
                                    self.mm(p, p[:], h, h[:, fc, sub * 128:(sub + 1) * 128], wd_,
                                            wd_[:, fc, half * 512:(half + 1) * 512], idx == 0 and fc == 0,
                                            idx == GE - 1 and fc == 1)
                            ysl = y[:, sub, half * 512:(half + 1) * 512]
                            self.tt(y, ysl, y, ysl, p, p[:], ALU.add)
                pipeline(16 // GE, gug, down)
                for sub in range(4):
                    def lnsub(sub=sub, y=y, t=t):
                        self.layer_norm(sm, y, y[:, sub, :], lg_, lb_, y, y[:, sub, :], gb_eng='dve')
                        rows = slice(t * TS + sub * 128, t * TS + (sub + 1) * 128)
                        dstb = self.out if last else self.xres
                        self.stor(dstb, dstb.ap[rows, :], y, y[:, sub, :], q='pool')
                    pending.append(lnsub)
                if not last:
                    def trs(y=y, tc_=tc_):
                        x3T = x3Tt.next()
                        for sub in range(4):
                            self.transpose_to_xT(c, y, y[:, sub, :], psGU, x3T, sub)
                        self.stor(self.xT, self.xT.ap[:, tc_].rearrange("(c p) t -> p c t", p=128), x3T, x3T[:],
                                  q='pool')
                    pending.append(trs)
            while pending:
                pending.pop(0)()
            self.S.barrier()

    def build(self):
        self.declare()
        if self.want('0'):
            self.phase0()
        for l in range(self.layers):
            if self.want('P%d' % l):
                self.phaseP(l)
            if self.want('HA%d' % l):
                self.phaseHA(l)
            if self.want('HB%d' % l):
                self.phaseHB(l)
            if self.want('HC%d' % l):
                self.phaseHC(l)
            if self.want('C1a%d' % l):
                self.phaseC1a(l)
            if self.want('C1b%d' % l):
                self.phaseC1b(l)
            if self.want('C2%d' % l):
                self.phaseC2(l)
        self.S.barrier(final=True)


def _rope_perm():
    perm = np.arange(NCOL)
    def blk(base, nheads):
        out = []
        for m in range(nheads // 4):
            a = [base + (4 * m + jj) * 64 + i for jj in range(4) for i in range(32)]
            b = [base + (4 * m + jj) * 64 + 32 + i for jj in range(4) for i in range(32)]
            out += a + b
        return out
    perm[0:512] = blk(0, 8)
    perm[512:1024] = blk(512, 8)
    perm[3080:3848] = blk(3080, 12)
    perm[3848:4616] = blk(3848, 12)
    return perm


def _consts():
    ident = np.eye(128, dtype=np.float32)
    k = np.arange(128)[:, None]
    q = np.arange(512)[None, :]
    masks = np.concatenate([np.where(128 * a + k <= q, 0.0, NEG).astype(np.float32) for a in range(4)], axis=1)
    qq = np.arange(128)[None, :]
    mprev = np.where(k >= qq, 0.0, NEG).astype(np.float32)
    sel = np.zeros((16, 16 * 128), np.float32)
    for e in range(16):
        sel[e, e * 128:(e + 1) * 128] = 1.0
    invf = (10000.0 ** (-(np.arange(32, dtype=np.float32)) / 32.0)).astype(np.float32)
    invf = np.tile(invf, 4).reshape(128, 1)
    return dict(c_ident=ident, c_masks=np.ascontiguousarray(masks), c_mprev=mprev, c_sel=sel, c_invf=invf)


def make_in_maps(inp, cores=range(8)):
    f = lambda a: np.ascontiguousarray(np.asarray(a, dtype=np.float32))
    sh = {}
    sh["w_in"] = np.ascontiguousarray(f(inp["w_in"])[:, :, _rope_perm()])
    sh["b_forget"] = f(inp["b_forget"]).reshape(DEPTH, 8, 1)
    sh["diff_lambda"] = f(inp["diff_lambda"]).reshape(DEPTH, 1, 256)
    sh["diff_subln"] = f(inp["diff_subln"]).reshape(DEPTH, 128, 1)
    for k in ["w_branch_a", "w_branch_b", "w_branch_c", "w_gate", "w_out", "w_xq", "w_xk", "w_xv", "w_xo",
              "ln_g", "ln_b"]:
        sh[k] = f(inp[k])
    sh["b_gate"] = np.ascontiguousarray(f(inp["b_gate"]).reshape(DEPTH, 24, 128).transpose(0, 2, 1))
    wre = f(inp["w_route_expert"]).transpose(0, 2, 1, 3).reshape(DEPTH, D, 16)
    sh["w_r"] = np.ascontiguousarray(np.concatenate([f(inp["w_route_group"]), wre], axis=2))
    sh["b_r"] = np.ascontiguousarray(np.concatenate([f(inp["b_route_group"]),
                                                     f(inp["b_route_expert"]).reshape(DEPTH, 16)], axis=1)
                                     ).reshape(DEPTH, 1, 20)
    sh["w_eg"] = f(inp["w_expert_gate"]).reshape(DEPTH, 16, D, 256)
    sh["w_eu"] = f(inp["w_expert_up"]).reshape(DEPTH, 16, D, 256)
    sh["w_ed"] = f(inp["w_expert_down"]).reshape(DEPTH, 16, 256, D)
    sh.update(_consts())
    x = f(inp["x"])
    mem = f(inp["mem"])
    pos = np.ascontiguousarray(np.asarray(inp["positions"], dtype=np.int32))
    maps = []
    for b in cores:
        m = dict(sh)
        m["x"] = x[b]
        m["mem"] = mem[b]
        m["pos"] = pos[b:b + 1]
        maps.append(m)
    return maps


def build_program(debug=False, layers=DEPTH, phases=None):
    nc = bass.Bass("TRN2", target_bir_lowering=False)
    with contextlib.ExitStack() as es:
        k = K(nc, es, debug=debug, layers=layers, phases=phases)
        k.build()
    return nc, k


def kernel(**inputs):
    nc, k = build_program()
    maps = make_in_maps(inputs)
    used = set(k.dram.keys())
    maps = [{n: v for n, v in m.items() if n in used} for m in maps]
    res = run_bass_kernel_spmd(nc, maps, core_ids=list(range(8)))
    return np.stack([np.asarray(r["out"], dtype=np.float32) for r in res.results], axis=0)
```
